# Optimizing a Trainium2 kernel written in Bass

```python
import jax, jax.numpy as jnp
from jax import lax
import numpy as np

D_MODEL = 1024
BATCH = 8
SEQ = 2048
DEPTH = 2

HEAD_DIM = 64
H_SB = 8
H_NSA = 8
NSA_KV_HEADS = 2
H_FOX = 8
H_MEM = 4
N_MEM = 256
N_BRANCH = 3
ROPE_THETA = 500000.0
ROPE_DIM = HEAD_DIM // 4
Q_BLOCK = 128
CMP_STRIDE = 16
CMP_LEN = 2 * CMP_STRIDE
CMP_HIDDEN = 256
SEL_BLOCK = 64
SEL_TOPK = 8
WINDOW = 512
D_FF = -(-(8 * D_MODEL) // (3 * 256)) * 256
W_SB = H_SB * HEAD_DIM
W_NSA = H_NSA * HEAD_DIM
W_NSA_KV = NSA_KV_HEADS * HEAD_DIM
W_FOX = H_FOX * HEAD_DIM
W_MEM = H_MEM * HEAD_DIM
IN_SIZES = (W_SB, W_SB, W_SB,
            W_NSA, W_NSA_KV, W_NSA_KV, W_NSA_KV, W_NSA_KV, W_NSA_KV, W_NSA_KV, 3 * H_NSA,
            W_FOX, W_FOX, W_FOX, H_FOX,
            N_BRANCH * D_MODEL)
D_IN = sum(IN_SIZES)

kernel_name = "hybrid_sb_nsa_fox_block"

F32 = jnp.float32


def rmsnorm(x, g, eps=1e-6):
    xf = x.astype(F32)
    y = xf * lax.rsqrt(jnp.mean(xf * xf, axis=-1, keepdims=True) + eps)
    return (y * g.astype(F32)).astype(x.dtype)


def masked_softmax(logits, mask):
    logits = jnp.where(mask, logits.astype(F32), -jnp.inf)
    m = jnp.max(logits, axis=-1, keepdims=True)
    m = jnp.where(jnp.isfinite(m), m, 0.0)
    e = jnp.where(mask, jnp.exp(logits - m), 0.0)
    den = jnp.sum(e, axis=-1, keepdims=True)
    return e / jnp.where(den > 0, den, 1.0)


def partial_rope(x, pos):
    half = ROPE_DIM // 2
    inv_freq = ROPE_THETA ** (-jnp.arange(half, dtype=F32) / half)
    ang = pos.astype(F32)[:, :, None] * inv_freq
    cos = jnp.cos(ang)[:, :, None, :].astype(x.dtype)
    sin = jnp.sin(ang)[:, :, None, :].astype(x.dtype)
    x1, x2, rest = x[..., :half], x[..., half:ROPE_DIM], x[..., ROPE_DIM:]
    return jnp.concatenate([x1 * cos - x2 * sin, x2 * cos + x1 * sin, rest], axis=-1)


def stick_breaking_attention(q, k, v):
    B, S, H, d = q.shape
    scale = d ** -0.5
    outs = []
    for i in range(S // Q_BLOCK):
        q0, q1 = i * Q_BLOCK, (i + 1) * Q_BLOCK
        z = jnp.einsum('bqhd,bkhd->bhqk', q[:, q0:q1], k[:, :q1]).astype(F32) * scale
        strict = jnp.arange(q1)[None, :] < jnp.arange(q0, q1)[:, None]
        log_keep = jnp.where(strict, jax.nn.log_sigmoid(-z), 0.0)
        later = lax.cumsum(log_keep, axis=3, reverse=True) - log_keep
        w = jnp.where(strict, jnp.exp(jax.nn.log_sigmoid(z) + later), 0.0)
        outs.append(jnp.einsum('bhqk,bkhd->bqhd', w.astype(v.dtype), v[:, :q1]))
    return jnp.concatenate(outs, axis=1)


def forgetting_attention(q, k, v, f_logit):
    B, S, H, d = q.shape
    scale = d ** -0.5
    c = jnp.cumsum(jax.nn.log_sigmoid(f_logit.astype(F32)), axis=1).transpose(0, 2, 1)
    outs = []
    for i in range(S // Q_BLOCK):
        q0, q1 = i * Q_BLOCK, (i + 1) * Q_BLOCK
        logits = (jnp.einsum('bqhd,bkhd->bhqk', q[:, q0:q1], k[:, :q1]).astype(F32) * scale
                  + c[:, :, q0:q1, None] - c[:, :, None, :q1])
        causal = jnp.arange(q1)[None, :] <= jnp.arange(q0, q1)[:, None]
        p = masked_softmax(logits, causal)
        outs.append(jnp.einsum('bhqk,bkhd->bqhd', p.astype(v.dtype), v[:, :q1]))
    return jnp.concatenate(outs, axis=1)


def compress_blocks(x, pe, w1, b1, w2):
    B, S, G, d = x.shape
    ch = x.reshape(B, S // CMP_STRIDE, CMP_STRIDE, G, d)
    blk = jnp.concatenate([ch[:, :-1], ch[:, 1:]], axis=2) + pe[None, None, :, None, :]
    n_cmp = blk.shape[1]
    flat = blk.transpose(0, 1, 3, 2, 4).reshape(B, n_cmp, G, CMP_LEN * d)
    return jax.nn.silu(flat @ w1 + b1) @ w2


def native_sparse_attention(q, kc, vc, ks, vs, kw, vw, gates, pos,
                            pe_k, w1_k, b1_k, w2_k, pe_v, w1_v, b1_v, w2_v):
    B, S, H, d = q.shape
    G = kc.shape[2]
    R = H // G
    scale = d ** -0.5
    t_all = jnp.arange(S)
    qg = q.reshape(B, S, G, R, d)
    qg_rope = partial_rope(q, pos).reshape(B, S, G, R, d)
    ks_r = partial_rope(ks, pos)
    kw_r = partial_rope(kw, pos)

    k_cmp = compress_blocks(kc, pe_k, w1_k, b1_k, w2_k)
    v_cmp = compress_blocks(vc, pe_v, w1_v, b1_v, w2_v)
    n_cmp = k_cmp.shape[1]
    cmp_start = jnp.arange(n_cmp) * CMP_STRIDE
    cmp_valid = (cmp_start + CMP_LEN - 1)[None, :] <= t_all[:, None]
    s_cmp = jnp.einsum('bsgrd,bcgd->bgrsc', qg, k_cmp).astype(F32) * scale
    p_cmp = masked_softmax(s_cmp, cmp_valid)
    o_cmp = jnp.einsum('bgrsc,bcgd->bsgrd', p_cmp.astype(v_cmp.dtype), v_cmp)

    n_sel = S // SEL_BLOCK
    sel_start = jnp.arange(n_sel) * SEL_BLOCK
    overlap = ((cmp_start[:, None] < sel_start[None, :] + SEL_BLOCK)
               & (cmp_start[:, None] + CMP_LEN > sel_start[None, :])).astype(F32)
    p_slc = jnp.einsum('bgrsc,cn->bgsn', p_cmp, overlap)
    blk_id = jnp.arange(n_sel)[None, :]
    sel_valid = sel_start[None, :] <= t_all[:, None]
    forced = (blk_id == 0) | (blk_id == (t_all // SEL_BLOCK)[:, None])
    sel_score = jnp.where(forced, 1e4, jnp.where(sel_valid, p_slc, -1.0))
    k_top = min(SEL_TOPK, n_sel)
    sel_idx = lax.top_k(sel_score, k_top)[1]
    ks_blk = ks_r.reshape(B, n_sel, SEL_BLOCK, G, d).transpose(0, 3, 1, 2, 4)
    vs_blk = vs.reshape(B, n_sel, SEL_BLOCK, G, d).transpose(0, 3, 1, 2, 4)
    b_ix = jnp.arange(B)[:, None, None, None]
    g_ix = jnp.arange(G)[None, :, None, None]
    o_sel, o_win = [], []
    for i in range(S // Q_BLOCK):
        q0, q1 = i * Q_BLOCK, (i + 1) * Q_BLOCK
        t_blk = jnp.arange(q0, q1)
        ib = sel_idx[:, :, q0:q1]
        kg = ks_blk[b_ix, g_ix, ib].reshape(B, G, Q_BLOCK, k_top * SEL_BLOCK, d)
        vg = vs_blk[b_ix, g_ix, ib].reshape(B, G, Q_BLOCK, k_top * SEL_BLOCK, d)
        kpos = (ib[..., None] * SEL_BLOCK + jnp.arange(SEL_BLOCK)).reshape(B, G, Q_BLOCK, k_top * SEL_BLOCK)
        logits = jnp.einsum('bqgrd,bgqnd->bgrqn', qg_rope[:, q0:q1], kg) * scale
        mask = kpos[:, :, None] <= t_blk[None, None, None, :, None]
        p = masked_softmax(logits, mask)
        o_sel.append(jnp.einsum('bgrqn,bgqnd->bqgrd', p.astype(vg.dtype), vg))
        k0 = max(0, q0 - WINDOW + 1)
        logits_w = jnp.einsum('bqgrd,bkgd->bgrqk', qg_rope[:, q0:q1], kw_r[:, k0:q1]) * scale
        s_pos = jnp.arange(k0, q1)[None, :]
        band = (s_pos <= t_blk[:, None]) & (t_blk[:, None] - s_pos < WINDOW)
        p_w = masked_softmax(logits_w, band)
        o_win.append(jnp.einsum('bgrqk,bkgd->bqgrd', p_w.astype(vw.dtype), vw[:, k0:q1]))
    o_sel = jnp.concatenate(o_sel, axis=1)
    o_win = jnp.concatenate(o_win, axis=1)
    g = gates.reshape(B, S, G, R, 3).astype(q.dtype)
    o = g[..., 0:1] * o_cmp + g[..., 1:2] * o_sel + g[..., 2:3] * o_win
    return o.reshape(B, S, H * d)


def hybrid_mixer(h, pos, w_in, b_fox_f, pe_k, w1_k, b1_k, w2_k, pe_v, w1_v, b1_v, w2_v,
                 w_up_sb, w_up_nsa, w_up_fox, w_out):
    B, S, _ = h.shape
    offsets = np.cumsum(IN_SIZES)[:-1].tolist()
    (sb_q, sb_k, sb_v, nsa_q, nsa_kc, nsa_vc, nsa_ks, nsa_vs, nsa_kw, nsa_vw, nsa_g,
     fox_q, fox_k, fox_v, fox_f, merge) = jnp.split(h @ w_in, offsets, axis=-1)

    def heads(t, n):
        return t.reshape(B, S, n, HEAD_DIM)

    o_sb = stick_breaking_attention(heads(sb_q, H_SB), heads(sb_k, H_SB), heads(sb_v, H_SB))
    nsa_gates = jax.nn.sigmoid(nsa_g.astype(F32)).reshape(B, S, H_NSA, 3)
    o_nsa = native_sparse_attention(
        heads(nsa_q, H_NSA), heads(nsa_kc, NSA_KV_HEADS), heads(nsa_vc, NSA_KV_HEADS),
        heads(nsa_ks, NSA_KV_HEADS), heads(nsa_vs, NSA_KV_HEADS),
        heads(nsa_kw, NSA_KV_HEADS), heads(nsa_vw, NSA_KV_HEADS), nsa_gates, pos,
        pe_k, w1_k, b1_k, w2_k, pe_v, w1_v, b1_v, w2_v)
    o_fox = forgetting_attention(heads(fox_q, H_FOX), heads(fox_k, H_FOX), heads(fox_v, H_FOX),
                                 fox_f + b_fox_f)
    gate = jax.nn.sigmoid(merge.astype(F32)).astype(h.dtype).reshape(B, S, N_BRANCH, D_MODEL)
    y = (gate[:, :, 0] * (o_sb.reshape(B, S, W_SB) @ w_up_sb)
         + gate[:, :, 1] * (o_nsa @ w_up_nsa)
         + gate[:, :, 2] * (o_fox.reshape(B, S, W_FOX) @ w_up_fox))
    return y @ w_out


def memory_attention(h, mem_n, wq, wk, wv, wo):
    B, S, _ = h.shape
    M = mem_n.shape[1]
    q = (h @ wq).reshape(B, S, H_MEM, HEAD_DIM)
    k = (mem_n @ wk).reshape(B, M, H_MEM, HEAD_DIM)
    v = (mem_n @ wv).reshape(B, M, H_MEM, HEAD_DIM)
    logits = jnp.einsum('bshd,bmhd->bhsm', q, k).astype(F32) * HEAD_DIM ** -0.5
    p = jax.nn.softmax(logits, axis=-1).astype(v.dtype)
    o = jnp.einsum('bhsm,bmhd->bshd', p, v).reshape(B, S, W_MEM)
    return o @ wo


def swiglu(h, w_gate, w_up, w_down):
    return (jax.nn.silu(h @ w_gate) * (h @ w_up)) @ w_down


def setup_inputs(seed: int = 0) -> dict:
    key = jax.random.key(seed)
    ks = jax.random.split(key, 40)
    nrm = jax.random.normal

    def w(k, shape, fan_in):
        return nrm(k, shape, F32) * fan_in ** -0.5

    def gain(k):
        return 1.0 + 0.05 * nrm(k, (DEPTH, D_MODEL), F32)

    start = jax.random.randint(ks[2], (BATCH, 1), 0, 4096, dtype=jnp.int32)
    positions = (start + jnp.arange(SEQ, dtype=jnp.int32)[None, :]).astype(jnp.int32)
    return {
        "x": nrm(ks[0], (BATCH, SEQ, D_MODEL), F32),
        "mem": nrm(ks[1], (BATCH, N_MEM, D_MODEL), F32),
        "positions": positions,
        "g_pre_mix": gain(ks[3]),
        "g_post_mix": gain(ks[4]),
        "g_pre_mem": gain(ks[5]),
        "g_mem": gain(ks[6]),
        "g_post_mem": gain(ks[7]),
        "g_pre_ffn": gain(ks[8]),
        "g_post_ffn": gain(ks[9]),
        "w_in": w(ks[10], (DEPTH, D_MODEL, D_IN), D_MODEL),
        "b_fox_f": 3.0 + 0.5 * nrm(ks[11], (DEPTH, H_FOX), F32),
        "cmp_pe_k": 0.02 * nrm(ks[12], (DEPTH, CMP_LEN, HEAD_DIM), F32),
        "cmp_w1_k": w(ks[13], (DEPTH, CMP_LEN * HEAD_DIM, CMP_HIDDEN), CMP_LEN * HEAD_DIM),
        "cmp_b1_k": 0.02 * nrm(ks[14], (DEPTH, CMP_HIDDEN), F32),
        "cmp_w2_k": w(ks[15], (DEPTH, CMP_HIDDEN, HEAD_DIM), CMP_HIDDEN),
        "cmp_pe_v": 0.02 * nrm(ks[16], (DEPTH, CMP_LEN, HEAD_DIM), F32),
        "cmp_w1_v": w(ks[17], (DEPTH, CMP_LEN * HEAD_DIM, CMP_HIDDEN), CMP_LEN * HEAD_DIM),
        "cmp_b1_v": 0.02 * nrm(ks[18], (DEPTH, CMP_HIDDEN), F32),
        "cmp_w2_v": w(ks[19], (DEPTH, CMP_HIDDEN, HEAD_DIM), CMP_HIDDEN),
        "w_up_sb": w(ks[20], (DEPTH, W_SB, D_MODEL), W_SB),
        "w_up_nsa": w(ks[21], (DEPTH, W_NSA, D_MODEL), W_NSA),
        "w_up_fox": w(ks[22], (DEPTH, W_FOX, D_MODEL), W_FOX),
        "w_out": w(ks[23], (DEPTH, D_MODEL, D_MODEL), D_MODEL),
        "w_mem_q": w(ks[24], (DEPTH, D_MODEL, W_MEM), D_MODEL),
        "w_mem_k": w(ks[25], (DEPTH, D_MODEL, W_MEM), D_MODEL),
        "w_mem_v": w(ks[26], (DEPTH, D_MODEL, W_MEM), D_MODEL),
        "w_mem_o": w(ks[27], (DEPTH, W_MEM, D_MODEL), W_MEM),
        "w_ffn_gate": w(ks[28], (DEPTH, D_MODEL, D_FF), D_MODEL),
        "w_ffn_up": w(ks[29], (DEPTH, D_MODEL, D_FF), D_MODEL),
        "w_ffn_down": w(ks[30], (DEPTH, D_FF, D_MODEL), D_FF),
    }


def reference(x, mem, positions, g_pre_mix, g_post_mix, g_pre_mem, g_mem, g_post_mem,
              g_pre_ffn, g_post_ffn, w_in, b_fox_f, cmp_pe_k, cmp_w1_k, cmp_b1_k, cmp_w2_k,
              cmp_pe_v, cmp_w1_v, cmp_b1_v, cmp_w2_v, w_up_sb, w_up_nsa, w_up_fox, w_out,
              w_mem_q, w_mem_k, w_mem_v, w_mem_o, w_ffn_gate, w_ffn_up, w_ffn_down):
    for l in range(DEPTH):
        h = rmsnorm(x, g_pre_mix[l])
        y = hybrid_mixer(h, positions, w_in[l], b_fox_f[l],
                         cmp_pe_k[l], cmp_w1_k[l], cmp_b1_k[l], cmp_w2_k[l],
                         cmp_pe_v[l], cmp_w1_v[l], cmp_b1_v[l], cmp_w2_v[l],
                         w_up_sb[l], w_up_nsa[l], w_up_fox[l], w_out[l])
        x = x + rmsnorm(y, g_post_mix[l])
        h = rmsnorm(x, g_pre_mem[l])
        y = memory_attention(h, rmsnorm(mem, g_mem[l]), w_mem_q[l], w_mem_k[l], w_mem_v[l], w_mem_o[l])
        x = x + rmsnorm(y, g_post_mem[l])
        h = rmsnorm(x, g_pre_ffn[l])
        y = swiglu(h, w_ffn_gate[l], w_ffn_up[l], w_ffn_down[l])
        x = x + rmsnorm(y, g_post_ffn[l])
    return x
```

```python
import contextlib
import math
import numpy as np
import ml_dtypes
import concourse.bass as bass
import concourse.mybir as mybir
from concourse.bass_utils import run_bass_kernel_spmd

F32 = mybir.dt.float32
BF16 = mybir.dt.bfloat16
I32 = mybir.dt.int32
AF = mybir.ActivationFunctionType
ALU = mybir.AluOpType
AX = mybir.AxisListType

ENGS = ("pe", "act", "dve", "pool", "sp")
DMA_RING = 6
T = 2048
D = 1024
NT = 16
DEPTH = 2
D_IN = 7456
D_FF = 2816
BIG = 30000.0
WMAX = 2048
DEBUG_LAYERS = None


class Buf:
    __slots__ = ("name", "w", "r")

    def __init__(self, name=""):
        self.name = name
        self.w = None
        self.r = []


class Sched:
    def __init__(self, nc, stack):
        self.nc = nc
        self.sems = {e: stack.enter_context(nc.semaphore("s_" + e)) for e in ENGS}
        self.ring = {q: [stack.enter_context(nc.semaphore("r_%s%d" % (q, i))) for i in range(DMA_RING)]
                     for q in ("sp",)}
        self.cnt = {e: 0 for e in ENGS}
        self.ring_n = {q: 0 for q in self.ring}
        self.ring_val = {q: [0] * DMA_RING for q in self.ring}
        self.reset()

    def reset(self):
        self.ops = []
        self.touched = {}

    def add(self, eng, fn, reads=(), writes=(), dma=False):
        import os
        if len(self.ops) >= int(os.environ.get("NOPS", "100000000")):
            return
        deps = set()
        for b in reads:
            if b.w is not None:
                deps.add(b.w)
        for b in writes:
            if b.w is not None:
                deps.add(b.w)
            deps.update(b.r)
        i = len(self.ops)
        self.ops.append(dict(eng=eng, fn=fn, deps=deps, dma=dma, sig=False))
        for b in reads:
            b.r.append(i)
            self.touched[id(b)] = b
        for b in writes:
            b.w = i
            b.r = []
            self.touched[id(b)] = b
        return i

    def emit(self):
        nc = self.nc
        ops = self.ops
        for op in ops:
            for d in op["deps"]:
                od = ops[d]
                if od["dma"] or od["eng"] != op["eng"] or op["eng"] != "pe":
                    od["sig"] = True
        for op in ops:
            e = op["eng"]
            if op["dma"]:
                n = self.ring_n[e]
                self.ring_n[e] += 1
                slot = n % DMA_RING
                prev = self.ring_val[e][slot]
                self.ring_val[e][slot] = prev + 16
                op["sem"] = self.ring[e][slot]
                op["val"] = prev + 16
                op["prev"] = prev
            elif op["sig"]:
                self.cnt[e] += 1
                op["sem"] = self.sems[e]
                op["val"] = self.cnt[e]
        per = {e: [] for e in ENGS}
        for i, op in enumerate(ops):
            per[op["eng"]].append(i)

        def run(e, engobj):
            seen = {}
            for i in per[e]:
                op = ops[i]
                waits = {}
                for d in sorted(op["deps"]):
                    od = ops[d]
                    if not od["dma"] and od["eng"] == e and e == "pe":
                        continue
                    s = od["sem"]
                    k = id(s)
                    if seen.get(k, 0) >= od["val"]:
                        continue
                    if k not in waits or waits[k][1] < od["val"]:
                        waits[k] = (s, od["val"])
                if op["dma"] and op["prev"] > 0:
                    s = op["sem"]
                    k = id(s)
                    if seen.get(k, 0) < op["prev"]:
                        if k not in waits or waits[k][1] < op["prev"]:
                            waits[k] = (s, op["prev"])
                for k, (s, v) in waits.items():
                    engobj.wait_ge(s, v)
                    seen[k] = v
                ins = op["fn"](engobj)
                if op["dma"]:
                    ins.then_inc(op["sem"], 16)
                elif op["sig"]:
                    ins.then_inc(op["sem"], 1)
            if e in self.ring:
                for slot in range(DMA_RING):
                    v = self.ring_val[e][slot]
                    if v > 0 and seen.get(id(self.ring[e][slot]), 0) < v:
                        engobj.wait_ge(self.ring[e][slot], v)

        with nc.Block() as block:
            @block.tensor
            def _(eng):
                run("pe", eng)

            @block.scalar
            def _(eng):
                run("act", eng)

            @block.vector
            def _(eng):
                run("dve", eng)

            @block.gpsimd
            def _(eng):
                run("pool", eng)

            @block.sync
            def _(eng):
                run("sp", eng)
        for b in self.touched.values():
            b.w = None
            b.r = []
        self.reset()


def _consts():
    bf = ml_dtypes.bfloat16
    j = np.arange(128)[:, None]
    t = np.arange(128)[None, :]
    c = {}
    c["identb"] = np.eye(128, dtype=np.float32).astype(bf)
    c["identf"] = np.eye(128, dtype=np.float32)
    c["tri_sb"] = np.where(j >= t, -BIG, 0.0).astype(bf)
    c["tri_c"] = np.where(j > t, -BIG, 0.0).astype(bf)
    c["tri_band"] = np.where(j <= t, -BIG, 0.0).astype(bf)
    c["negut8"] = np.where(j >= t, -8.0, 0.0).astype(bf)
    c["neg8ones"] = np.full((128, 128), -8.0, np.float32).astype(bf)
    rot = np.zeros((128, 128), np.float32)
    for blk in (0, 64):
        for m in range(8):
            rot[blk + m + 8, blk + m] = -1.0
            rot[blk + m, blk + m + 8] = 1.0
    c["rotT"] = rot.astype(bf)
    half = 8
    inv = (500000.0 ** (-np.arange(half, dtype=np.float32) / half)).astype(np.float32)
    invf = np.zeros((128, 1), np.float32)
    for p in range(128):
        if p % 64 < 16:
            invf[p, 0] = inv[(p % 64) % 8]
    c["invf"] = invf
    cidx = np.arange(127)[:, None]
    tt_ = np.arange(T)[None, :]
    c["cmpbias"] = np.where(16 * cidx + 31 <= tt_, 0.0, -BIG).astype(bf)
    ex = np.zeros((32, 16, 128), np.float32)
    for kb in range(16):
        for p in range(128):
            ex[2 * kb + (p >= 64), kb, p] = BIG
    c["expand"] = ex.astype(bf)
    vm = np.zeros((128, 16, 32), np.float32)
    addc = np.zeros((128, 16, 32), np.float32)
    for i in range(16):
        for p in range(128):
            tpos = 128 * i + p
            for n in range(32):
                forced = (n == 0) or (n == tpos // 64)
                valid = 64 * n <= tpos
                if forced:
                    addc[p, i, n] = 1e4
                elif valid:
                    vm[p, i, n] = 1.0
                else:
                    addc[p, i, n] = -1.0
    c["vm"] = vm
    c["addc"] = addc
    cs = np.arange(127)[:, None] * 16
    ss = np.arange(32)[None, :] * 64
    c["ov"] = ((cs < ss + 64) & (cs + 32 > ss)).astype(np.float32).astype(bf)
    return c


CONST_DT = dict(identb=BF16, identf=F32, tri_sb=BF16, tri_c=BF16, tri_band=BF16, negut8=BF16, neg8ones=BF16,
                rotT=BF16, invf=F32, cmpbias=BF16, expand=BF16, vm=F32, addc=F32, ov=BF16)

W_SHAPES = dict(
    g_pre_mix=[DEPTH, D], g_post_mix=[DEPTH, D], g_pre_mem=[DEPTH, D], g_mem=[DEPTH, D], g_post_mem=[DEPTH, D],
    g_pre_ffn=[DEPTH, D], g_post_ffn=[DEPTH, D], w_in=[DEPTH, D, D_IN], b_fox_f=[DEPTH, 8],
    cmp_pe_k=[DEPTH, 32, 64], cmp_w1_k=[DEPTH, 2048, 256], cmp_b1_k=[DEPTH, 256], cmp_w2_k=[DEPTH, 256, 64],
    cmp_pe_v=[DEPTH, 32, 64], cmp_w1_v=[DEPTH, 2048, 256], cmp_b1_v=[DEPTH, 256], cmp_w2_v=[DEPTH, 256, 64],
    w_up_sb=[DEPTH, 512, D], w_up_nsa=[DEPTH, 512, D], w_up_fox=[DEPTH, 512, D], w_out=[DEPTH, D, D],
    w_mem_q=[DEPTH, D, 256], w_mem_k=[DEPTH, D, 256], w_mem_v=[DEPTH, D, 256], w_mem_o=[DEPTH, 256, D],
    w_ffn_gate=[DEPTH, D, D_FF], w_ffn_up=[DEPTH, D, D_FF], w_ffn_down=[DEPTH, D_FF, D])

C_SBQ, C_SBK, C_SBV = 0, 512, 1024
C_NQ, C_KC, C_VC, C_KS, C_VS, C_KW, C_VW, C_NG = 1536, 2048, 2176, 2304, 2432, 2560, 2688, 2816
C_FQ, C_FK, C_FV, C_FF, C_MG = 2840, 3352, 3864, 4376, 4384


class Prog:
    def __init__(self, depth=DEPTH, dbg=None, stop=None):
        self.depth = depth
        self.dbg = dbg
        self.stop = stop
        nc = self.nc = bass.Bass("TRN2", target_bir_lowering=False)
        self.x_in = nc.dram_tensor("x", [T, D], F32, kind="ExternalInput").ap()
        self.mem_in = nc.dram_tensor("mem", [256, D], F32, kind="ExternalInput").ap()
        self.pos_in = nc.dram_tensor("positions", [1, T], I32, kind="ExternalInput").ap()
        self.w = {k: nc.dram_tensor(k, s, F32, kind="ExternalInput").ap() for k, s in W_SHAPES.items()}
        cs = _consts()
        self.c = {k: nc.dram_tensor("c_" + k, list(v.shape), CONST_DT[k], kind="ExternalInput").ap()
                  for k, v in cs.items()}
        self.out = nc.dram_tensor("out", [T, D], F32, kind="ExternalOutput").ap()
        self.o_scr = nc.dram_tensor("o_scr", [3, T, 512], BF16, kind=("ExternalOutput" if dbg else "Internal")).ap()
        self.row_scr = nc.dram_tensor("row_scr", [8, 4, T], BF16, kind="Internal").ap()
        with contextlib.ExitStack() as st:
            self.S = Sched(nc, st)
            self.ps = [st.enter_context(nc.psum_tensor("ps%d" % i, [128, 512], F32)) for i in range(8)]
            self.pb = [Buf("ps%d" % i) for i in range(8)]
            self.B_x = Buf("x")
            self.B_oscr = Buf("oscr")
            self.st = st
            self.build()

    def mm(self, out, lhsT, rhs, start, stop, R, W):
        self.S.add("pe", lambda e: e.matmul(out, lhsT=lhsT, rhs=rhs, start=start, stop=stop, skip_group_check=True), R, W)

    def tr(self, out, in_, ident, R, W):
        self.S.add("pe", lambda e: e.transpose(out=out, in_=in_, identity=ident), R, W)

    def act(self, out, in_, func, R, W, bias=None, scale=None, accum=None):
        kw = {}
        if bias is not None:
            kw["bias"] = bias
        if scale is not None:
            kw["scale"] = scale
        if accum is not None:
            kw["accum_out"] = accum
        self.S.add("act", lambda e: e.activation(out=out, in_=in_, func=func, **kw), R, W)

    def tt(self, out, in0, in1, op, R, W, eng="dve"):
        self.S.add(eng, lambda e: e.tensor_tensor(out=out, in0=in0, in1=in1, op=op), R, W)

    def ts(self, out, in0, s1, s2, op0, op1, R, W, eng="dve"):
        if op1 is None:
            self.S.add(eng, lambda e: e.tensor_scalar(out=out, in0=in0, scalar1=s1, scalar2=None, op0=op0), R, W)
        else:
            self.S.add(eng, lambda e: e.tensor_scalar(out=out, in0=in0, scalar1=s1, scalar2=s2, op0=op0, op1=op1), R, W)

    def stt(self, out, in0, scalar, in1, op0, op1, R, W):
        self.S.add("dve", lambda e: e.scalar_tensor_tensor(out=out, in0=in0, scalar=scalar, in1=in1, op0=op0, op1=op1), R, W)

    def cp(self, out, in_, R, W, eng="dve"):
        self.S.add(eng, lambda e: e.tensor_copy(out=out, in_=in_), R, W)

    def ms(self, ap, val, W, eng="dve"):
        self.S.add(eng, lambda e: e.memset(ap, val), [], W)

    def rcp(self, out, in_, R, W):
        self.S.add("dve", lambda e: e.reciprocal(out=out, in_=in_), R, W)

    def dma(self, out, in_, R, W, nc_ok=False):
        if nc_ok:
            self.S.add("sp", lambda q: q.dma_start(out=out, in_=in_, allow_slow_non_contiguous=True), R, W, dma=True)
        else:
            self.S.add("sp", lambda q: q.dma_start(out=out, in_=in_), R, W, dma=True)

    def sb(self, st, name, shape, dt):
        self._n = getattr(self, "_n", 0) + 1
        return st.enter_context(self.nc.sbuf_tensor("%s_%d" % (name, self._n), shape, dt))

    def winit(self, st):
        self.wst = [self.sb(st, "wst%d" % i, [128, WMAX], F32) for i in range(2)]
        self.wbf = [self.sb(st, "wbf%d" % i, [128, WMAX], BF16) for i in range(3)]
        self.wst_b = [Buf("wst%d" % i) for i in range(2)]
        self.wbf_b = [Buf("wbf%d" % i) for i in range(3)]
        self.wn = 0

    def wload(self, w2d, r0, kc, c0, n, dst=None, dst_b=None, prows=128):
        assert kc * n <= WMAX
        i = self.wn
        self.wn += 1
        stg, stg_b = self.wst[i % 2], self.wst_b[i % 2]
        src = w2d[r0:r0 + kc * prows, c0:c0 + n].rearrange("(c p) n -> p c n", p=prows)
        sview = stg[0:prows, 0:kc * n].rearrange("p (c n) -> p c n", c=kc)
        self.dma(sview, src, [], [stg_b])
        if dst is None:
            j = i % 3
            dst = self.wbf[j][0:prows, 0:kc * n].rearrange("p (c n) -> p c n", c=kc)
            dst_b = self.wbf_b[j]
        self.cp(dst, sview, [stg_b], [dst_b], eng="pool")
        return dst, dst_b

    def lin_fm(self, w2d, c0, ncols, hT, hT_b, ntok, out_fn, out_b, chunk=128, func=AF.Identity, bias_fn=None,
               banks=(6, 7)):
        tbw = min(512, ntok)
        ntb = ntok // tbw
        per = max(chunk, (WMAX // 8) // chunk * chunk)
        cnt = 0
        for g0 in range(0, ncols, per):
            gn = min(per, ncols - g0)
            wb, wb_b = self.wload(w2d, 0, 8, c0 + g0, gn)
            for cc in range(gn // chunk):
                ci = (g0 // chunk) + cc
                for tb in range(ntb):
                    bk = banks[cnt % len(banks)]
                    cnt += 1
                    for kc in range(8):
                        self.mm(self.ps[bk][0:chunk, 0:tbw], wb[:, kc, cc * chunk:(cc + 1) * chunk],
                                hT[:, kc, tb * tbw:(tb + 1) * tbw], kc == 0, kc == 7, [wb_b, hT_b], [self.pb[bk]])
                    outs = out_fn(ci, tb)
                    if not isinstance(outs, list):
                        outs = [(slice(0, chunk), outs)]
                    for (psl, dst) in outs:
                        self.act(dst, self.ps[bk][psl, 0:tbw], func, [self.pb[bk]], [out_b],
                                 bias=(bias_fn(ci) if bias_fn else None))

    def lin_tm(self, w2d, c0, ncols, hT, hT_b, ntiles, out_fn, out_b, func=AF.Identity, banks=(6, 7), blk=256):
        cnt = 0
        for cb in range(0, ncols, blk):
            n = min(blk, ncols - cb)
            wb, wb_b = self.wload(w2d, 0, 8, c0 + cb, n)
            for j in range(ntiles):
                bk = banks[cnt % len(banks)]
                cnt += 1
                for kc in range(8):
                    self.mm(self.ps[bk][:, 0:n], hT[:, kc, j * 128:(j + 1) * 128], wb[:, kc, 0:n],
                            kc == 0, kc == 7, [wb_b, hT_b], [self.pb[bk]])
                self.act(out_fn(j, cb, n), self.ps[bk][:, 0:n], func, [self.pb[bk]], [out_b])

    def rstd_from_ss(self, ss, rstd, b_ss, b_rstd, n=D):
        self.ts(rstd, ss, 1.0 / n, 1e-6, ALU.mult, ALU.add, [b_ss], [b_rstd])
        self.act(rstd, rstd, AF.Sqrt, [b_rstd], [b_rstd])
        self.rcp(rstd, rstd, [b_rstd], [b_rstd])

    def norm_to_hT(self, src, ntiles, gname, l, hT, hT_b, st):
        gt = self.sb(st, "n_g", [128, D], F32)
        b_g = Buf("g")
        self.dma(gt[:], self.w[gname][l:l + 1, :].partition_broadcast(128), [], [b_g])
        xs = [self.sb(st, "n_x%d" % i, [128, D], F32) for i in range(2)]
        xb = [Buf() for _ in range(2)]
        sq = self.sb(st, "n_sq", [128, D], F32)
        ssr = [self.sb(st, "n_ss%d" % i, [128, 2], F32) for i in range(2)]
        sb_ = [Buf() for _ in range(2)]
        hb = [self.sb(st, "n_h%d" % i, [128, D], BF16) for i in range(2)]
        hbb = [Buf() for _ in range(2)]
        b_sq = Buf()
        for j in range(ntiles):
            k = j % 2
            self.dma(xs[k][:], src[j * 128:(j + 1) * 128, :], [self.B_x], [xb[k]])
            self.act(sq[:], xs[k][:], AF.Square, [xb[k]], [b_sq, sb_[k]], accum=ssr[k][:, 0:1])
            self.rstd_from_ss(ssr[k][:, 0:1], ssr[k][:, 1:2], sb_[k], sb_[k])
            self.stt(hb[k][:], xs[k][:], ssr[k][:, 1:2], gt[:], ALU.mult, ALU.mult, [xb[k], sb_[k], b_g], [hbb[k]])
            bk = 4 + k
            pT = self.ps[bk][:].bitcast(BF16)
            for c in range(8):
                self.tr(pT[:, c * 128:(c + 1) * 128], hb[k][:, c * 128:(c + 1) * 128], self.identb[:],
                        [hbb[k], self.b_const], [self.pb[bk]])
            self.cp(hT[:, :, j * 128:(j + 1) * 128], pT[:, 0:1024].rearrange("p (c n) -> p c n", c=8),
                    [self.pb[bk]], [hT_b])

    def post_norm_add(self, j, banks, gt, b_g, st_tiles):
        sq, ss, xt, yt, bufs = st_tiles
        k = j % 2
        b_ss, b_x, b_y, b_sq = bufs[k]
        for h in range(2):
            self.act(sq[:, 0:512], self.ps[banks[h]][:, :], AF.Square, [self.pb[banks[h]]], [b_sq, b_ss],
                     accum=ss[k][:, h:h + 1])
        self.tt(ss[k][:, 2:3], ss[k][:, 0:1], ss[k][:, 1:2], ALU.add, [b_ss], [b_ss])
        self.rstd_from_ss(ss[k][:, 2:3], ss[k][:, 3:4], b_ss, b_ss)
        self.dma(xt[k][:], self.out[j * 128:(j + 1) * 128, :], [self.B_x], [b_x])
        for h in range(2):
            self.stt(yt[k][:, h * 512:(h + 1) * 512], self.ps[banks[h]][:, :], ss[k][:, 3:4],
                     gt[:, h * 512:(h + 1) * 512], ALU.mult, ALU.mult, [self.pb[banks[h]], b_ss, b_g], [b_y])
        self.tt(yt[k][:], yt[k][:], xt[k][:], ALU.add, [b_y, b_x], [b_y], eng="pool")
        self.dma(self.out[j * 128:(j + 1) * 128, :], yt[k][:], [b_y], [self.B_x])

    def post_tiles(self, st):
        sq = self.sb(st, "p_sq", [128, 512], F32)
        ss = [self.sb(st, "p_ss%d" % i, [128, 4], F32) for i in range(2)]
        xt = [self.sb(st, "p_x%d" % i, [128, D], F32) for i in range(2)]
        yt = [self.sb(st, "p_y%d" % i, [128, D], F32) for i in range(2)]
        bufs = [(Buf(), Buf(), Buf(), Buf()) for _ in range(2)]
        return (sq, ss, xt, yt, bufs)

    def attn_step(self, lbank, regions, nk, P, P_b, pvbank, ncol, v_list, acc, acc_b, scale=0.125):
        S = self
        pb = self.pb[lbank]
        for r, mms in enumerate(regions):
            cols = slice(r * 128, (r + 1) * 128)
            for idx, (lhsT, rhs, R) in enumerate(mms):
                S.mm(self.ps[lbank][0:nk, cols], lhsT, rhs, idx == 0, idx == len(mms) - 1, R, [pb])
        S.act(P[0:nk, :], self.ps[lbank][0:nk, :], AF.Exp, [pb], [P_b], scale=scale)
        for r, (rhs, R) in enumerate(v_list):
            S.mm(self.ps[pvbank][:, r * ncol:(r + 1) * ncol], P[0:nk, r * 128:(r + 1) * 128], rhs, True, True,
                 [P_b] + R, [self.pb[pvbank]])
        if acc is not None:
            S.tt(acc[:, 0:4 * ncol], acc[:, 0:4 * ncol], self.ps[pvbank][:, 0:4 * ncol], ALU.add,
                 [acc_b, self.pb[pvbank]], [acc_b])

    def build(self):
        nc = self.nc
        st = self.st
        S = self.S
        self.b_const = Buf("const")
        cst = {}
        for k in ("identb", "tri_sb", "tri_c", "tri_band", "negut8", "neg8ones", "rotT"):
            cst[k] = self.sb(st, "k_" + k, [128, 128], BF16)
            self.dma(cst[k][:], self.c[k], [], [self.b_const])
        self.identb = cst["identb"]
        self.cst = cst
        self.onecol = self.sb(st, "k_one", [128, 1], F32)
        self.ms(self.onecol[:], 1.0, [self.b_const])
        self.negpi = self.sb(st, "k_negpi", [128, 1], F32)
        self.ms(self.negpi[:], -math.pi, [self.b_const])
        with contextlib.ExitStack() as s0:
            xt = [self.sb(s0, "c_x%d" % i, [128, 4, D], F32) for i in range(2)]
            xb = [Buf() for _ in range(2)]
            for j in range(4):
                k = j % 2
                self.dma(xt[k][:], self.x_in[j * 512:(j + 1) * 512, :].rearrange("(c p) n -> p c n", p=128), [], [xb[k]])
                self.dma(self.out[j * 512:(j + 1) * 512, :].rearrange("(c p) n -> p c n", p=128), xt[k][:], [xb[k]], [self.B_x])
            S.emit()
        for l in range(self.depth):
            self.layer(l)

    def layer(self, l):
        S = self.S
        with contextlib.ExitStack() as sl:
            hT = self.sb(sl, "hT", [128, 8, T], BF16)
            hT_b = Buf("hT")
            self.winit(sl)
            with contextlib.ExitStack() as s1:
                self.norm_to_hT(self.out, NT, "g_pre_mix", l, hT, hT_b, s1)
                S.emit()
            for nm, fn in (("sb", self.sb_branch), ("fox", self.fox_branch), ("nsa", self.nsa_branch),
                           ("merge", self.merge), ("mem", self.mem_attn), ("ffn", self.ffn)):
                if self.stop is not None and nm not in self.stop:
                    continue
                fn(l, hT, hT_b)

    def sb_branch(self, l, hT, hT_b):
        S = self.S
        w_in = self.w["w_in"][l]
        with contextlib.ExitStack() as st:
            qT = self.sb(st, "sb_qT", [128, 4, T], BF16)
            kT = self.sb(st, "sb_kT", [128, 8, T], BF16)
            v = self.sb(st, "sb_v", [128, NT, 512], BF16)
            b_q, b_k, b_v = Buf(), Buf(), Buf()
            self.lin_fm(w_in, C_SBQ, 512, hT, hT_b, T, lambda ci, tb: qT[:, ci, tb * 512:(tb + 1) * 512], b_q)
            self.ms(kT[:], 0.0, [b_k])
            self.lin_fm(w_in, C_SBK, 512, hT, hT_b, T,
                        lambda ci, tb: [(slice(0, 64), kT[0:64, 2 * ci, tb * 512:(tb + 1) * 512]),
                                        (slice(64, 128), kT[64:128, 2 * ci + 1, tb * 512:(tb + 1) * 512])], b_k)
            self.lin_tm(w_in, C_SBV, 512, hT, hT_b, NT, lambda j, cb, n: v[:, j, cb:cb + n], b_v)
            print("MARK sb_proj", len(S.ops))
            e_t = [self.sb(st, "sb_e%d" % i, [128, 512], F32) for i in range(2)]
            L_t = [self.sb(st, "sb_L%d" % i, [128, 512], BF16) for i in range(2)]
            tmp = [self.sb(st, "sb_t%d" % i, [128, 512], F32) for i in range(2)]
            P_t = [self.sb(st, "sb_P%d" % i, [128, 512], BF16) for i in range(2)]
            carry = [self.sb(st, "sb_c%d" % i, [128, 512], F32) for i in range(2)]
            o_t = [self.sb(st, "sb_o%d" % i, [128, 256], BF16) for i in range(2)]
            be, bL, bt, bP, bc, bo = ([Buf() for _ in range(2)] for _ in range(6))
            tri = self.cst["tri_sb"]
            accs = [self.sb(st, "sb_acc%d" % i, [128, 256], F32) for i in range(2)]
            bacc = [Buf() for _ in range(2)]
            step = 0
            it = 0
            for hg in range(2):
                for i in range(NT):
                    ci = it % 2
                    it += 1
                    self.ms(carry[ci][:], 0.0, [bc[ci]])
                    self.ms(accs[ci][:], 0.0, [bacc[ci]])
                    for kb in range(i, -1, -1):
                        s2 = step % 2
                        step += 1
                        zb, wbk, cbk, pvb = s2, 2 + s2, 6, 4 + s2
                        ksl = slice(kb * 128, (kb + 1) * 128)
                        qsl = slice(i * 128, (i + 1) * 128)
                        for hh in range(4):
                            h = 4 * hg + hh
                            cols = slice(hh * 128, (hh + 1) * 128)
                            self.mm(self.ps[zb][:, cols], kT[:, h, ksl], qT[:, h // 2, qsl], True, kb != i,
                                    [b_q, b_k], [self.pb[zb]])
                            if kb == i:
                                self.mm(self.ps[zb][:, cols], self.identb[:], tri[:], False, True,
                                        [self.b_const], [self.pb[zb]])
                        self.act(e_t[s2][:], self.ps[zb][:, :], AF.Exp, [self.pb[zb]], [be[s2]], scale=0.125)
                        self.act(L_t[s2][:], e_t[s2][:], AF.Ln, [be[s2], self.b_const], [bL[s2]], bias=self.onecol[:, 0:1])
                        for hh in range(4):
                            h = 4 * hg + hh
                            cols = slice(hh * 128, (hh + 1) * 128)
                            self.mm(self.ps[wbk][:, cols], kT[:, h, ksl], qT[:, h // 2, qsl], True, False,
                                    [b_q, b_k], [self.pb[wbk]])
                            if kb == i:
                                self.mm(self.ps[wbk][:, cols], self.identb[:], tri[:], False, False,
                                        [self.b_const], [self.pb[wbk]])
                            self.mm(self.ps[wbk][:, cols], self.cst["negut8"][:], L_t[s2][:, cols], False, True,
                                    [bL[s2], self.b_const], [self.pb[wbk]])
                        if kb > 0:
                            self.mm(self.ps[cbk][:, :], self.cst["neg8ones"][:], L_t[s2][:], True, True,
                                    [bL[s2], self.b_const], [self.pb[cbk]])
                        self.tt(tmp[s2][:], self.ps[wbk][:, :], carry[ci][:], ALU.add, [self.pb[wbk], bc[ci]], [bt[s2]])
                        self.act(P_t[s2][:], tmp[s2][:], AF.Exp, [bt[s2]], [bP[s2]], scale=0.125)
                        if kb > 0:
                            self.tt(carry[ci][:], self.ps[cbk][:, :], carry[ci][:], ALU.add, [self.pb[cbk], bc[ci]], [bc[ci]])
                        for hh in range(4):
                            h = 4 * hg + hh
                            self.mm(self.ps[pvb][:, hh * 64:(hh + 1) * 64], P_t[s2][:, hh * 128:(hh + 1) * 128],
                                    v[:, kb, h * 64:(h + 1) * 64], True, True, [bP[s2], b_v], [self.pb[pvb]])
                        self.tt(accs[ci][:], accs[ci][:], self.ps[pvb][:, 0:256], ALU.add, [bacc[ci], self.pb[pvb]], [bacc[ci]])
                    self.cp(o_t[ci][:], accs[ci][:], [bacc[ci]], [bo[ci]], eng="pool")
                    self.dma(self.o_scr[0, i * 128:(i + 1) * 128, hg * 256:(hg + 1) * 256], o_t[ci][:], [bo[ci]], [self.B_oscr])
            S.emit()

    def fox_branch(self, l, hT, hT_b):
        S = self.S
        w_in = self.w["w_in"][l]
        with contextlib.ExitStack() as st:
            v = self.sb(st, "fx_v", [128, NT, 8, 65], BF16)
            b_v = Buf()
            self.ms(v[:, :, :, 64:65], 1.0, [b_v])
            self.lin_tm(w_in, C_FV, 512, hT, hT_b, NT,
                        lambda j, cb, n: v[:, j, cb // 64:(cb + n) // 64, 0:64], b_v)
            rows = self.sb(st, "fx_rows", [8, 4, T], BF16)
            b_rows = Buf()
            with contextlib.ExitStack() as s2:
                fT = self.sb(s2, "fx_f", [8, T], F32)
                ones = self.sb(s2, "fx_ones", [8, T], F32)
                cT = self.sb(s2, "fx_c", [8, T], F32)
                hif = self.sb(s2, "fx_hif", [8, T], F32)
                bcol = self.sb(s2, "fx_b", [8, 1], F32)
                b_f, b_o, b_c, b_h, b_b = Buf(), Buf(), Buf(), Buf(), Buf()
                self.dma(bcol[:], self.w["b_fox_f"][l:l + 1, :].rearrange("o h -> h o"), [], [b_b], nc_ok=True)
                self.ms(ones[:], 1.0, [b_o])
                self.lin_fm(w_in, C_FF, 8, hT, hT_b, T, lambda ci, tb: fT[:, tb * 512:(tb + 1) * 512], b_f, chunk=8,
                            bias_fn=lambda ci: bcol[:, 0:1])
                self.act(fT[:], fT[:], AF.Exp, [b_f, b_b], [b_f], scale=-1.0)
                self.act(fT[:], fT[:], AF.Ln, [b_f, self.b_const], [b_f], bias=self.onecol[0:8, 0:1])
                self.ts(fT[:], fT[:], -1.0, None, ALU.mult, None, [b_f], [b_f])
                S.add("dve", lambda e: e.tensor_tensor_scan(out=cT[:], data0=fT[:], data1=ones[:], initial=0.0,
                                                            op0=ALU.add, op1=ALU.mult), [b_f, b_o], [b_c])
                self.ts(cT[:], cT[:], -8.0, None, ALU.mult, None, [b_c], [b_c])
                self.cp(rows[:, 0, :], cT[:], [b_c], [b_rows])
                self.cp(hif[:], rows[:, 0, :], [b_rows], [b_h])
                self.tt(rows[:, 1, :], cT[:], hif[:], ALU.subtract, [b_c, b_h], [b_rows])
                self.ts(rows[:, 2, :], hif[:], -1.0, None, ALU.mult, None, [b_h], [b_rows])
                b_rs = Buf()
                self.cp(rows[:, 3, :], ones[:], [b_o], [b_rows])
                self.dma(self.row_scr[:, :, :], rows[:], [b_rows], [b_rs])
                S.emit()
            qa = self.sb(st, "fx_qa", [96, 4, T], BF16)
            ka = self.sb(st, "fx_ka", [96, 4, T], BF16)
            P_t = [self.sb(st, "fx_P%d" % i, [128, 512], BF16) for i in range(2)]
            bP = [Buf() for _ in range(2)]
            rden = [self.sb(st, "fx_rd%d" % i, [128, 4], F32) for i in range(2)]
            o_t = [self.sb(st, "fx_o%d" % i, [128, 4, 64], BF16) for i in range(2)]
            bo = [Buf() for _ in range(2)]
            b_q, b_k = Buf(), Buf()
            tri = self.cst["tri_c"]
            facc = [self.sb(st, "fx_acc%d" % i, [128, 260], F32) for i in range(2)]
            bfacc = [Buf() for _ in range(2)]
            for hg in range(2):
                self.ms(qa[64:96, :, :], 0.0, [b_q])
                self.ms(ka[64:96, :, :], 0.0, [b_k])
                for hh in range(4):
                    h = 4 * hg + hh
                    self.dma(ka[64:66, hh, :], self.row_scr[h, 0:2, :], [], [b_k])
                    self.dma(ka[66:67, hh, :], self.row_scr[h, 3:4, :], [], [b_k])
                    self.dma(qa[64:65, hh, :], self.row_scr[h, 3:4, :], [], [b_q])
                    self.dma(qa[65:66, hh, :], self.row_scr[h, 3:4, :], [], [b_q])
                    self.dma(qa[66:67, hh, :], self.row_scr[h, 2:3, :], [], [b_q])
                self.lin_fm(w_in, C_FQ + hg * 256, 256, hT, hT_b, T, lambda ci, tb: qa[0:64, ci, tb * 512:(tb + 1) * 512],
                            b_q, chunk=64)
                self.lin_fm(w_in, C_FK + hg * 256, 256, hT, hT_b, T, lambda ci, tb: ka[0:64, ci, tb * 512:(tb + 1) * 512],
                            b_k, chunk=64)
                step = 0
                for i in range(NT):
                    ci = i % 2
                    qsl = slice(i * 128, (i + 1) * 128)
                    self.ms(facc[ci][:], 0.0, [bfacc[ci]])
                    for kb in range(i, -1, -1):
                        s2 = step % 2
                        step += 1
                        ksl = slice(kb * 128, (kb + 1) * 128)
                        regions = []
                        for hh in range(4):
                            mms = [(ka[0:96, hh, ksl], qa[0:96, hh, qsl], [b_q, b_k])]
                            if kb == i:
                                mms.append((self.identb[:], tri[:], [self.b_const]))
                            regions.append(mms)
                        vl = [(v[:, kb, 4 * hg + hh, :], [b_v]) for hh in range(4)]
                        self.attn_step(s2, regions, 128, P_t[s2], bP[s2], 4 + s2, 65, vl, facc[ci], bfacc[ci])
                    pv3 = facc[ci][:, 0:260].rearrange("p (h c) -> p h c", h=4)
                    self.rcp(rden[ci][:], pv3[:, :, 64], [bfacc[ci]], [bo[ci]])
                    self.tt(o_t[ci][:], pv3[:, :, 0:64], rden[ci][:].unsqueeze(2).to_broadcast([128, 4, 64]), ALU.mult,
                            [bfacc[ci], bo[ci]], [bo[ci]])
                    self.dma(self.o_scr[2, i * 128:(i + 1) * 128, hg * 256:(hg + 1) * 256],
                             o_t[ci][:].rearrange("p h c -> p (h c)"), [bo[ci]], [self.B_oscr])
                S.emit()

    def nsa_branch(self, l, hT, hT_b):
        S = self.S
        w_in = self.w["w_in"][l]
        with contextlib.ExitStack() as st:
            qT = self.sb(st, "ns_qT", [128, 4, T], BF16)
            qrT = self.sb(st, "ns_qrT", [128, 4, T], BF16)
            ksr = self.sb(st, "ns_ksr", [128, 2, T], BF16)
            kwr = self.sb(st, "ns_kwr", [128, 2, T], BF16)
            vs = self.sb(st, "ns_vs", [128, NT, 2, 65], BF16)
            vw = self.sb(st, "ns_vw", [128, NT, 2, 65], BF16)
            gates = self.sb(st, "ns_g", [128, NT, 24], F32)
            kcmpT = self.sb(st, "ns_kcmpT", [128, 2, 127], BF16)
            vcmp = self.sb(st, "ns_vcmp", [127, 2, 97], BF16)
            b_q, b_qr, b_ks, b_kw, b_vs, b_vw, b_g, b_kc, b_vc = (Buf() for _ in range(9))
            with contextlib.ExitStack() as sa:
                kcT = self.sb(sa, "ns_kcT", [128, 2, T], BF16)
                vcT = self.sb(sa, "ns_vcT", [128, 2, T], BF16)
                ksT = self.sb(sa, "ns_ksT", [128, T], BF16)
                kwT = self.sb(sa, "ns_kwT", [128, T], BF16)
                b_kcT, b_vcT, b_ksT, b_kwT = Buf(), Buf(), Buf(), Buf()
                self.lin_fm(w_in, C_NQ, 512, hT, hT_b, T, lambda ci, tb: qT[:, ci, tb * 512:(tb + 1) * 512], b_q)
                for (c0, dst, bb) in ((C_KS, ksT, b_ksT), (C_KW, kwT, b_kwT)):
                    self.lin_fm(w_in, c0, 128, hT, hT_b, T, lambda ci, tb, dst=dst: dst[:, tb * 512:(tb + 1) * 512], bb)
                for (c0, dst, bb) in ((C_KC, kcT, b_kcT), (C_VC, vcT, b_vcT)):
                    self.ms(dst[:], 0.0, [bb])
                    self.lin_fm(w_in, c0, 128, hT, hT_b, T,
                                lambda ci, tb, dst=dst: [(slice(0, 64), dst[0:64, 0, tb * 512:(tb + 1) * 512]),
                                                         (slice(64, 128), dst[64:128, 1, tb * 512:(tb + 1) * 512])], bb)
                self.ms(ksr[:], 0.0, [b_ks])
                self.ms(kwr[:], 0.0, [b_kw])
                self.ms(kcmpT[:], 0.0, [b_kc])
                self.ms(vs[:, :, :, 64:65], 1.0, [b_vs])
                self.ms(vw[:, :, :, 64:65], 1.0, [b_vw])
                self.lin_tm(w_in, C_VS, 128, hT, hT_b, NT, lambda j, cb, n: vs[:, j, :, 0:64], b_vs)
                self.lin_tm(w_in, C_VW, 128, hT, hT_b, NT, lambda j, cb, n: vw[:, j, :, 0:64], b_vw)
                self.lin_tm(w_in, C_NG, 24, hT, hT_b, NT, lambda j, cb, n: gates[:, j, :], b_g, func=AF.Sigmoid)
                posi = self.sb(sa, "ns_posi", [128, 512], I32)
                ang = self.sb(sa, "ns_ang", [128, 512], F32)
                sinT = self.sb(sa, "ns_sin", [128, T], BF16)
                cosT = self.sb(sa, "ns_cos", [128, T], BF16)
                invf = self.sb(sa, "ns_invf", [128, 1], F32)
                b_pos, b_ang, b_sin, b_cos, b_inv = Buf(), Buf(), Buf(), Buf(), Buf()
                self.dma(invf[:], self.c["invf"], [], [b_inv])
                C1 = 6.28125
                C2 = 2 * math.pi - C1
                tr_r = self.sb(sa, "ns_trr", [128, 512], F32)
                tr_k = self.sb(sa, "ns_trk", [128, 512], I32)
                tr_a = self.sb(sa, "ns_tra", [128, 512], F32)
                tr_u = self.sb(sa, "ns_tru", [128, 512], F32)
                tr_m = self.sb(sa, "ns_trm", [128, 512], F32)
                b_tr = Buf()
                for tb in range(4):
                    sl = slice(tb * 512, (tb + 1) * 512)
                    self.dma(posi[:], self.pos_in[:, sl].partition_broadcast(128), [], [b_pos])
                    self.cp(ang[:], posi[:], [b_pos], [b_ang])
                    self.ts(ang[:], ang[:], invf[:, 0:1], None, ALU.mult, None, [b_ang, b_inv], [b_ang])
                    for (dstT, shift, bd) in ((sinT, 0.0, b_sin), (cosT, 0.5 * math.pi, b_cos)):
                        self.ts(tr_a[:], ang[:], shift, None, ALU.add, None, [b_ang], [b_tr])
                        self.ts(tr_r[:], tr_a[:], 1.0 / (2 * math.pi), None, ALU.mult, None, [b_tr], [b_tr])
                        self.cp(tr_k[:], tr_r[:], [b_tr], [b_tr])
                        self.cp(tr_r[:], tr_k[:], [b_tr], [b_tr])
                        self.stt(tr_u[:], tr_r[:], -C1, tr_a[:], ALU.mult, ALU.add, [b_tr], [b_tr])
                        self.stt(tr_u[:], tr_r[:], -C2, tr_u[:], ALU.mult, ALU.add, [b_tr], [b_tr])
                        self.ts(tr_m[:], tr_u[:], math.pi, None, ALU.is_gt, None, [b_tr], [b_tr])
                        self.stt(tr_u[:], tr_m[:], -2 * math.pi, tr_u[:], ALU.mult, ALU.add, [b_tr], [b_tr])
                        self.ts(tr_u[:], tr_u[:], -math.pi, math.pi, ALU.max, ALU.min, [b_tr], [b_tr])
                        self.act(dstT[:, sl], tr_u[:], AF.Sin, [b_tr], [bd])
                t1 = [self.sb(sa, "ns_t1%d" % i, [128, 512], F32) for i in range(2)]
                t2 = [self.sb(sa, "ns_t2%d" % i, [128, 512], F32) for i in range(2)]
                bt1 = [Buf() for _ in range(2)]
                bt2 = [Buf() for _ in range(2)]
                rn = 0
                jobs = [(qT[:, c, :], qrT[:, c, :], b_q, b_qr, False) for c in range(4)] + [(ksT[:], ksr, b_ksT, b_ks, True), (kwT[:], kwr, b_kwT, b_kw, True)]
                for (src, dst, bs, bd, msk) in jobs:
                    for tb in range(4):
                        k = rn % 2
                        rn += 1
                        sl = slice(tb * 512, (tb + 1) * 512)
                        self.mm(self.ps[k][:, :], self.cst["rotT"][:], src[:, sl], True, True, [bs, self.b_const], [self.pb[k]])
                        self.tt(t1[k][:], self.ps[k][:, :], sinT[:, sl], ALU.mult, [self.pb[k], b_sin], [bt1[k]])
                        self.tt(t2[k][:], src[:, sl], cosT[:, sl], ALU.mult, [bs, b_cos], [bt2[k]], eng="pool")
                        if msk:
                            self.tt(dst[0:64, 0, sl], t1[k][0:64, :], t2[k][0:64, :], ALU.add, [bt1[k], bt2[k]], [bd])
                            self.tt(dst[64:128, 1, sl], t1[k][64:128, :], t2[k][64:128, :], ALU.add, [bt1[k], bt2[k]], [bd])
                        else:
                            self.tt(dst[:, sl], t1[k][:], t2[k][:], ALU.add, [bt1[k], bt2[k]], [bd])
                ov_t = self.sb(sa, "ns_ov", [127, 32], BF16)
                b_ov = Buf()
                self.dma(ov_t[:], self.c["ov"], [], [b_ov])
                for g in range(2):
                    self.ms(vcmp[:, g, 64:65], 1.0, [b_vc])
                    self.cp(vcmp[:, g, 65:97], ov_t[:], [b_ov], [b_vc], eng="pool")
                w1 = self.sb(sa, "ns_w1", [128, 32, 256], BF16)
                w2 = self.sb(sa, "ns_w2", [128, 2, 128], BF16)
                pe2 = self.sb(sa, "ns_pe2", [32, 128], F32)
                peT = self.sb(sa, "ns_peT", [128, 32], BF16)
                b1 = self.sb(sa, "ns_b1", [128, 2], F32)
                biasT = self.sb(sa, "ns_biasT", [128, 2], F32)
                hidT = self.sb(sa, "ns_hidT", [128, 2, 127], BF16)
                identf = self.sb(sa, "ns_idf", [128, 128], F32)
                b_w1, b_w2, b_pe, b_peT, b_b1, b_bias, b_hid, b_idf = (Buf() for _ in range(8))
                self.dma(identf[:], self.c["identf"], [], [b_idf])
                for which, srcT, b_src in (("k", kcT, b_kcT), ("v", vcT, b_vcT)):
                    w1d = self.w["cmp_w1_" + which][l]
                    for half in range(2):
                        for l0 in range(0, 32, 8):
                            src = w1d[l0 * 64:(l0 + 8) * 64, :]
                            self.wload(src, 0, 8, 0, 256, dst=w1[half * 64:(half + 1) * 64, l0:l0 + 8, :], dst_b=b_w1, prows=64)
                    w2d = self.w["cmp_w2_" + which][l]
                    for dup in range(2):
                        self.wload(w2d, 0, 2, 0, 64, dst=w2[:, :, dup * 64:(dup + 1) * 64], dst_b=b_w2)
                    for dup in range(2):
                        self.dma(pe2[:, dup * 64:(dup + 1) * 64], self.w["cmp_pe_" + which][l], [], [b_pe])
                    self.tr(self.ps[2][:, 0:32], pe2[:, :], identf[0:32, 0:32], [b_pe, b_idf], [self.pb[2]])
                    self.act(peT[:], self.ps[2][:, 0:32], AF.Identity, [self.pb[2]], [b_peT])
                    self.dma(b1[:], self.w["cmp_b1_" + which][l:l + 1, :].rearrange("o (c p) -> p (o c)", p=128), [], [b_b1], nc_ok=True)
                    for hc in range(2):
                        for ll in range(32):
                            self.mm(self.ps[3][:, hc:hc + 1], w1[0:64, ll, hc * 128:(hc + 1) * 128], peT[0:64, ll:ll + 1],
                                    ll == 0, ll == 31, [b_w1, b_peT], [self.pb[3]])
                    self.tt(biasT[:], self.ps[3][:, 0:2], b1[:], ALU.add, [self.pb[3], b_b1], [b_bias])
                    for g in range(2):
                        base = 64 * g
                        for hc in range(2):
                            bk = hc
                            for ll in range(32):
                                self.mm(self.ps[bk][:, 0:127], w1[:, ll, hc * 128:(hc + 1) * 128],
                                        srcT[:, g, ll:ll + 16 * 126 + 1:16], ll == 0, ll == 31,
                                        [b_w1, b_src], [self.pb[bk]])
                            self.act(hidT[:, hc, :], self.ps[bk][:, 0:127], AF.Silu, [self.pb[bk], b_bias], [b_hid],
                                     bias=biasT[:, hc:hc + 1])
                        if which == "k":
                            for hc in range(2):
                                self.mm(self.ps[2][:, 0:127], w2[:, hc, :], hidT[:, hc, :], hc == 0, hc == 1,
                                        [b_w2, b_hid], [self.pb[2]])
                            self.act(kcmpT[base:base + 64, g, :], self.ps[2][base:base + 64, 0:127], AF.Identity,
                                     [self.pb[2]], [b_kc])
                        else:
                            for hc in range(2):
                                self.mm(self.ps[2][0:127, 0:64], hidT[:, hc, :], w2[:, hc, 0:64], hc == 0, hc == 1,
                                        [b_w2, b_hid], [self.pb[2]])
                            self.act(vcmp[:, g, 0:64], self.ps[2][0:127, 0:64], AF.Identity, [self.pb[2]], [b_vc])
                S.emit()
            cmpb = self.sb(st, "ns_cmpb", [127, T], BF16)
            expand = self.sb(st, "ns_exp", [32, 16, 128], BF16)
            vm = self.sb(st, "ns_vm", [128, 16, 32], F32)
            addc = self.sb(st, "ns_addc", [128, 16, 32], F32)
            b_k2 = Buf()
            self.dma(cmpb[:], self.c["cmpbias"], [], [b_k2])
            self.dma(expand[:], self.c["expand"], [], [b_k2])
            self.dma(vm[:], self.c["vm"], [], [b_k2])
            self.dma(addc[:], self.c["addc"], [], [b_k2])
            P_t = [self.sb(st, "ns_P%d" % i, [128, 512], BF16) for i in range(2)]
            bP = [Buf() for _ in range(2)]
            acc = [self.sb(st, "ns_acc%d" % i, [128, 8, 64], F32) for i in range(2)]
            b_acc = [Buf() for _ in range(2)]
            o_t = [self.sb(st, "ns_o%d" % i, [128, 512], BF16) for i in range(2)]
            b_o = [Buf() for _ in range(2)]
            rd = self.sb(st, "ns_rd", [128, 4], F32)
            sc = self.sb(st, "ns_sc", [128, 4], F32)
            tmp = self.sb(st, "ns_tmp", [128, 4, 64], F32)
            tslc = self.sb(st, "ns_tslc", [128, 4, 32], F32)
            score = self.sb(st, "ns_score", [128, 32], F32)
            m8 = self.sb(st, "ns_m8", [128, 8], F32)
            selb = self.sb(st, "ns_selb", [128, 32], BF16)
            selbT = self.sb(st, "ns_selbT", [32, 128], BF16)
            b_rd, b_sc, b_tmp, b_tslc, b_score, b_m8, b_selb, b_selbT = (Buf() for _ in range(8))
            step = 0

            pacc = self.sb(st, "ns_pacc", [128, 388], F32)
            b_pacc = Buf()

            def finish(src_ap, src_bufs, i, g, gi, first):
                a = acc[i % 2]
                self.ts(rd[:], src_ap[:, :, 64], 1e-30, None, ALU.max, None, src_bufs, [b_rd])
                self.rcp(rd[:], rd[:], [b_rd], [b_rd])
                gv = gates[:, i, :].rearrange("p (h t) -> p h t", t=3)[:, 4 * g:4 * g + 4, gi]
                self.tt(sc[:], rd[:], gv, ALU.mult, [b_rd, b_g], [b_sc])
                if first:
                    self.tt(a[:, 4 * g:4 * g + 4, :], src_ap[:, :, 0:64], sc[:].unsqueeze(2).to_broadcast([128, 4, 64]), ALU.mult,
                            src_bufs + [b_sc], [b_acc[i % 2]])
                else:
                    self.tt(tmp[:], src_ap[:, :, 0:64], sc[:].unsqueeze(2).to_broadcast([128, 4, 64]), ALU.mult,
                            src_bufs + [b_sc], [b_tmp])
                    self.tt(a[:, 4 * g:4 * g + 4, :], a[:, 4 * g:4 * g + 4, :], tmp[:], ALU.add, [b_tmp, b_acc[i % 2]],
                            [b_acc[i % 2]])

            for i in range(NT):
                qsl = slice(i * 128, (i + 1) * 128)
                for g in range(2):
                    s2 = step % 2
                    step += 1
                    regions = [[(kcmpT[:, g, :], qT[:, r, qsl], [b_kc, b_q]),
                                (self.identb[0:127, 0:127], cmpb[:, qsl], [self.b_const, b_k2])] for r in range(4)]
                    vl = [(vcmp[:, g, :], [b_vc]) for r in range(4)]
                    self.attn_step(s2, regions, 127, P_t[s2], bP[s2], 2, 97, vl, None, None)
                    pv3 = self.ps[2][:, 0:388].rearrange("p (h c) -> p h c", h=4)
                    finish(pv3, [self.pb[2]], i, g, 0, True)
                    self.tt(tslc[:], pv3[:, :, 65:97], rd[:].unsqueeze(2).to_broadcast([128, 4, 32]), ALU.mult,
                            [self.pb[2], b_rd], [b_tslc])
                    S.add("dve", lambda e: e.tensor_reduce(out=score[:], in_=tslc[:].rearrange("p r n -> p n r"),
                                                           axis=AX.X, op=ALU.add), [b_tslc], [b_score])
                    self.tt(score[:], score[:], vm[:, i, :], ALU.mult, [b_score, b_k2], [b_score])
                    self.tt(score[:], score[:], addc[:, i, :], ALU.add, [b_score, b_k2], [b_score])
                    S.add("dve", lambda e: e.max(out=m8[:], in_=score[:]), [b_score], [b_m8])
                    self.ts(score[:], score[:], m8[:, 7:8], None, ALU.is_ge, None, [b_score, b_m8], [b_score])
                    self.ts(selb[:], score[:], -1.0, None, ALU.add, None, [b_score], [b_selb])
                    pT = self.ps[3][:].bitcast(BF16)
                    self.tr(pT[0:32, 0:128], selb[:, :], self.identb[:], [b_selb, self.b_const], [self.pb[3]])
                    self.act(selbT[:], pT[0:32, 0:128], AF.Identity, [self.pb[3]], [b_selbT])
                    for (kind, kbs, kT_, v_, gi, bk_, bv_) in (
                            ("sel", list(range(i, -1, -1)), ksr, vs, 1, b_ks, b_vs),
                            ("win", list(range(i, max(0, i - 4) - 1, -1)), kwr, vw, 2, b_kw, b_vw)):
                        self.ms(pacc[:, 0:260], 0.0, [b_pacc])
                        for kb in kbs:
                            s2 = step % 2
                            step += 1
                            ksl = slice(kb * 128, (kb + 1) * 128)
                            regions = []
                            for r in range(4):
                                mms = [(kT_[:, g, ksl], qrT[:, r, qsl], [bk_, b_qr])]
                                if kind == "sel":
                                    mms.append((expand[:, kb, :], selbT[:], [b_k2, b_selbT]))
                                if kb == i:
                                    mms.append((self.identb[:], self.cst["tri_c"][:], [self.b_const]))
                                if kind == "win" and kb == i - 4:
                                    mms.append((self.identb[:], self.cst["tri_band"][:], [self.b_const]))
                                regions.append(mms)
                            vl = [(v_[:, kb, g, :], [bv_]) for r in range(4)]
                            self.attn_step(s2, regions, 128, P_t[s2], bP[s2], 4 + s2, 65, vl, pacc, b_pacc)
                        finish(pacc[:, 0:260].rearrange("p (h c) -> p h c", h=4), [b_pacc], i, g, gi, False)
                k = i % 2
                self.cp(o_t[k][:], acc[k][:].rearrange("p h c -> p (h c)"), [b_acc[k]], [b_o[k]], eng="pool")
                self.dma(self.o_scr[1, i * 128:(i + 1) * 128, :], o_t[k][:], [b_o[k]], [self.B_oscr])
            S.emit()

    def merge(self, l, hT, hT_b):
        S = self.S
        with contextlib.ExitStack() as st:
            wm = self.sb(st, "mg_wm", [128, 8, 3072], BF16)
            wup = self.sb(st, "mg_wup", [128, 3, 4, D], BF16)
            wout = self.sb(st, "mg_wout", [128, 8, D], BF16)
            b_wm, b_wup, b_wout = Buf(), Buf(), Buf()
            for cb in range(0, 3072, 256):
                self.wload(self.w["w_in"][l], 0, 8, C_MG + cb, 256, dst=wm[:, :, cb:cb + 256], dst_b=b_wm)
            for b, nm in enumerate(("w_up_sb", "w_up_nsa", "w_up_fox")):
                for cb in range(0, D, 512):
                    self.wload(self.w[nm][l], 0, 4, cb, 512, dst=wup[:, b, :, cb:cb + 512], dst_b=b_wup)
            for cb in range(0, D, 256):
                self.wload(self.w["w_out"][l], 0, 8, cb, 256, dst=wout[:, :, cb:cb + 256], dst_b=b_wout)
            gt = self.sb(st, "mg_g", [128, D], F32)
            b_gt = Buf()
            self.dma(gt[:], self.w["g_post_mix"][l:l + 1, :].partition_broadcast(128), [], [b_gt])
            post = self.post_tiles(st)
            o_in = [self.sb(st, "mg_o%d" % i, [128, 3, 512], BF16) for i in range(2)]
            oT = [self.sb(st, "mg_oT%d" % i, [128, 12, 128], BF16) for i in range(2)]
            sg = self.sb(st, "mg_sg", [128, 512], F32)
            y = self.sb(st, "mg_y", [128, D], F32)
            ytmp = self.sb(st, "mg_yt", [128, 512], F32)
            yb = self.sb(st, "mg_yb", [128, D], BF16)
            yT = self.sb(st, "mg_yT", [128, 8, 128], BF16)
            b_oin = [Buf() for _ in range(2)]
            b_oT = [Buf() for _ in range(2)]
            b_sg, b_y, b_ytmp, b_yb, b_yT = (Buf() for _ in range(5))
            for j in range(NT):
                k = j % 2
                self.dma(o_in[k][:], self.o_scr[:, j * 128:(j + 1) * 128, :].rearrange("b p n -> p b n"), [self.B_oscr], [b_oin[k]])
                for half in range(2):
                    bk = 4 + half
                    pT = self.ps[bk][:].bitcast(BF16)
                    for c in range(6):
                        cc = half * 6 + c
                        self.tr(pT[:, c * 128:(c + 1) * 128], o_in[k][:, cc // 4, (cc % 4) * 128:(cc % 4 + 1) * 128],
                                self.identb[:], [b_oin[k], self.b_const], [self.pb[bk]])
                    self.cp(oT[k][:, half * 6:(half + 1) * 6, :], pT[:, 0:768].rearrange("p (c n) -> p c n", c=6),
                            [self.pb[bk]], [b_oT[k]])
                for cb in range(2):
                    csl = slice(cb * 512, (cb + 1) * 512)
                    for b in range(3):
                        ub, gb = 0 + (b % 2), 2 + (b % 2)
                        for kc in range(4):
                            self.mm(self.ps[ub][:, :], oT[k][:, 4 * b + kc, :], wup[:, b, kc, csl], kc == 0, kc == 3,
                                    [b_oT[k], b_wup], [self.pb[ub]])
                        for kc in range(8):
                            self.mm(self.ps[gb][:, :], hT[:, kc, j * 128:(j + 1) * 128],
                                    wm[:, kc, b * 1024 + cb * 512:b * 1024 + (cb + 1) * 512], kc == 0, kc == 7,
                                    [hT_b, b_wm], [self.pb[gb]])
                        self.act(sg[:], self.ps[gb][:, :], AF.Sigmoid, [self.pb[gb]], [b_sg])
                        if b == 0:
                            self.tt(y[:, csl], self.ps[ub][:, :], sg[:], ALU.mult, [self.pb[ub], b_sg], [b_y])
                        else:
                            self.tt(ytmp[:], self.ps[ub][:, :], sg[:], ALU.mult, [self.pb[ub], b_sg], [b_ytmp])
                            self.tt(y[:, csl], y[:, csl], ytmp[:], ALU.add, [b_y, b_ytmp], [b_y])
                self.cp(yb[:], y[:], [b_y], [b_yb], eng="pool")
                pT = self.ps[6][:].bitcast(BF16)
                for c in range(8):
                    self.tr(pT[:, c * 128:(c + 1) * 128], yb[:, c * 128:(c + 1) * 128], self.identb[:],
                            [b_yb, self.b_const], [self.pb[6]])
                self.cp(yT[:], pT[:, 0:1024].rearrange("p (c n) -> p c n", c=8), [self.pb[6]], [b_yT])
                ob = (7, 4 + (j % 2)) if False else (7, 6)
                for h2 in range(2):
                    bk = ob[h2]
                    for kc in range(8):
                        self.mm(self.ps[bk][:, :], yT[:, kc, :], wout[:, kc, h2 * 512:(h2 + 1) * 512], kc == 0, kc == 7,
                                [b_yT, b_wout], [self.pb[bk]])
                self.post_norm_add(j, ob, gt, b_gt, post)
            S.emit()

    def mem_attn(self, l, hT, hT_b):
        S = self.S
        with contextlib.ExitStack() as st:
            with contextlib.ExitStack() as s1:
                self.norm_to_hT(self.out, NT, "g_pre_mem", l, hT, hT_b, s1)
                S.emit()
            memT = self.sb(st, "mm_memT", [128, 8, 256], BF16)
            b_memT = Buf()
            with contextlib.ExitStack() as s1:
                self.norm_to_hT(self.mem_in, 2, "g_mem", l, memT, b_memT, s1)
                S.emit()
            qT = self.sb(st, "mm_qT", [128, 2, T], BF16)
            kT = self.sb(st, "mm_kT", [128, 4, 256], BF16)
            v = self.sb(st, "mm_v", [128, 2, 4, 65], BF16)
            wo = self.sb(st, "mm_wo", [128, 2, D], BF16)
            b_q, b_k, b_v, b_wo = Buf(), Buf(), Buf(), Buf()
            self.ms(v[:, :, :, 64:65], 1.0, [b_v])
            self.lin_fm(self.w["w_mem_q"][l], 0, 256, hT, hT_b, T, lambda ci, tb: qT[:, ci, tb * 512:(tb + 1) * 512], b_q)
            self.ms(kT[:], 0.0, [b_k])
            self.lin_fm(self.w["w_mem_k"][l], 0, 256, memT, b_memT, 256,
                        lambda ci, tb: [(slice(0, 64), kT[0:64, 2 * ci, :]), (slice(64, 128), kT[64:128, 2 * ci + 1, :])], b_k)
            self.lin_tm(self.w["w_mem_v"][l], 0, 256, memT, b_memT, 2, lambda j, cb, n: v[:, j, :, 0:64], b_v)
            for cb in range(0, D, 512):
                self.wload(self.w["w_mem_o"][l], 0, 2, cb, 512, dst=wo[:, :, cb:cb + 512], dst_b=b_wo)
            gt = self.sb(st, "mm_g", [128, D], F32)
            b_gt = Buf()
            self.dma(gt[:], self.w["g_post_mem"][l:l + 1, :].partition_broadcast(128), [], [b_gt])
            post = self.post_tiles(st)
            P_t = [self.sb(st, "mm_P%d" % i, [128, 512], BF16) for i in range(2)]
            bP = [Buf() for _ in range(2)]
            rden = self.sb(st, "mm_rd", [128, 4], F32)
            o_t = self.sb(st, "mm_o", [128, 4, 64], BF16)
            oT = self.sb(st, "mm_oT", [128, 2, 128], BF16)
            b_o, b_oT = Buf(), Buf()
            macc = self.sb(st, "mm_acc", [128, 260], F32)
            b_macc = Buf()
            step = 0
            for i in range(NT):
                qsl = slice(i * 128, (i + 1) * 128)
                self.ms(macc[:], 0.0, [b_macc])
                for kb in range(2):
                    s2 = step % 2
                    step += 1
                    ksl = slice(kb * 128, (kb + 1) * 128)
                    regions = [[(kT[:, hh, ksl], qT[:, hh // 2, qsl], [b_q, b_k])] for hh in range(4)]
                    vl = [(v[:, kb, hh, :], [b_v]) for hh in range(4)]
                    self.attn_step(s2, regions, 128, P_t[s2], bP[s2], 2 + s2, 65, vl, macc, b_macc)
                pv3 = macc[:, 0:260].rearrange("p (h c) -> p h c", h=4)
                self.rcp(rden[:], pv3[:, :, 64], [b_macc], [b_o])
                self.tt(o_t[:], pv3[:, :, 0:64], rden[:].unsqueeze(2).to_broadcast([128, 4, 64]), ALU.mult,
                        [b_macc, b_o], [b_o])
                pT = self.ps[4][:].bitcast(BF16)
                of = o_t[:].rearrange("p h c -> p (h c)")
                for c in range(2):
                    self.tr(pT[:, c * 128:(c + 1) * 128], of[:, c * 128:(c + 1) * 128], self.identb[:],
                            [b_o, self.b_const], [self.pb[4]])
                self.cp(oT[:], pT[:, 0:256].rearrange("p (c n) -> p c n", c=2), [self.pb[4]], [b_oT])
                ob = (6, 7)
                for h2 in range(2):
                    for kc in range(2):
                        self.mm(self.ps[ob[h2]][:, :], oT[:, kc, :], wo[:, kc, h2 * 512:(h2 + 1) * 512], kc == 0, kc == 1,
                                [b_oT, b_wo], [self.pb[ob[h2]]])
                self.post_norm_add(i, ob, gt, b_gt, post)
            S.emit()

    def ffn(self, l, hT, hT_b):
        S = self.S
        with contextlib.ExitStack() as st:
            with contextlib.ExitStack() as s1:
                self.norm_to_hT(self.out, NT, "g_pre_ffn", l, hT, hT_b, s1)
                S.emit()
            NK = D_FF // 128
            wd = self.sb(st, "ff_wd", [128, NK, D], BF16)
            b_wd = Buf()
            for k0 in range(0, NK, 8):
                kn = min(8, NK - k0)
                for cb in range(0, D, 256):
                    self.wload(self.w["w_ffn_down"][l], k0 * 128, kn, cb, 256, dst=wd[:, k0:k0 + kn, cb:cb + 256], dst_b=b_wd)
            gt = self.sb(st, "ff_g", [128, D], F32)
            b_gt = Buf()
            self.dma(gt[:], self.w["g_post_ffn"][l:l + 1, :].partition_broadcast(128), [], [b_gt])
            post = self.post_tiles(st)
            aT = self.sb(st, "ff_aT", [128, NK, 1024], BF16)
            sg = [self.sb(st, "ff_sg%d" % i, [128, 512], F32) for i in range(2)]
            b_sg = [Buf() for _ in range(2)]
            for half in range(2):
                b_aT = Buf()
                t0 = half * 1024
                cnt = 0
                for c3 in range(0, NK, 2):
                    nch = min(2, NK - c3)
                    wg, wg_b = self.wload(self.w["w_ffn_gate"][l], 0, 8, c3 * 128, nch * 128)
                    wu, wu_b = self.wload(self.w["w_ffn_up"][l], 0, 8, c3 * 128, nch * 128)
                    for cc in range(nch):
                        for tb in range(2):
                            k = cnt % 2
                            cnt += 1
                            gb, ub = k, 2 + k
                            tsl = slice(t0 + tb * 512, t0 + (tb + 1) * 512)
                            for kc in range(8):
                                self.mm(self.ps[gb][:, :], wg[:, kc, cc * 128:(cc + 1) * 128], hT[:, kc, tsl], kc == 0, kc == 7,
                                        [wg_b, hT_b], [self.pb[gb]])
                            for kc in range(8):
                                self.mm(self.ps[ub][:, :], wu[:, kc, cc * 128:(cc + 1) * 128], hT[:, kc, tsl], kc == 0, kc == 7,
                                        [wu_b, hT_b], [self.pb[ub]])
                            self.act(sg[k][:], self.ps[gb][:, :], AF.Silu, [self.pb[gb]], [b_sg[k]])
                            self.tt(aT[:, c3 + cc, tb * 512:(tb + 1) * 512], self.ps[ub][:, :], sg[k][:], ALU.mult,
                                    [self.pb[ub], b_sg[k]], [b_aT])
                for jj in range(8):
                    j = half * 8 + jj
                    ob = (4 + 2 * (jj % 2), 5 + 2 * (jj % 2))
                    for h2 in range(2):
                        for kc in range(NK):
                            self.mm(self.ps[ob[h2]][:, :], aT[:, kc, jj * 128:(jj + 1) * 128], wd[:, kc, h2 * 512:(h2 + 1) * 512],
                                    kc == 0, kc == NK - 1, [b_aT, b_wd], [self.pb[ob[h2]]])
                    self.post_norm_add(j, ob, gt, b_gt, post)
            S.emit()


_CACHE = {}


def _perm_w_in(w_in):
    w = np.array(w_in, copy=True)
    order = [0, 4, 1, 5, 2, 6, 3, 7]
    src = w_in[:, :, C_NQ:C_NQ + 512].reshape(w_in.shape[0], w_in.shape[1], 8, 64)
    w[:, :, C_NQ:C_NQ + 512] = src[:, :, order, :].reshape(w_in.shape[0], w_in.shape[1], 512)
    return w


def kernel(**inputs):
    depth = DEBUG_LAYERS or DEPTH
    if "prog" not in _CACHE:
        _CACHE["prog"] = Prog(depth)
    prog = _CACHE["prog"]
    cs = _consts()
    base = {("c_" + k): v for k, v in cs.items()}
    for k in W_SHAPES:
        a = np.ascontiguousarray(np.asarray(inputs[k], dtype=np.float32))
        if k == "w_in":
            a = _perm_w_in(a)
        base[k] = a
    x = np.asarray(inputs["x"], dtype=np.float32)
    mem = np.asarray(inputs["mem"], dtype=np.float32)
    pos = np.asarray(inputs["positions"], dtype=np.int32)
    in_maps = []
    for b in range(8):
        m = dict(base)
        m["x"] = np.ascontiguousarray(x[b])
        m["mem"] = np.ascontiguousarray(mem[b])
        m["positions"] = np.ascontiguousarray(pos[b:b + 1])
        in_maps.append(m)
    res = run_bass_kernel_spmd(prog.nc, in_maps, core_ids=list(range(8)))
    return np.stack([np.asarray(r["out"], dtype=np.float32) for r in res.results], axis=0)
```

```python
import contextlib
import math
import numpy as np
import ml_dtypes
import concourse.bass as bass
import concourse.mybir as mybir
from concourse.bass_utils import run_bass_kernel_spmd

F32 = mybir.dt.float32
BF16 = mybir.dt.bfloat16
I32 = mybir.dt.int32
AF = mybir.ActivationFunctionType
ALU = mybir.AluOpType
AX = mybir.AxisListType

ENGS = ("pe", "act", "dve", "pool", "sp")
DMA_RING = 6
T = 2048
D = 1024
NT = 16
DEPTH = 2
D_IN = 7456
D_FF = 2816
BIG = 30000.0
WMAX = 2048
DEBUG_LAYERS = None


class Buf:
    __slots__ = ("name", "w", "r")

    def __init__(self, name=""):
        self.name = name
        self.w = None
        self.r = []


class Sched:
    def __init__(self, nc, stack):
        self.nc = nc
        self.sems = {e: stack.enter_context(nc.semaphore("s_" + e)) for e in ENGS}
        self.ring = {q: [stack.enter_context(nc.semaphore("r_%s%d" % (q, i))) for i in range(DMA_RING)]
                     for q in ("sp",)}
        self.cnt = {e: 0 for e in ENGS}
        self.ring_n = {q: 0 for q in self.ring}
        self.ring_val = {q: [0] * DMA_RING for q in self.ring}
        self.reset()

    def reset(self):
        self.ops = []
        self.touched = {}

    def add(self, eng, fn, reads=(), writes=(), dma=False):
        import os
        if len(self.ops) >= int(os.environ.get("NOPS", "100000000")):
            return
        deps = set()
        for b in reads:
            if b.w is not None:
                deps.add(b.w)
        for b in writes:
            if b.w is not None:
                deps.add(b.w)
            deps.update(b.r)
        i = len(self.ops)
        self.ops.append(dict(eng=eng, fn=fn, deps=deps, dma=dma, sig=False))
        for b in reads:
            b.r.append(i)
            self.touched[id(b)] = b
        for b in writes:
            b.w = i
            b.r = []
            self.touched[id(b)] = b
        return i

    def emit(self):
        nc = self.nc
        ops = self.ops
        for op in ops:
            for d in op["deps"]:
                od = ops[d]
                if od["dma"] or od["eng"] != op["eng"] or op["eng"] != "pe":
                    od["sig"] = True
        for op in ops:
            e = op["eng"]
            if op["dma"]:
                n = self.ring_n[e]
                self.ring_n[e] += 1
                slot = n % DMA_RING
                prev = self.ring_val[e][slot]
                self.ring_val[e][slot] = prev + 16
                op["sem"] = self.ring[e][slot]
                op["val"] = prev + 16
                op["prev"] = prev
            elif op["sig"]:
                self.cnt[e] += 1
                op["sem"] = self.sems[e]
                op["val"] = self.cnt[e]
        per = {e: [] for e in ENGS}
        for i, op in enumerate(ops):
            per[op["eng"]].append(i)

        def run(e, engobj):
            seen = {}
            for i in per[e]:
                op = ops[i]
                waits = {}
                for d in sorted(op["deps"]):
                    od = ops[d]
                    if not od["dma"] and od["eng"] == e and e == "pe":
                        continue
                    s = od["sem"]
                    k = id(s)
                    if seen.get(k, 0) >= od["val"]:
                        continue
                    if k not in waits or waits[k][1] < od["val"]:
                        waits[k] = (s, od["val"])
                if op["dma"] and op["prev"] > 0:
                    s = op["sem"]
                    k = id(s)
                    if seen.get(k, 0) < op["prev"]:
                        if k not in waits or waits[k][1] < op["prev"]:
                            waits[k] = (s, op["prev"])
                for k, (s, v) in waits.items():
                    engobj.wait_ge(s, v)
                    seen[k] = v
                ins = op["fn"](engobj)
                if op["dma"]:
                    ins.then_inc(op["sem"], 16)
                elif op["sig"]:
                    ins.then_inc(op["sem"], 1)
            if e in self.ring:
                for slot in range(DMA_RING):
                    v = self.ring_val[e][slot]
                    if v > 0 and seen.get(id(self.ring[e][slot]), 0) < v:
                        engobj.wait_ge(self.ring[e][slot], v)

        with nc.Block() as block:
            @block.tensor
            def _(eng):
                run("pe", eng)

            @block.scalar
            def _(eng):
                run("act", eng)

            @block.vector
            def _(eng):
                run("dve", eng)

            @block.gpsimd
            def _(eng):
                run("pool", eng)

            @block.sync
            def _(eng):
                run("sp", eng)
        for b in self.touched.values():
            b.w = None
            b.r = []
        self.reset()


def _consts():
    bf = ml_dtypes.bfloat16
    j = np.arange(128)[:, None]
    t = np.arange(128)[None, :]
    c = {}
    c["identb"] = np.eye(128, dtype=np.float32).astype(bf)
    c["identf"] = np.eye(128, dtype=np.float32)
    c["tri_sb"] = np.where(j >= t, -BIG, 0.0).astype(bf)
    c["tri_c"] = np.where(j > t, -BIG, 0.0).astype(bf)
    c["tri_band"] = np.where(j <= t, -BIG, 0.0).astype(bf)
    c["negut8"] = np.where(j >= t, -8.0, 0.0).astype(bf)
    c["neg8ones"] = np.full((128, 128), -8.0, np.float32).astype(bf)
    rot = np.zeros((128, 128), np.float32)
    for blk in (0, 64):
        for m in range(8):
            rot[blk + m + 8, blk + m] = -1.0
            rot[blk + m, blk + m + 8] = 1.0
    c["rotT"] = rot.astype(bf)
    rot_b = np.zeros((128, 64), np.float32)
    sel_b = np.zeros((128, 64), np.float32)
    for m in range(64):
        sel_b[64 + m, m] = 1.0
    for m in range(8):
        rot_b[64 + m + 8, m] = -1.0
        rot_b[64 + m, m + 8] = 1.0
    c["rot_b"] = rot_b.astype(bf)
    c["sel_b"] = sel_b.astype(bf)
    c["tri_c4"] = np.tile(np.where(j > t, -BIG, 0.0), (1, 4)).astype(bf)
    c["tri_band4"] = np.tile(np.where(j <= t, -BIG, 0.0), (1, 4)).astype(bf)
    exr = np.zeros((32, T), np.float32)
    for key in range(T):
        exr[key // 64, key] = BIG
    c["exrows"] = exr.astype(bf)
    half = 8
    inv = (500000.0 ** (-np.arange(half, dtype=np.float32) / half)).astype(np.float32)
    invf = np.zeros((128, 1), np.float32)
    for p in range(128):
        if p % 64 < 16:
            invf[p, 0] = inv[(p % 64) % 8]
    c["invf"] = invf
    cidx = np.arange(127)[:, None]
    tt_ = np.arange(T)[None, :]
    c["cmpbias"] = np.where(16 * cidx + 31 <= tt_, 0.0, -BIG).astype(bf)
    ex = np.zeros((32, 16, 128), np.float32)
    for kb in range(16):
        for p in range(128):
            ex[2 * kb + (p >= 64), kb, p] = BIG
    c["expand"] = ex.astype(bf)
    vm = np.zeros((128, 16, 32), np.float32)
    addc = np.zeros((128, 16, 32), np.float32)
    for i in range(16):
        for p in range(128):
            tpos = 128 * i + p
            for n in range(32):
                forced = (n == 0) or (n == tpos // 64)
                valid = 64 * n <= tpos
                if forced:
                    addc[p, i, n] = 1e4
                elif valid:
                    vm[p, i, n] = 1.0
                else:
                    addc[p, i, n] = -1.0
    c["vm"] = vm
    c["addc"] = addc
    cs = np.arange(127)[:, None] * 16
    ss = np.arange(32)[None, :] * 64
    c["ov"] = ((cs < ss + 64) & (cs + 32 > ss)).astype(np.float32).astype(bf)
    return c


CONST_DT = dict(identb=BF16, identf=F32, tri_sb=BF16, tri_c=BF16, tri_band=BF16, negut8=BF16, neg8ones=BF16,
                rotT=BF16, rot_b=BF16, sel_b=BF16, tri_c4=BF16, tri_band4=BF16, exrows=BF16, invf=F32, cmpbias=BF16, expand=BF16, vm=F32, addc=F32, ov=BF16)

W_SHAPES = dict(
    g_pre_mix=[DEPTH, D], g_post_mix=[DEPTH, D], g_pre_mem=[DEPTH, D], g_mem=[DEPTH, D], g_post_mem=[DEPTH, D],
    g_pre_ffn=[DEPTH, D], g_post_ffn=[DEPTH, D], w_in=[DEPTH, D, D_IN], b_fox_f=[DEPTH, 8],
    cmp_pe_k=[DEPTH, 32, 64], cmp_w1_k=[DEPTH, 2048, 256], cmp_b1_k=[DEPTH, 256], cmp_w2_k=[DEPTH, 256, 64],
    cmp_pe_v=[DEPTH, 32, 64], cmp_w1_v=[DEPTH, 2048, 256], cmp_b1_v=[DEPTH, 256], cmp_w2_v=[DEPTH, 256, 64],
    w_up_sb=[DEPTH, 512, D], w_up_nsa=[DEPTH, 512, D], w_up_fox=[DEPTH, 512, D], w_out=[DEPTH, D, D],
    w_mem_q=[DEPTH, D, 256], w_mem_k=[DEPTH, D, 256], w_mem_v=[DEPTH, D, 256], w_mem_o=[DEPTH, 256, D],
    w_ffn_gate=[DEPTH, D, D_FF], w_ffn_up=[DEPTH, D, D_FF], w_ffn_down=[DEPTH, D_FF, D])

C_SBQ, C_SBK, C_SBV = 0, 512, 1024
C_NQ, C_KC, C_VC, C_KS, C_VS, C_KW, C_VW, C_NG = 1536, 2048, 2176, 2304, 2432, 2560, 2688, 2816
C_FQ, C_FK, C_FV, C_FF, C_MG = 2840, 3352, 3864, 4376, 4384


class Prog:
    def __init__(self, depth=DEPTH, dbg=None, stop=None):
        self.depth = depth
        self.dbg = dbg
        self.stop = stop
        nc = self.nc = bass.Bass("TRN2", target_bir_lowering=False)
        self.x_in = nc.dram_tensor("x", [T, D], F32, kind="ExternalInput").ap()
        self.mem_in = nc.dram_tensor("mem", [256, D], F32, kind="ExternalInput").ap()
        self.pos_in = nc.dram_tensor("positions", [1, T], I32, kind="ExternalInput").ap()
        self.w = {k: nc.dram_tensor(k, s, F32, kind="ExternalInput").ap() for k, s in W_SHAPES.items()}
        cs = _consts()
        self.c = {k: nc.dram_tensor("c_" + k, list(v.shape), CONST_DT[k], kind="ExternalInput").ap()
                  for k, v in cs.items()}
        self.out = nc.dram_tensor("out", [T, D], F32, kind="ExternalOutput").ap()
        self.o_scr = nc.dram_tensor("o_scr", [3, T, 512], BF16, kind=("ExternalOutput" if dbg else "Internal")).ap()
        self.row_scr = nc.dram_tensor("row_scr", [8, 4, T], BF16, kind="Internal").ap()
        with contextlib.ExitStack() as st:
            self.S = Sched(nc, st)
            self.ps = [st.enter_context(nc.psum_tensor("ps%d" % i, [128, 512], F32)) for i in range(8)]
            self.pb = [Buf("ps%d" % i) for i in range(8)]
            self.B_x = Buf("x")
            self.B_oscr = Buf("oscr")
            self.st = st
            self.build()

    def mm(self, out, lhsT, rhs, start, stop, R, W):
        self.S.add("pe", lambda e: e.matmul(out, lhsT=lhsT, rhs=rhs, start=start, stop=stop, skip_group_check=True), R, W)

    def tr(self, out, in_, ident, R, W):
        self.S.add("pe", lambda e: e.transpose(out=out, in_=in_, identity=ident), R, W)

    def act(self, out, in_, func, R, W, bias=None, scale=None, accum=None):
        kw = {}
        if bias is not None:
            kw["bias"] = bias
        if scale is not None:
            kw["scale"] = scale
        if accum is not None:
            kw["accum_out"] = accum
        self.S.add("act", lambda e: e.activation(out=out, in_=in_, func=func, **kw), R, W)

    def tt(self, out, in0, in1, op, R, W, eng="dve"):
        self.S.add(eng, lambda e: e.tensor_tensor(out=out, in0=in0, in1=in1, op=op), R, W)

    def ts(self, out, in0, s1, s2, op0, op1, R, W, eng="dve"):
        if op1 is None:
            self.S.add(eng, lambda e: e.tensor_scalar(out=out, in0=in0, scalar1=s1, scalar2=None, op0=op0), R, W)
        else:
            self.S.add(eng, lambda e: e.tensor_scalar(out=out, in0=in0, scalar1=s1, scalar2=s2, op0=op0, op1=op1), R, W)

    def stt(self, out, in0, scalar, in1, op0, op1, R, W):
        self.S.add("dve", lambda e: e.scalar_tensor_tensor(out=out, in0=in0, scalar=scalar, in1=in1, op0=op0, op1=op1), R, W)

    def cp(self, out, in_, R, W, eng="dve"):
        self.S.add(eng, lambda e: e.tensor_copy(out=out, in_=in_), R, W)

    def ms(self, ap, val, W, eng="dve"):
        self.S.add(eng, lambda e: e.memset(ap, val), [], W)

    def rcp(self, out, in_, R, W):
        self.S.add("dve", lambda e: e.reciprocal(out=out, in_=in_), R, W)

    def dma(self, out, in_, R, W, nc_ok=False):
        if nc_ok:
            self.S.add("sp", lambda q: q.dma_start(out=out, in_=in_, allow_slow_non_contiguous=True), R, W, dma=True)
        else:
            self.S.add("sp", lambda q: q.dma_start(out=out, in_=in_), R, W, dma=True)

    def sb(self, st, name, shape, dt):
        self._n = getattr(self, "_n", 0) + 1
        return st.enter_context(self.nc.sbuf_tensor("%s_%d" % (name, self._n), shape, dt))

    def winit(self, st):
        self.wst = [self.sb(st, "wst%d" % i, [128, WMAX], F32) for i in range(2)]
        self.wbf = [self.sb(st, "wbf%d" % i, [128, WMAX], BF16) for i in range(3)]
        self.wst_b = [Buf("wst%d" % i) for i in range(2)]
        self.wbf_b = [Buf("wbf%d" % i) for i in range(3)]
        self.wn = 0

    def wload(self, w2d, r0, kc, c0, n, dst=None, dst_b=None, prows=128):
        assert kc * n <= WMAX
        i = self.wn
        self.wn += 1
        stg, stg_b = self.wst[i % 2], self.wst_b[i % 2]
        src = w2d[r0:r0 + kc * prows, c0:c0 + n].rearrange("(c p) n -> p c n", p=prows)
        sview = stg[0:prows, 0:kc * n].rearrange("p (c n) -> p c n", c=kc)
        self.dma(sview, src, [], [stg_b])
        if dst is None:
            j = i % 3
            dst = self.wbf[j][0:prows, 0:kc * n].rearrange("p (c n) -> p c n", c=kc)
            dst_b = self.wbf_b[j]
        self.cp(dst, sview, [stg_b], [dst_b], eng="pool")
        return dst, dst_b

    def lin_fm(self, w2d, c0, ncols, hT, hT_b, ntok, out_fn, out_b, chunk=128, func=AF.Identity, bias_fn=None,
               banks=(6, 7), bias_b=None):
        tbw = min(512, ntok)
        ntb = ntok // tbw
        per = max(chunk, (WMAX // 8) // chunk * chunk)
        cnt = 0
        for g0 in range(0, ncols, per):
            gn = min(per, ncols - g0)
            wb, wb_b = self.wload(w2d, 0, 8, c0 + g0, gn)
            for cc in range(gn // chunk):
                ci = (g0 // chunk) + cc
                for tb in range(ntb):
                    bk = banks[cnt % len(banks)]
                    cnt += 1
                    for kc in range(8):
                        self.mm(self.ps[bk][0:chunk, 0:tbw], wb[:, kc, cc * chunk:(cc + 1) * chunk],
                                hT[:, kc, tb * tbw:(tb + 1) * tbw], kc == 0, kc == 7, [wb_b, hT_b], [self.pb[bk]])
                    outs = out_fn(ci, tb)
                    if not isinstance(outs, list):
                        outs = [(slice(0, chunk), outs)]
                    for (psl, dst) in outs:
                        self.act(dst, self.ps[bk][psl, 0:tbw], func, [self.pb[bk]] + ([bias_b] if bias_b else []), [out_b],
                                 bias=(bias_fn(ci) if bias_fn else None))

    def lin_tm(self, w2d, c0, ncols, hT, hT_b, ntiles, out_fn, out_b, func=AF.Identity, banks=(6, 7), blk=256):
        cnt = 0
        for cb in range(0, ncols, blk):
            n = min(blk, ncols - cb)
            wb, wb_b = self.wload(w2d, 0, 8, c0 + cb, n)
            for j in range(ntiles):
                bk = banks[cnt % len(banks)]
                cnt += 1
                for kc in range(8):
                    self.mm(self.ps[bk][:, 0:n], hT[:, kc, j * 128:(j + 1) * 128], wb[:, kc, 0:n],
                            kc == 0, kc == 7, [wb_b, hT_b], [self.pb[bk]])
                self.act(out_fn(j, cb, n), self.ps[bk][:, 0:n], func, [self.pb[bk]], [out_b])

    def rstd_from_ss(self, ss, rstd, b_ss, b_rstd, n=D):
        self.ts(rstd, ss, 1.0 / n, 1e-6, ALU.mult, ALU.add, [b_ss], [b_rstd])
        self.act(rstd, rstd, AF.Sqrt, [b_rstd], [b_rstd])
        self.rcp(rstd, rstd, [b_rstd], [b_rstd])

    def norm_to_hT(self, src, ntiles, gname, l, hT, hT_b, st):
        gt = self.sb(st, "n_g", [128, D], F32)
        b_g = Buf("g")
        self.dma(gt[:], self.w[gname][l:l + 1, :].partition_broadcast(128), [], [b_g])
        xs = [self.sb(st, "n_x%d" % i, [128, D], F32) for i in range(2)]
        xb = [Buf() for _ in range(2)]
        sq = self.sb(st, "n_sq", [128, D], F32)
        ssr = [self.sb(st, "n_ss%d" % i, [128, 2], F32) for i in range(2)]
        sb_ = [Buf() for _ in range(2)]
        hb = [self.sb(st, "n_h%d" % i, [128, D], BF16) for i in range(2)]
        hbb = [Buf() for _ in range(2)]
        b_sq = Buf()
        for j in range(ntiles):
            k = j % 2
            self.dma(xs[k][:], src[j * 128:(j + 1) * 128, :], [self.B_x], [xb[k]])
            self.act(sq[:], xs[k][:], AF.Square, [xb[k]], [b_sq, sb_[k]], accum=ssr[k][:, 0:1])
            self.rstd_from_ss(ssr[k][:, 0:1], ssr[k][:, 1:2], sb_[k], sb_[k])
            self.stt(hb[k][:], xs[k][:], ssr[k][:, 1:2], gt[:], ALU.mult, ALU.mult, [xb[k], sb_[k], b_g], [hbb[k]])
            bk = 4 + k
            pT = self.ps[bk][:].bitcast(BF16)
            for c in range(8):
                self.tr(pT[:, c * 128:(c + 1) * 128], hb[k][:, c * 128:(c + 1) * 128], self.identb[:],
                        [hbb[k], self.b_const], [self.pb[bk]])
            self.cp(hT[:, :, j * 128:(j + 1) * 128], pT[:, 0:1024].rearrange("p (c n) -> p c n", c=8),
                    [self.pb[bk]], [hT_b])

    def post_norm_add(self, j, banks, gt, b_g, st_tiles):
        sq, ss, xt, yt, bufs = st_tiles
        k = j % 2
        b_ss, b_x, b_y, b_sq = bufs[k]
        for h in range(2):
            self.act(sq[:, 0:512], self.ps[banks[h]][:, :], AF.Square, [self.pb[banks[h]]], [b_sq, b_ss],
                     accum=ss[k][:, h:h + 1])
        self.tt(ss[k][:, 2:3], ss[k][:, 0:1], ss[k][:, 1:2], ALU.add, [b_ss], [b_ss])
        self.rstd_from_ss(ss[k][:, 2:3], ss[k][:, 3:4], b_ss, b_ss)
        self.dma(xt[k][:], self.out[j * 128:(j + 1) * 128, :], [self.B_x], [b_x])
        for h in range(2):
            self.stt(yt[k][:, h * 512:(h + 1) * 512], self.ps[banks[h]][:, :], ss[k][:, 3:4],
                     gt[:, h * 512:(h + 1) * 512], ALU.mult, ALU.mult, [self.pb[banks[h]], b_ss, b_g], [b_y])
        self.tt(yt[k][:], yt[k][:], xt[k][:], ALU.add, [b_y, b_x], [b_y], eng="pool")
        self.dma(self.out[j * 128:(j + 1) * 128, :], yt[k][:], [b_y], [self.B_x])

    def post_tiles(self, st):
        sq = self.sb(st, "p_sq", [128, 512], F32)
        ss = [self.sb(st, "p_ss%d" % i, [128, 4], F32) for i in range(2)]
        xt = [self.sb(st, "p_x%d" % i, [128, D], F32) for i in range(2)]
        yt = [self.sb(st, "p_y%d" % i, [128, D], F32) for i in range(2)]
        bufs = [(Buf(), Buf(), Buf(), Buf()) for _ in range(2)]
        return (sq, ss, xt, yt, bufs)

    def attn_step(self, lbank, regions, nk, P, P_b, pvbank, ncol, v_list, acc, acc_b, scale=0.125):
        S = self
        pb = self.pb[lbank]
        for r, mms in enumerate(regions):
            cols = slice(r * 128, (r + 1) * 128)
            if isinstance(mms, tuple):
                cols, mms = mms
            for idx, (lhsT, rhs, R) in enumerate(mms):
                o = self.ps[lbank][0:nk, cols]
                if len(rhs.shape) == 3:
                    o = o.rearrange("p (h q) -> p h q", h=rhs.shape[1])
                S.mm(o, lhsT, rhs, idx == 0, idx == len(mms) - 1, R, [pb])
        S.act(P[0:nk, :], self.ps[lbank][0:nk, :], AF.Exp, [pb], [P_b], scale=scale)
        for r, (rhs, R) in enumerate(v_list):
            S.mm(self.ps[pvbank][:, r * ncol:(r + 1) * ncol], P[0:nk, r * 128:(r + 1) * 128], rhs, True, True,
                 [P_b] + R, [self.pb[pvbank]])
        if acc is not None:
            S.tt(acc[:, 0:4 * ncol], acc[:, 0:4 * ncol], self.ps[pvbank][:, 0:4 * ncol], ALU.add,
                 [acc_b, self.pb[pvbank]], [acc_b])

    def build(self):
        nc = self.nc
        st = self.st
        S = self.S
        self.b_const = Buf("const")
        cst = {}
        for k in ("identb", "tri_sb", "tri_c", "tri_band", "negut8", "neg8ones", "rotT"):
            cst[k] = self.sb(st, "k_" + k, [128, 128], BF16)
            self.dma(cst[k][:], self.c[k], [], [self.b_const])
        self.identb = cst["identb"]
        self.cst = cst
        self.onecol = self.sb(st, "k_one", [128, 1], F32)
        self.ms(self.onecol[:], 1.0, [self.b_const])
        self.negpi = self.sb(st, "k_negpi", [128, 1], F32)
        self.ms(self.negpi[:], -math.pi, [self.b_const])
        with contextlib.ExitStack() as s0:
            xt = [self.sb(s0, "c_x%d" % i, [128, 4, D], F32) for i in range(2)]
            xb = [Buf() for _ in range(2)]
            for j in range(4):
                k = j % 2
                self.dma(xt[k][:], self.x_in[j * 512:(j + 1) * 512, :].rearrange("(c p) n -> p c n", p=128), [], [xb[k]])
                self.dma(self.out[j * 512:(j + 1) * 512, :].rearrange("(c p) n -> p c n", p=128), xt[k][:], [xb[k]], [self.B_x])
            S.emit()
        for l in range(self.depth):
            self.layer(l)

    def layer(self, l):
        S = self.S
        with contextlib.ExitStack() as sl:
            hT = self.sb(sl, "hT", [128, 8, T], BF16)
            hT_b = Buf("hT")
            self.winit(sl)
            with contextlib.ExitStack() as s1:
                self.norm_to_hT(self.out, NT, "g_pre_mix", l, hT, hT_b, s1)
                S.emit()
            for nm, fn in (("sb", self.sb_branch), ("fox", self.fox_branch), ("nsa", self.nsa_branch),
                           ("merge", self.merge), ("mem", self.mem_attn), ("ffn", self.ffn)):
                if self.stop is not None and nm not in self.stop:
                    continue
                fn(l, hT, hT_b)

    def sb_branch(self, l, hT, hT_b):
        S = self.S
        w_in = self.w["w_in"][l]
        with contextlib.ExitStack() as st:
            qT = self.sb(st, "sb_qT", [128, 4, T], BF16)
            kT = self.sb(st, "sb_kT", [128, 8, T], BF16)
            v = self.sb(st, "sb_v", [128, NT, 512], BF16)
            b_q, b_k, b_v = Buf(), Buf(), Buf()
            self.lin_fm(w_in, C_SBQ, 512, hT, hT_b, T, lambda ci, tb: qT[:, ci, tb * 512:(tb + 1) * 512], b_q)
            self.ms(kT[:], 0.0, [b_k])
            self.lin_fm(w_in, C_SBK, 512, hT, hT_b, T,
                        lambda ci, tb: [(slice(0, 64), kT[0:64, 2 * ci, tb * 512:(tb + 1) * 512]),
                                        (slice(64, 128), kT[64:128, 2 * ci + 1, tb * 512:(tb + 1) * 512])], b_k)
            self.lin_tm(w_in, C_SBV, 512, hT, hT_b, NT, lambda j, cb, n: v[:, j, cb:cb + n], b_v)
            e_t = [self.sb(st, "sb_e%d" % i, [128, 512], F32) for i in range(2)]
            L_t = [self.sb(st, "sb_L%d" % i, [128, 512], BF16) for i in range(2)]
            tmp = [self.sb(st, "sb_t%d" % i, [128, 512], F32) for i in range(2)]
            P_t = [self.sb(st, "sb_P%d" % i, [128, 512], BF16) for i in range(2)]
            carry = [self.sb(st, "sb_c%d" % i, [128, 512], F32) for i in range(2)]
            o_t = [self.sb(st, "sb_o%d" % i, [128, 256], BF16) for i in range(2)]
            be, bL, bt, bP, bc, bo = ([Buf() for _ in range(2)] for _ in range(6))
            tri = self.cst["tri_sb"]
            accs = [self.sb(st, "sb_acc%d" % i, [128, 256], F32) for i in range(2)]
            bacc = [Buf() for _ in range(2)]
            step = 0
            it = 0
            for hg in range(2):
                for i in range(NT):
                    ci = it % 2
                    it += 1
                    self.ms(carry[ci][:], 0.0, [bc[ci]])
                    self.ms(accs[ci][:], 0.0, [bacc[ci]])
                    for kb in range(i, -1, -1):
                        s2 = step % 2
                        step += 1
                        zb, wbk, cbk, pvb = s2, 2 + s2, 6, 4 + s2
                        ksl = slice(kb * 128, (kb + 1) * 128)
                        qsl = slice(i * 128, (i + 1) * 128)
                        for hh in range(4):
                            h = 4 * hg + hh
                            cols = slice(hh * 128, (hh + 1) * 128)
                            self.mm(self.ps[zb][:, cols], kT[:, h, ksl], qT[:, h // 2, qsl], True, kb != i,
                                    [b_q, b_k], [self.pb[zb]])
                            if kb == i:
                                self.mm(self.ps[zb][:, cols], self.identb[:], tri[:], False, True,
                                        [self.b_const], [self.pb[zb]])
                        self.act(e_t[s2][:], self.ps[zb][:, :], AF.Exp, [self.pb[zb]], [be[s2]], scale=0.125)
                        self.act(L_t[s2][:], e_t[s2][:], AF.Ln, [be[s2], self.b_const], [bL[s2]], bias=self.onecol[:, 0:1])
                        for hh in range(4):
                            h = 4 * hg + hh
                            cols = slice(hh * 128, (hh + 1) * 128)
                            self.mm(self.ps[wbk][:, cols], kT[:, h, ksl], qT[:, h // 2, qsl], True, False,
                                    [b_q, b_k], [self.pb[wbk]])
                            if kb == i:
                                self.mm(self.ps[wbk][:, cols], self.identb[:], tri[:], False, False,
                                        [self.b_const], [self.pb[wbk]])
                            self.mm(self.ps[wbk][:, cols], self.cst["negut8"][:], L_t[s2][:, cols], False, True,
                                    [bL[s2], self.b_const], [self.pb[wbk]])
                        if kb > 0:
                            self.mm(self.ps[cbk][:, :], self.cst["neg8ones"][:], L_t[s2][:], True, True,
                                    [bL[s2], self.b_const], [self.pb[cbk]])
                        self.tt(tmp[s2][:], self.ps[wbk][:, :], carry[ci][:], ALU.add, [self.pb[wbk], bc[ci]], [bt[s2]])
                        self.act(P_t[s2][:], tmp[s2][:], AF.Exp, [bt[s2]], [bP[s2]], scale=0.125)
                        if kb > 0:
                            self.tt(carry[ci][:], self.ps[cbk][:, :], carry[ci][:], ALU.add, [self.pb[cbk], bc[ci]], [bc[ci]])
                        for hh in range(4):
                            h = 4 * hg + hh
                            self.mm(self.ps[pvb][:, hh * 64:(hh + 1) * 64], P_t[s2][:, hh * 128:(hh + 1) * 128],
                                    v[:, kb, h * 64:(h + 1) * 64], True, True, [bP[s2], b_v], [self.pb[pvb]])
                        self.tt(accs[ci][:], accs[ci][:], self.ps[pvb][:, 0:256], ALU.add, [bacc[ci], self.pb[pvb]], [bacc[ci]])
                    self.cp(o_t[ci][:], accs[ci][:], [bacc[ci]], [bo[ci]], eng="pool")
                    self.dma(self.o_scr[0, i * 128:(i + 1) * 128, hg * 256:(hg + 1) * 256], o_t[ci][:], [bo[ci]], [self.B_oscr])
            S.emit()

    def fox_branch(self, l, hT, hT_b):
        S = self.S
        w_in = self.w["w_in"][l]
        with contextlib.ExitStack() as st:
            v = self.sb(st, "fx_v", [128, NT, 8, 65], BF16)
            b_v = Buf()
            self.ms(v[:, :, :, 64:65], 1.0, [b_v])
            self.lin_tm(w_in, C_FV, 512, hT, hT_b, NT,
                        lambda j, cb, n: v[:, j, cb // 64:(cb + n) // 64, 0:64], b_v)
            rows = self.sb(st, "fx_rows", [8, 4, T], BF16)
            b_rows = Buf()
            with contextlib.ExitStack() as s2:
                fT = self.sb(s2, "fx_f", [8, T], F32)
                ones = self.sb(s2, "fx_ones", [8, T], F32)
                cT = self.sb(s2, "fx_c", [8, T], F32)
                hif = self.sb(s2, "fx_hif", [8, T], F32)
                bcol = self.sb(s2, "fx_b", [8, 1], F32)
                b_f, b_o, b_c, b_h, b_b = Buf(), Buf(), Buf(), Buf(), Buf()
                self.dma(bcol[:], self.w["b_fox_f"][l:l + 1, :].rearrange("o h -> h o"), [], [b_b], nc_ok=True)
                self.ms(ones[:], 1.0, [b_o])
                self.lin_fm(w_in, C_FF, 8, hT, hT_b, T, lambda ci, tb: fT[:, tb * 512:(tb + 1) * 512], b_f, chunk=8,
                            bias_fn=lambda ci: bcol[:, 0:1], bias_b=b_b)
                self.act(fT[:], fT[:], AF.Exp, [b_f, b_b], [b_f], scale=-1.0)
                self.act(fT[:], fT[:], AF.Ln, [b_f, self.b_const], [b_f], bias=self.onecol[0:8, 0:1])
                self.ts(fT[:], fT[:], -1.0, None, ALU.mult, None, [b_f], [b_f])
                S.add("dve", lambda e: e.tensor_tensor_scan(out=cT[:], data0=fT[:], data1=ones[:], initial=0.0,
                                                            op0=ALU.add, op1=ALU.mult), [b_f, b_o], [b_c])
                self.ts(cT[:], cT[:], -8.0, None, ALU.mult, None, [b_c], [b_c])
                self.cp(rows[:, 0, :], cT[:], [b_c], [b_rows])
                self.cp(hif[:], rows[:, 0, :], [b_rows], [b_h])
                self.tt(rows[:, 1, :], cT[:], hif[:], ALU.subtract, [b_c, b_h], [b_rows])
                self.ts(rows[:, 2, :], hif[:], -1.0, None, ALU.mult, None, [b_h], [b_rows])
                b_rs = Buf()
                self.cp(rows[:, 3, :], ones[:], [b_o], [b_rows])
                self.dma(self.row_scr[:, :, :], rows[:], [b_rows], [b_rs])
                S.emit()
            qa = self.sb(st, "fx_qa", [96, 4, T], BF16)
            ka = self.sb(st, "fx_ka", [96, 4, T], BF16)
            P_t = [self.sb(st, "fx_P%d" % i, [128, 512], BF16) for i in range(2)]
            bP = [Buf() for _ in range(2)]
            rden = [self.sb(st, "fx_rd%d" % i, [128, 4], F32) for i in range(2)]
            o_t = [self.sb(st, "fx_o%d" % i, [128, 4, 64], BF16) for i in range(2)]
            bo = [Buf() for _ in range(2)]
            b_q, b_k = Buf(), Buf()
            tri = self.cst["tri_c"]
            facc = [self.sb(st, "fx_acc%d" % i, [128, 260], F32) for i in range(2)]
            bfacc = [Buf() for _ in range(2)]
            for hg in range(2):
                self.ms(qa[64:96, :, :], 0.0, [b_q])
                self.ms(ka[64:96, :, :], 0.0, [b_k])
                for hh in range(4):
                    h = 4 * hg + hh
                    self.dma(ka[64:66, hh, :], self.row_scr[h, 0:2, :], [], [b_k])
                    self.dma(ka[66:67, hh, :], self.row_scr[h, 3:4, :], [], [b_k])
                    self.dma(qa[64:65, hh, :], self.row_scr[h, 3:4, :], [], [b_q])
                    self.dma(qa[65:66, hh, :], self.row_scr[h, 3:4, :], [], [b_q])
                    self.dma(qa[66:67, hh, :], self.row_scr[h, 2:3, :], [], [b_q])
                self.lin_fm(w_in, C_FQ + hg * 256, 256, hT, hT_b, T, lambda ci, tb: qa[0:64, ci, tb * 512:(tb + 1) * 512],
                            b_q, chunk=64)
                self.lin_fm(w_in, C_FK + hg * 256, 256, hT, hT_b, T, lambda ci, tb: ka[0:64, ci, tb * 512:(tb + 1) * 512],
                            b_k, chunk=64)
                step = 0
                for i in range(NT):
                    ci = i % 2
                    qsl = slice(i * 128, (i + 1) * 128)
                    self.ms(facc[ci][:], 0.0, [bfacc[ci]])
                    for kb in range(i, -1, -1):
                        s2 = step % 2
                        step += 1
                        ksl = slice(kb * 128, (kb + 1) * 128)
                        regions = []
                        for hh in range(4):
                            mms = [(ka[0:96, hh, ksl], qa[0:96, hh, qsl], [b_q, b_k])]
                            if kb == i:
                                mms.append((self.identb[:], tri[:], [self.b_const]))
                            regions.append(mms)
                        vl = [(v[:, kb, 4 * hg + hh, :], [b_v]) for hh in range(4)]
                        self.attn_step(s2, regions, 128, P_t[s2], bP[s2], 4 + s2, 65, vl, facc[ci], bfacc[ci])
                    pv3 = facc[ci][:, 0:260].rearrange("p (h c) -> p h c", h=4)
                    self.rcp(rden[ci][:], pv3[:, :, 64], [bfacc[ci]], [bo[ci]])
                    self.tt(o_t[ci][:], pv3[:, :, 0:64], rden[ci][:].unsqueeze(2).to_broadcast([128, 4, 64]), ALU.mult,
                            [bfacc[ci], bo[ci]], [bo[ci]])
                    self.dma(self.o_scr[2, i * 128:(i + 1) * 128, hg * 256:(hg + 1) * 256],
                             o_t[ci][:].rearrange("p h c -> p (h c)"), [bo[ci]], [self.B_oscr])
                S.emit()

    def nsa_branch(self, l, hT, hT_b):
        S = self.S
        w_in = self.w["w_in"][l]
        with contextlib.ExitStack() as st:
            qT = self.sb(st, "ns_qT", [128, 4, T], BF16)
            qrT = self.sb(st, "ns_qrT", [128, 8, T], BF16)
            ksr = self.sb(st, "ns_ksr", [128, 2, T], BF16)
            kwr = self.sb(st, "ns_kwr", [128, 2, T], BF16)
            tri4 = self.sb(st, "ns_tri4", [128, 2, 512], BF16)
            rotb = self.sb(st, "ns_rotb", [128, 2, 64], BF16)
            b_tri4 = Buf()
            self.dma(tri4[:, 0, :], self.c["tri_c4"], [], [b_tri4])
            self.dma(tri4[:, 1, :], self.c["tri_band4"], [], [b_tri4])
            self.dma(rotb[:, 0, :], self.c["rot_b"], [], [b_tri4])
            self.dma(rotb[:, 1, :], self.c["sel_b"], [], [b_tri4])
            vs = self.sb(st, "ns_vs", [128, NT, 2, 65], BF16)
            vw = self.sb(st, "ns_vw", [128, NT, 2, 65], BF16)
            gates = self.sb(st, "ns_g", [128, NT, 24], F32)
            kcmpT = self.sb(st, "ns_kcmpT", [128, 2, 127], BF16)
            vcmp = self.sb(st, "ns_vcmp", [127, 2, 97], BF16)
            b_q, b_qr, b_ks, b_kw, b_vs, b_vw, b_g, b_kc, b_vc = (Buf() for _ in range(9))
            with contextlib.ExitStack() as sa:
                kcT = self.sb(sa, "ns_kcT", [128, 2, T], BF16)
                vcT = self.sb(sa, "ns_vcT", [128, 2, T], BF16)
                ksT = self.sb(sa, "ns_ksT", [128, T], BF16)
                kwT = self.sb(sa, "ns_kwT", [128, T], BF16)
                b_kcT, b_vcT, b_ksT, b_kwT = Buf(), Buf(), Buf(), Buf()
                self.lin_fm(w_in, C_NQ, 512, hT, hT_b, T, lambda ci, tb: qT[:, ci, tb * 512:(tb + 1) * 512], b_q)
                for (c0, dst, bb) in ((C_KS, ksT, b_ksT), (C_KW, kwT, b_kwT)):
                    self.lin_fm(w_in, c0, 128, hT, hT_b, T, lambda ci, tb, dst=dst: dst[:, tb * 512:(tb + 1) * 512], bb)
                for (c0, dst, bb) in ((C_KC, kcT, b_kcT), (C_VC, vcT, b_vcT)):
                    self.ms(dst[:], 0.0, [bb])
                    self.lin_fm(w_in, c0, 128, hT, hT_b, T,
                                lambda ci, tb, dst=dst: [(slice(0, 64), dst[0:64, 0, tb * 512:(tb + 1) * 512]),
                                                         (slice(64, 128), dst[64:128, 1, tb * 512:(tb + 1) * 512])], bb)
                self.ms(ksr[:], 0.0, [b_ks])
                self.ms(kwr[:], 0.0, [b_kw])
                self.ms(qrT[64:128, :, :], 0.0, [b_qr])
                for g in range(2):
                    self.dma(ksr[64:96, g, :], self.c["exrows"], [b_ks], [b_ks])
                self.ms(kcmpT[:], 0.0, [b_kc])
                self.ms(vs[:, :, :, 64:65], 1.0, [b_vs])
                self.ms(vw[:, :, :, 64:65], 1.0, [b_vw])
                self.lin_tm(w_in, C_VS, 128, hT, hT_b, NT, lambda j, cb, n: vs[:, j, :, 0:64], b_vs)
                self.lin_tm(w_in, C_VW, 128, hT, hT_b, NT, lambda j, cb, n: vw[:, j, :, 0:64], b_vw)
                self.lin_tm(w_in, C_NG, 24, hT, hT_b, NT, lambda j, cb, n: gates[:, j, :], b_g, func=AF.Sigmoid)
                sinT = self.sb(sa, "ns_sin", [128, T], BF16)
                cosT = self.sb(sa, "ns_cos", [128, T], BF16)
                invf = self.sb(sa, "ns_invf", [128, 1], F32)
                sx1 = contextlib.ExitStack()
                posi = self.sb(sx1, "ns_posi", [128, 512], I32)
                ang = self.sb(sx1, "ns_ang", [128, 512], F32)
                b_pos, b_ang, b_sin, b_cos, b_inv = Buf(), Buf(), Buf(), Buf(), Buf()
                self.dma(invf[:], self.c["invf"], [], [b_inv])
                C1 = 6.28125
                C2 = 2 * math.pi - C1
                tr_r = self.sb(sx1, "ns_trr", [128, 512], F32)
                tr_k = self.sb(sx1, "ns_trk", [128, 512], I32)
                tr_a = self.sb(sx1, "ns_tra", [128, 512], F32)
                tr_u = self.sb(sx1, "ns_tru", [128, 512], F32)
                tr_m = self.sb(sx1, "ns_trm", [128, 512], F32)
                b_tr = Buf()
                for tb in range(4):
                    sl = slice(tb * 512, (tb + 1) * 512)
                    self.dma(posi[:], self.pos_in[:, sl].partition_broadcast(128), [], [b_pos])
                    self.cp(ang[:], posi[:], [b_pos], [b_ang])
                    self.ts(ang[:], ang[:], invf[:, 0:1], None, ALU.mult, None, [b_ang, b_inv], [b_ang])
                    for (dstT, shift, bd) in ((sinT, 0.0, b_sin), (cosT, 0.5 * math.pi, b_cos)):
                        self.ts(tr_a[:], ang[:], shift, None, ALU.add, None, [b_ang], [b_tr])
                        self.ts(tr_r[:], tr_a[:], 1.0 / (2 * math.pi), None, ALU.mult, None, [b_tr], [b_tr])
                        self.cp(tr_k[:], tr_r[:], [b_tr], [b_tr])
                        self.cp(tr_r[:], tr_k[:], [b_tr], [b_tr])
                        self.stt(tr_u[:], tr_r[:], -C1, tr_a[:], ALU.mult, ALU.add, [b_tr], [b_tr])
                        self.stt(tr_u[:], tr_r[:], -C2, tr_u[:], ALU.mult, ALU.add, [b_tr], [b_tr])
                        self.ts(tr_m[:], tr_u[:], math.pi, None, ALU.is_gt, None, [b_tr], [b_tr])
                        self.stt(tr_u[:], tr_m[:], -2 * math.pi, tr_u[:], ALU.mult, ALU.add, [b_tr], [b_tr])
                        self.ts(tr_u[:], tr_u[:], -math.pi, math.pi, ALU.max, ALU.min, [b_tr], [b_tr])
                        self.act(dstT[:, sl], tr_u[:], AF.Sin, [b_tr], [bd])
                S.emit()
                sx1.close()
                sx2 = contextlib.ExitStack()
                t1 = [self.sb(sx2, "ns_t1%d" % i, [64, 512], F32) for i in range(2)]
                t2 = [self.sb(sx2, "ns_t2%d" % i, [64, 512], F32) for i in range(2)]
                bt1 = [Buf() for _ in range(2)]
                bt2 = [Buf() for _ in range(2)]
                t3 = [self.sb(sx2, "ns_t3%d" % i, [64, 512], F32) for i in range(2)]
                t4 = [self.sb(sx2, "ns_t4%d" % i, [64, 512], F32) for i in range(2)]
                bt3 = [Buf() for _ in range(2)]
                bt4 = [Buf() for _ in range(2)]
                rn = 0
                order = [0, 4, 1, 5, 2, 6, 3, 7]
                jobs = [(qT[:, c, :], (lambda sl, c=c: qrT[0:64, order[2 * c], sl]), (lambda sl, c=c: qrT[0:64, order[2 * c + 1], sl]), b_q, b_qr)
                        for c in range(4)]
                jobs.append((ksT[:], (lambda sl: ksr[0:64, 0, sl]), (lambda sl: ksr[0:64, 1, sl]), b_ksT, b_ks))
                jobs.append((kwT[:], (lambda sl: kwr[0:64, 0, sl]), (lambda sl: kwr[0:64, 1, sl]), b_kwT, b_kw))
                for (src, dsta, dstb, bs, bd) in jobs:
                    for tb in range(4):
                        k = rn % 2
                        rn += 1
                        sl = slice(tb * 512, (tb + 1) * 512)
                        self.mm(self.ps[k][0:64, :], self.cst["rotT"][:, 0:64], src[:, sl], True, True, [bs, self.b_const], [self.pb[k]])
                        self.tt(t1[k][0:64, :], self.ps[k][0:64, :], sinT[0:64, sl], ALU.mult, [self.pb[k], b_sin], [bt1[k]])
                        self.tt(t2[k][0:64, :], src[0:64, sl], cosT[0:64, sl], ALU.mult, [bs, b_cos], [bt2[k]], eng="pool")
                        self.tt(dsta(sl), t1[k][0:64, :], t2[k][0:64, :], ALU.add, [bt1[k], bt2[k]], [bd])
                        self.mm(self.ps[2 + k][0:64, :], rotb[:, 0, :], src[:, sl], True, True, [bs, b_tri4], [self.pb[2 + k]])
                        self.mm(self.ps[4 + k][0:64, :], rotb[:, 1, :], src[:, sl], True, True, [bs, b_tri4], [self.pb[4 + k]])
                        self.tt(t3[k][0:64, :], self.ps[2 + k][0:64, :], sinT[0:64, sl], ALU.mult, [self.pb[2 + k], b_sin], [bt3[k]])
                        self.tt(t4[k][0:64, :], self.ps[4 + k][0:64, :], cosT[0:64, sl], ALU.mult, [self.pb[4 + k], b_cos], [bt4[k]])
                        self.tt(dstb(sl), t3[k][0:64, :], t4[k][0:64, :], ALU.add, [bt3[k], bt4[k]], [bd])
                S.emit()
                sx2.close()
                ov_t = self.sb(sa, "ns_ov", [127, 32], BF16)
                b_ov = Buf()
                self.dma(ov_t[:], self.c["ov"], [], [b_ov])
                for g in range(2):
                    self.ms(vcmp[:, g, 64:65], 1.0, [b_vc])
                    self.cp(vcmp[:, g, 65:97], ov_t[:], [b_ov], [b_vc], eng="pool")
                w1 = self.sb(sa, "ns_w1", [128, 32, 256], BF16)
                w2 = self.sb(sa, "ns_w2", [128, 2, 128], BF16)
                pe2 = self.sb(sa, "ns_pe2", [32, 128], F32)
                peT = self.sb(sa, "ns_peT", [128, 32], BF16)
                b1 = self.sb(sa, "ns_b1", [128, 2], F32)
                biasT = self.sb(sa, "ns_biasT", [128, 2], F32)
                hidT = self.sb(sa, "ns_hidT", [128, 2, 127], BF16)
                identf = self.sb(sa, "ns_idf", [128, 128], F32)
                b_w1, b_w2, b_pe, b_peT, b_b1, b_bias, b_hid, b_idf = (Buf() for _ in range(8))
                self.dma(identf[:], self.c["identf"], [], [b_idf])
                for which, srcT, b_src in (("k", kcT, b_kcT), ("v", vcT, b_vcT)):
                    w1d = self.w["cmp_w1_" + which][l]
                    for half in range(2):
                        for l0 in range(0, 32, 8):
                            src = w1d[l0 * 64:(l0 + 8) * 64, :]
                            self.wload(src, 0, 8, 0, 256, dst=w1[half * 64:(half + 1) * 64, l0:l0 + 8, :], dst_b=b_w1, prows=64)
                    w2d = self.w["cmp_w2_" + which][l]
                    for dup in range(2):
                        self.wload(w2d, 0, 2, 0, 64, dst=w2[:, :, dup * 64:(dup + 1) * 64], dst_b=b_w2)
                    for dup in range(2):
                        self.dma(pe2[:, dup * 64:(dup + 1) * 64], self.w["cmp_pe_" + which][l], [], [b_pe])
                    self.tr(self.ps[2][:, 0:32], pe2[:, :], identf[0:32, 0:32], [b_pe, b_idf], [self.pb[2]])
                    self.act(peT[:], self.ps[2][:, 0:32], AF.Identity, [self.pb[2]], [b_peT])
                    self.dma(b1[:], self.w["cmp_b1_" + which][l:l + 1, :].rearrange("o (c p) -> p (o c)", p=128), [], [b_b1], nc_ok=True)
                    for hc in range(2):
                        for ll in range(32):
                            self.mm(self.ps[3][:, hc:hc + 1], w1[0:64, ll, hc * 128:(hc + 1) * 128], peT[0:64, ll:ll + 1],
                                    ll == 0, ll == 31, [b_w1, b_peT], [self.pb[3]])
                    self.tt(biasT[:], self.ps[3][:, 0:2], b1[:], ALU.add, [self.pb[3], b_b1], [b_bias])
                    for g in range(2):
                        base = 64 * g
                        for hc in range(2):
                            bk = hc
                            for ll in range(32):
                                self.mm(self.ps[bk][:, 0:127], w1[:, ll, hc * 128:(hc + 1) * 128],
                                        srcT[:, g, ll:ll + 16 * 126 + 1:16], ll == 0, ll == 31,
                                        [b_w1, b_src], [self.pb[bk]])
                            self.act(hidT[:, hc, :], self.ps[bk][:, 0:127], AF.Silu, [self.pb[bk], b_bias], [b_hid],
                                     bias=biasT[:, hc:hc + 1])
                        if which == "k":
                            for hc in range(2):
                                self.mm(self.ps[2][:, 0:127], w2[:, hc, :], hidT[:, hc, :], hc == 0, hc == 1,
                                        [b_w2, b_hid], [self.pb[2]])
                            self.act(kcmpT[base:base + 64, g, :], self.ps[2][base:base + 64, 0:127], AF.Identity,
                                     [self.pb[2]], [b_kc])
                        else:
                            for hc in range(2):
                                self.mm(self.ps[2][0:127, 0:64], hidT[:, hc, :], w2[:, hc, 0:64], hc == 0, hc == 1,
                                        [b_w2, b_hid], [self.pb[2]])
                            self.act(vcmp[:, g, 0:64], self.ps[2][0:127, 0:64], AF.Identity, [self.pb[2]], [b_vc])
                S.emit()
            cmpb = self.sb(st, "ns_cmpb", [127, T], BF16)
            vm = self.sb(st, "ns_vm", [128, 16, 32], F32)
            addc = self.sb(st, "ns_addc", [128, 16, 32], F32)
            b_k2 = Buf()
            self.dma(cmpb[:], self.c["cmpbias"], [], [b_k2])
            self.dma(vm[:], self.c["vm"], [], [b_k2])
            self.dma(addc[:], self.c["addc"], [], [b_k2])
            P_t = [self.sb(st, "ns_P%d" % i, [128, 512], BF16) for i in range(2)]
            bP = [Buf() for _ in range(2)]
            acc = [self.sb(st, "ns_acc%d" % i, [128, 8, 64], F32) for i in range(2)]
            b_acc = [Buf() for _ in range(2)]
            o_t = [self.sb(st, "ns_o%d" % i, [128, 512], BF16) for i in range(2)]
            b_o = [Buf() for _ in range(2)]
            b_rd_init = Buf()
            rd = self.sb(st, "ns_rd", [128, 4], F32)
            sc = self.sb(st, "ns_sc", [128, 4], F32)
            tmp = self.sb(st, "ns_tmp", [128, 4, 64], F32)
            tslc = self.sb(st, "ns_tslc", [128, 4, 32], F32)
            score = self.sb(st, "ns_score", [128, 32], F32)
            m8 = self.sb(st, "ns_m8", [128, 8], F32)
            selb = self.sb(st, "ns_selb", [128, 96], BF16)
            self.ms(selb[:], 0.0, [b_rd_init])
            b_rd, b_sc, b_tmp, b_tslc, b_score, b_m8, b_selb, b_selbT = (Buf() for _ in range(8))
            b_selrows = {}
            step = 0

            pacc = self.sb(st, "ns_pacc", [128, 388], F32)
            b_pacc = Buf()

            def finish(src_ap, src_bufs, i, g, gi, first):
                a = acc[i % 2]
                self.ts(rd[:], src_ap[:, :, 64], 1e-30, None, ALU.max, None, src_bufs, [b_rd])
                self.rcp(rd[:], rd[:], [b_rd], [b_rd])
                gv = gates[:, i, :].rearrange("p (h t) -> p h t", t=3)[:, 4 * g:4 * g + 4, gi]
                self.tt(sc[:], rd[:], gv, ALU.mult, [b_rd, b_g], [b_sc])
                if first:
                    self.tt(a[:, 4 * g:4 * g + 4, :], src_ap[:, :, 0:64], sc[:].unsqueeze(2).to_broadcast([128, 4, 64]), ALU.mult,
                            src_bufs + [b_sc], [b_acc[i % 2]])
                else:
                    self.tt(tmp[:], src_ap[:, :, 0:64], sc[:].unsqueeze(2).to_broadcast([128, 4, 64]), ALU.mult,
                            src_bufs + [b_sc], [b_tmp])
                    self.tt(a[:, 4 * g:4 * g + 4, :], a[:, 4 * g:4 * g + 4, :], tmp[:], ALU.add, [b_tmp, b_acc[i % 2]],
                            [b_acc[i % 2]])

            for i in range(NT):
                qsl = slice(i * 128, (i + 1) * 128)
                for g in range(2):
                    s2 = step % 2
                    step += 1
                    regions = [[(kcmpT[:, g, :], qT[:, r, qsl], [b_kc, b_q]),
                                (self.identb[0:127, 0:127], cmpb[:, qsl], [self.b_const, b_k2])] for r in range(4)]
                    vl = [(vcmp[:, g, :], [b_vc]) for r in range(4)]
                    self.attn_step(s2, regions, 127, P_t[s2], bP[s2], 2, 97, vl, None, None)
                    pv3 = self.ps[2][:, 0:388].rearrange("p (h c) -> p h c", h=4)
                    finish(pv3, [self.pb[2]], i, g, 0, True)
                    self.tt(tslc[:], pv3[:, :, 65:97], rd[:].unsqueeze(2).to_broadcast([128, 4, 32]), ALU.mult,
                            [self.pb[2], b_rd], [b_tslc])
                    S.add("dve", lambda e: e.tensor_reduce(out=score[:], in_=tslc[:].rearrange("p r n -> p n r"),
                                                           axis=AX.X, op=ALU.add), [b_tslc], [b_score])
                    self.tt(score[:], score[:], vm[:, i, :], ALU.mult, [b_score, b_k2], [b_score])
                    self.tt(score[:], score[:], addc[:, i, :], ALU.add, [b_score, b_k2], [b_score])
                    S.add("dve", lambda e: e.max(out=m8[:], in_=score[:]), [b_score], [b_m8])
                    self.ts(score[:], score[:], m8[:, 7:8], None, ALU.is_ge, None, [b_score, b_m8], [b_score])
                    self.ts(selb[:, 64:96], score[:], -1.0, None, ALU.add, None, [b_score, b_rd_init], [b_selb])
                    pT = self.ps[3][:].bitcast(BF16)
                    self.tr(pT[0:96, 0:128], selb[:, :], self.identb[:], [b_selb, self.b_const], [self.pb[3]])
                    bsr = b_selrows.setdefault((i % 2, g), Buf())
                    self.act(qrT[64:96, 4 * g:4 * g + 4, qsl], pT[64:96, 0:128].unsqueeze(1).to_broadcast([32, 4, 128]), AF.Identity,
                             [self.pb[3]], [bsr])
                    for (kind, kbs, kT_, v_, gi, bk_, bv_) in (
                            ("sel", list(range(i, -1, -1)), ksr, vs, 1, b_ks, b_vs),
                            ("win", list(range(i, max(0, i - 4) - 1, -1)), kwr, vw, 2, b_kw, b_vw)):
                        self.ms(pacc[:, 0:260], 0.0, [b_pacc])
                        for kb in kbs:
                            s2 = step % 2
                            step += 1
                            ksl = slice(kb * 128, (kb + 1) * 128)
                            mms = [(kT_[:, g, ksl], qrT[:, 4 * g:4 * g + 4, qsl], [bk_, b_qr, bsr])]
                            if kb == i:
                                mms.append((self.identb[:], tri4[:, 0, :], [self.b_const, b_tri4]))
                            if kind == "win" and kb == i - 4:
                                mms.append((self.identb[:], tri4[:, 1, :], [self.b_const, b_tri4]))
                            regions = [(slice(0, 512), mms)]
                            vl = [(v_[:, kb, g, :], [bv_]) for r in range(4)]
                            self.attn_step(s2, regions, 128, P_t[s2], bP[s2], 4 + s2, 65, vl, pacc, b_pacc)
                        finish(pacc[:, 0:260].rearrange("p (h c) -> p h c", h=4), [b_pacc], i, g, gi, False)
                k = i % 2
                self.cp(o_t[k][:], acc[k][:].rearrange("p h c -> p (h c)"), [b_acc[k]], [b_o[k]], eng="pool")
                self.dma(self.o_scr[1, i * 128:(i + 1) * 128, :], o_t[k][:], [b_o[k]], [self.B_oscr])
            S.emit()

    def merge(self, l, hT, hT_b):
        S = self.S
        with contextlib.ExitStack() as st:
            wm = self.sb(st, "mg_wm", [128, 8, 3072], BF16)
            wup = self.sb(st, "mg_wup", [128, 3, 4, D], BF16)
            wout = self.sb(st, "mg_wout", [128, 8, D], BF16)
            b_wm, b_wup, b_wout = Buf(), Buf(), Buf()
            for cb in range(0, 3072, 256):
                self.wload(self.w["w_in"][l], 0, 8, C_MG + cb, 256, dst=wm[:, :, cb:cb + 256], dst_b=b_wm)
            for b, nm in enumerate(("w_up_sb", "w_up_nsa", "w_up_fox")):
                for cb in range(0, D, 512):
                    self.wload(self.w[nm][l], 0, 4, cb, 512, dst=wup[:, b, :, cb:cb + 512], dst_b=b_wup)
            for cb in range(0, D, 256):
                self.wload(self.w["w_out"][l], 0, 8, cb, 256, dst=wout[:, :, cb:cb + 256], dst_b=b_wout)
            gt = self.sb(st, "mg_g", [128, D], F32)
            b_gt = Buf()
            self.dma(gt[:], self.w["g_post_mix"][l:l + 1, :].partition_broadcast(128), [], [b_gt])
            post = self.post_tiles(st)
            o_in = [self.sb(st, "mg_o%d" % i, [128, 3, 512], BF16) for i in range(2)]
            oT = [self.sb(st, "mg_oT%d" % i, [128, 12, 128], BF16) for i in range(2)]
            sg = self.sb(st, "mg_sg", [128, 512], F32)
            y = self.sb(st, "mg_y", [128, D], F32)
            ytmp = self.sb(st, "mg_yt", [128, 512], F32)
            yb = self.sb(st, "mg_yb", [128, D], BF16)
            yT = self.sb(st, "mg_yT", [128, 8, 128], BF16)
            b_oin = [Buf() for _ in range(2)]
            b_oT = [Buf() for _ in range(2)]
            b_sg, b_y, b_ytmp, b_yb, b_yT = (Buf() for _ in range(5))
            for j in range(NT):
                k = j % 2
                self.dma(o_in[k][:], self.o_scr[:, j * 128:(j + 1) * 128, :].rearrange("b p n -> p b n"), [self.B_oscr], [b_oin[k]])
                for half in range(2):
                    bk = 4 + half
                    pT = self.ps[bk][:].bitcast(BF16)
                    for c in range(6):
                        cc = half * 6 + c
                        self.tr(pT[:, c * 128:(c + 1) * 128], o_in[k][:, cc // 4, (cc % 4) * 128:(cc % 4 + 1) * 128],
                                self.identb[:], [b_oin[k], self.b_const], [self.pb[bk]])
                    self.cp(oT[k][:, half * 6:(half + 1) * 6, :], pT[:, 0:768].rearrange("p (c n) -> p c n", c=6),
                            [self.pb[bk]], [b_oT[k]])
                for cb in range(2):
                    csl = slice(cb * 512, (cb + 1) * 512)
                    for b in range(3):
                        ub, gb = 0 + (b % 2), 2 + (b % 2)
                        for kc in range(4):
                            self.mm(self.ps[ub][:, :], oT[k][:, 4 * b + kc, :], wup[:, b, kc, csl], kc == 0, kc == 3,
                                    [b_oT[k], b_wup], [self.pb[ub]])
                        for kc in range(8):
                            self.mm(self.ps[gb][:, :], hT[:, kc, j * 128:(j + 1) * 128],
                                    wm[:, kc, b * 1024 + cb * 512:b * 1024 + (cb + 1) * 512], kc == 0, kc == 7,
                                    [hT_b, b_wm], [self.pb[gb]])
                        self.act(sg[:], self.ps[gb][:, :], AF.Sigmoid, [self.pb[gb]], [b_sg])
                        if b == 0:
                            self.tt(y[:, csl], self.ps[ub][:, :], sg[:], ALU.mult, [self.pb[ub], b_sg], [b_y])
                        else:
                            self.tt(ytmp[:], self.ps[ub][:, :], sg[:], ALU.mult, [self.pb[ub], b_sg], [b_ytmp])
                            self.tt(y[:, csl], y[:, csl], ytmp[:], ALU.add, [b_y, b_ytmp], [b_y])
                self.cp(yb[:], y[:], [b_y], [b_yb], eng="pool")
                pT = self.ps[6][:].bitcast(BF16)
                for c in range(8):
                    self.tr(pT[:, c * 128:(c + 1) * 128], yb[:, c * 128:(c + 1) * 128], self.identb[:],
                            [b_yb, self.b_const], [self.pb[6]])
                self.cp(yT[:], pT[:, 0:1024].rearrange("p (c n) -> p c n", c=8), [self.pb[6]], [b_yT])
                ob = (7, 4 + (j % 2)) if False else (7, 6)
                for h2 in range(2):
                    bk = ob[h2]
                    for kc in range(8):
                        self.mm(self.ps[bk][:, :], yT[:, kc, :], wout[:, kc, h2 * 512:(h2 + 1) * 512], kc == 0, kc == 7,
                                [b_yT, b_wout], [self.pb[bk]])
                self.post_norm_add(j, ob, gt, b_gt, post)
            S.emit()

    def mem_attn(self, l, hT, hT_b):
        S = self.S
        with contextlib.ExitStack() as st:
            with contextlib.ExitStack() as s1:
                self.norm_to_hT(self.out, NT, "g_pre_mem", l, hT, hT_b, s1)
                S.emit()
            memT = self.sb(st, "mm_memT", [128, 8, 256], BF16)
            b_memT = Buf()
            with contextlib.ExitStack() as s1:
                self.norm_to_hT(self.mem_in, 2, "g_mem", l, memT, b_memT, s1)
                S.emit()
            qT = self.sb(st, "mm_qT", [128, 2, T], BF16)
            kT = self.sb(st, "mm_kT", [128, 4, 256], BF16)
            v = self.sb(st, "mm_v", [128, 2, 4, 65], BF16)
            wo = self.sb(st, "mm_wo", [128, 2, D], BF16)
            b_q, b_k, b_v, b_wo = Buf(), Buf(), Buf(), Buf()
            self.ms(v[:, :, :, 64:65], 1.0, [b_v])
            self.lin_fm(self.w["w_mem_q"][l], 0, 256, hT, hT_b, T, lambda ci, tb: qT[:, ci, tb * 512:(tb + 1) * 512], b_q)
            self.ms(kT[:], 0.0, [b_k])
            self.lin_fm(self.w["w_mem_k"][l], 0, 256, memT, b_memT, 256,
                        lambda ci, tb: [(slice(0, 64), kT[0:64, 2 * ci, :]), (slice(64, 128), kT[64:128, 2 * ci + 1, :])], b_k)
            self.lin_tm(self.w["w_mem_v"][l], 0, 256, memT, b_memT, 2, lambda j, cb, n: v[:, j, :, 0:64], b_v)
            for cb in range(0, D, 512):
                self.wload(self.w["w_mem_o"][l], 0, 2, cb, 512, dst=wo[:, :, cb:cb + 512], dst_b=b_wo)
            gt = self.sb(st, "mm_g", [128, D], F32)
            b_gt = Buf()
            self.dma(gt[:], self.w["g_post_mem"][l:l + 1, :].partition_broadcast(128), [], [b_gt])
            post = self.post_tiles(st)
            P_t = [self.sb(st, "mm_P%d" % i, [128, 512], BF16) for i in range(2)]
            bP = [Buf() for _ in range(2)]
            rden = self.sb(st, "mm_rd", [128, 4], F32)
            o_t = self.sb(st, "mm_o", [128, 4, 64], BF16)
            oT = self.sb(st, "mm_oT", [128, 2, 128], BF16)
            b_o, b_oT = Buf(), Buf()
            macc = self.sb(st, "mm_acc", [128, 260], F32)
            b_macc = Buf()
            step = 0
            for i in range(NT):
                qsl = slice(i * 128, (i + 1) * 128)
                self.ms(macc[:], 0.0, [b_macc])
                for kb in range(2):
                    s2 = step % 2
                    step += 1
                    ksl = slice(kb * 128, (kb + 1) * 128)
                    regions = [[(kT[:, hh, ksl], qT[:, hh // 2, qsl], [b_q, b_k])] for hh in range(4)]
                    vl = [(v[:, kb, hh, :], [b_v]) for hh in range(4)]
                    self.attn_step(s2, regions, 128, P_t[s2], bP[s2], 2 + s2, 65, vl, macc, b_macc)
                pv3 = macc[:, 0:260].rearrange("p (h c) -> p h c", h=4)
                self.rcp(rden[:], pv3[:, :, 64], [b_macc], [b_o])
                self.tt(o_t[:], pv3[:, :, 0:64], rden[:].unsqueeze(2).to_broadcast([128, 4, 64]), ALU.mult,
                        [b_macc, b_o], [b_o])
                pT = self.ps[4][:].bitcast(BF16)
                of = o_t[:].rearrange("p h c -> p (h c)")
                for c in range(2):
                    self.tr(pT[:, c * 128:(c + 1) * 128], of[:, c * 128:(c + 1) * 128], self.identb[:],
                            [b_o, self.b_const], [self.pb[4]])
                self.cp(oT[:], pT[:, 0:256].rearrange("p (c n) -> p c n", c=2), [self.pb[4]], [b_oT])
                ob = (6, 7)
                for h2 in range(2):
                    for kc in range(2):
                        self.mm(self.ps[ob[h2]][:, :], oT[:, kc, :], wo[:, kc, h2 * 512:(h2 + 1) * 512], kc == 0, kc == 1,
                                [b_oT, b_wo], [self.pb[ob[h2]]])
                self.post_norm_add(i, ob, gt, b_gt, post)
            S.emit()

    def ffn(self, l, hT, hT_b):
        S = self.S
        with contextlib.ExitStack() as st:
            with contextlib.ExitStack() as s1:
                self.norm_to_hT(self.out, NT, "g_pre_ffn", l, hT, hT_b, s1)
                S.emit()
            NK = D_FF // 128
            wd = self.sb(st, "ff_wd", [128, NK, D], BF16)
            b_wd = Buf()
            for k0 in range(0, NK, 8):
                kn = min(8, NK - k0)
                for cb in range(0, D, 256):
                    self.wload(self.w["w_ffn_down"][l], k0 * 128, kn, cb, 256, dst=wd[:, k0:k0 + kn, cb:cb + 256], dst_b=b_wd)
            gt = self.sb(st, "ff_g", [128, D], F32)
            b_gt = Buf()
            self.dma(gt[:], self.w["g_post_ffn"][l:l + 1, :].partition_broadcast(128), [], [b_gt])
            post = self.post_tiles(st)
            aT = self.sb(st, "ff_aT", [128, NK, 1024], BF16)
            sg = [self.sb(st, "ff_sg%d" % i, [128, 512], F32) for i in range(2)]
            b_sg = [Buf() for _ in range(2)]
            for half in range(2):
                b_aT = Buf()
                t0 = half * 1024
                cnt = 0
                for c3 in range(0, NK, 2):
                    nch = min(2, NK - c3)
                    wg, wg_b = self.wload(self.w["w_ffn_gate"][l], 0, 8, c3 * 128, nch * 128)
                    wu, wu_b = self.wload(self.w["w_ffn_up"][l], 0, 8, c3 * 128, nch * 128)
                    for cc in range(nch):
                        for tb in range(2):
                            k = cnt % 2
                            cnt += 1
                            gb, ub = k, 2 + k
                            tsl = slice(t0 + tb * 512, t0 + (tb + 1) * 512)
                            for kc in range(8):
                                self.mm(self.ps[gb][:, :], wg[:, kc, cc * 128:(cc + 1) * 128], hT[:, kc, tsl], kc == 0, kc == 7,
                                        [wg_b, hT_b], [self.pb[gb]])
                            for kc in range(8):
                                self.mm(self.ps[ub][:, :], wu[:, kc, cc * 128:(cc + 1) * 128], hT[:, kc, tsl], kc == 0, kc == 7,
                                        [wu_b, hT_b], [self.pb[ub]])
                            self.act(sg[k][:], self.ps[gb][:, :], AF.Silu, [self.pb[gb]], [b_sg[k]])
                            self.tt(aT[:, c3 + cc, tb * 512:(tb + 1) * 512], self.ps[ub][:, :], sg[k][:], ALU.mult,
                                    [self.pb[ub], b_sg[k]], [b_aT])
                for jj in range(8):
                    j = half * 8 + jj
                    ob = (4 + 2 * (jj % 2), 5 + 2 * (jj % 2))
                    for h2 in range(2):
                        for kc in range(NK):
                            self.mm(self.ps[ob[h2]][:, :], aT[:, kc, jj * 128:(jj + 1) * 128], wd[:, kc, h2 * 512:(h2 + 1) * 512],
                                    kc == 0, kc == NK - 1, [b_aT, b_wd], [self.pb[ob[h2]]])
                    self.post_norm_add(j, ob, gt, b_gt, post)
            S.emit()


_CACHE = {}


def _perm_w_in(w_in):
    w = np.array(w_in, copy=True)
    order = [0, 4, 1, 5, 2, 6, 3, 7]
    src = w_in[:, :, C_NQ:C_NQ + 512].reshape(w_in.shape[0], w_in.shape[1], 8, 64)
    w[:, :, C_NQ:C_NQ + 512] = src[:, :, order, :].reshape(w_in.shape[0], w_in.shape[1], 512)
    return w


def kernel(**inputs):
    depth = DEBUG_LAYERS or DEPTH
    if "prog" not in _CACHE:
        _CACHE["prog"] = Prog(depth)
    prog = _CACHE["prog"]
    cs = _consts()
    base = {("c_" + k): v for k, v in cs.items()}
    for k in W_SHAPES:
        a = np.ascontiguousarray(np.asarray(inputs[k], dtype=np.float32))
        if k == "w_in":
            a = _perm_w_in(a)
        base[k] = a
    x = np.asarray(inputs["x"], dtype=np.float32)
    mem = np.asarray(inputs["mem"], dtype=np.float32)
    pos = np.asarray(inputs["positions"], dtype=np.int32)
    in_maps = []
    for b in range(8):
        m = dict(base)
        m["x"] = np.ascontiguousarray(x[b])
        m["mem"] = np.ascontiguousarray(mem[b])
        m["positions"] = np.ascontiguousarray(pos[b:b + 1])
        in_maps.append(m)
    res = run_bass_kernel_spmd(prog.nc, in_maps, core_ids=list(range(8)))
    return np.stack([np.asarray(r["out"], dtype=np.float32) for r in res.results], axis=0)
```

```python
import contextlib
import math
import numpy as np
import ml_dtypes
import concourse.bass as bass
import concourse.mybir as mybir
from concourse.bass_utils import run_bass_kernel_spmd

F32 = mybir.dt.float32
BF16 = mybir.dt.bfloat16
I32 = mybir.dt.int32
AF = mybir.ActivationFunctionType
ALU = mybir.AluOpType
AX = mybir.AxisListType

ENGS = ("pe", "act", "dve", "pool", "sp")
DMA_RING = 6
T = 2048
D = 1024
NT = 16
DEPTH = 2
D_IN = 7456
D_FF = 2816
BIG = 30000.0
WMAX = 2048
DEBUG_LAYERS = None


class Buf:
    __slots__ = ("name", "w", "r")

    def __init__(self, name=""):
        self.name = name
        self.w = None
        self.r = []


class Pipe:
    def __init__(self):
        self.q = []

    def push(self, stages):
        self.q.append(stages)
        n = len(self.q) - 1
        for k in range(3):
            idx = n - k
            if idx >= 0 and k < len(self.q[idx]):
                self.q[idx][k]()

    def flush(self):
        n = len(self.q)
        for extra in range(1, 3):
            for k in range(extra, 3):
                idx = n - 1 - (k - extra)
                if idx >= 0 and k < len(self.q[idx]):
                    self.q[idx][k]()
        self.q = []


class Sched:
    def __init__(self, nc, stack):
        self.nc = nc
        self.sems = {e: stack.enter_context(nc.semaphore("s_" + e)) for e in ENGS}
        self.ring = {q: [stack.enter_context(nc.semaphore("r_%s%d" % (q, i))) for i in range(DMA_RING)]
                     for q in ("sp",)}
        self.cnt = {e: 0 for e in ENGS}
        self.ring_n = {q: 0 for q in self.ring}
        self.ring_val = {q: [0] * DMA_RING for q in self.ring}
        self.reset()

    def reset(self):
        self.ops = []
        self.touched = {}

    def add(self, eng, fn, reads=(), writes=(), dma=False):
        import os
        if len(self.ops) >= int(os.environ.get("NOPS", "100000000")):
            return
        deps = set()
        for b in reads:
            if b.w is not None:
                deps.add(b.w)
        for b in writes:
            if b.w is not None:
                deps.add(b.w)
            deps.update(b.r)
        i = len(self.ops)
        self.ops.append(dict(eng=eng, fn=fn, deps=deps, dma=dma, sig=False))
        for b in reads:
            b.r.append(i)
            self.touched[id(b)] = b
        for b in writes:
            b.w = i
            b.r = []
            self.touched[id(b)] = b
        return i

    def emit(self):
        nc = self.nc
        ops = self.ops
        for op in ops:
            for d in op["deps"]:
                od = ops[d]
                if od["dma"] or od["eng"] != op["eng"] or op["eng"] != "pe":
                    od["sig"] = True
        for op in ops:
            e = op["eng"]
            if op["dma"]:
                n = self.ring_n[e]
                self.ring_n[e] += 1
                slot = n % DMA_RING
                prev = self.ring_val[e][slot]
                self.ring_val[e][slot] = prev + 16
                op["sem"] = self.ring[e][slot]
                op["val"] = prev + 16
                op["prev"] = prev
            elif op["sig"]:
                self.cnt[e] += 1
                op["sem"] = self.sems[e]
                op["val"] = self.cnt[e]
        per = {e: [] for e in ENGS}
        for i, op in enumerate(ops):
            per[op["eng"]].append(i)

        def run(e, engobj):
            seen = {}
            for i in per[e]:
                op = ops[i]
                waits = {}
                for d in sorted(op["deps"]):
                    od = ops[d]
                    if not od["dma"] and od["eng"] == e and e == "pe":
                        continue
                    s = od["sem"]
                    k = id(s)
                    if seen.get(k, 0) >= od["val"]:
                        continue
                    if k not in waits or waits[k][1] < od["val"]:
                        waits[k] = (s, od["val"])
                if op["dma"] and op["prev"] > 0:
                    s = op["sem"]
                    k = id(s)
                    if seen.get(k, 0) < op["prev"]:
                        if k not in waits or waits[k][1] < op["prev"]:
                            waits[k] = (s, op["prev"])
                for k, (s, v) in waits.items():
                    engobj.wait_ge(s, v)
                    seen[k] = v
                ins = op["fn"](engobj)
                if op["dma"]:
                    ins.then_inc(op["sem"], 16)
                elif op["sig"]:
                    ins.then_inc(op["sem"], 1)
            if e in self.ring:
                for slot in range(DMA_RING):
                    v = self.ring_val[e][slot]
                    if v > 0 and seen.get(id(self.ring[e][slot]), 0) < v:
                        engobj.wait_ge(self.ring[e][slot], v)

        with nc.Block() as block:
            @block.tensor
            def _(eng):
                run("pe", eng)

            @block.scalar
            def _(eng):
                run("act", eng)

            @block.vector
            def _(eng):
                run("dve", eng)

            @block.gpsimd
            def _(eng):
                run("pool", eng)

            @block.sync
            def _(eng):
                run("sp", eng)
        for b in self.touched.values():
            b.w = None
            b.r = []
        self.reset()


def _consts():
    bf = ml_dtypes.bfloat16
    j = np.arange(128)[:, None]
    t = np.arange(128)[None, :]
    c = {}
    c["identb"] = np.eye(128, dtype=np.float32).astype(bf)
    c["identf"] = np.eye(128, dtype=np.float32)
    c["tri_sb"] = np.where(j >= t, -BIG, 0.0).astype(bf)
    c["tri_c"] = np.where(j > t, -BIG, 0.0).astype(bf)
    c["tri_band"] = np.where(j <= t, -BIG, 0.0).astype(bf)
    c["negut8"] = np.where(j >= t, -8.0, 0.0).astype(bf)
    c["neg8ones"] = np.full((128, 128), -8.0, np.float32).astype(bf)
    rot = np.zeros((128, 128), np.float32)
    for blk in (0, 64):
        for m in range(8):
            rot[blk + m + 8, blk + m] = -1.0
            rot[blk + m, blk + m + 8] = 1.0
    c["rotT"] = rot.astype(bf)
    rot_b = np.zeros((128, 64), np.float32)
    sel_b = np.zeros((128, 64), np.float32)
    for m in range(64):
        sel_b[64 + m, m] = 1.0
    for m in range(8):
        rot_b[64 + m + 8, m] = -1.0
        rot_b[64 + m, m + 8] = 1.0
    c["rot_b"] = rot_b.astype(bf)
    c["sel_b"] = sel_b.astype(bf)
    c["tri_c4"] = np.tile(np.where(j > t, -BIG, 0.0), (1, 4)).astype(bf)
    c["tri_band4"] = np.tile(np.where(j <= t, -BIG, 0.0), (1, 4)).astype(bf)
    exr = np.zeros((32, T), np.float32)
    for key in range(T):
        exr[key // 64, key] = BIG
    c["exrows"] = exr.astype(bf)
    half = 8
    inv = (500000.0 ** (-np.arange(half, dtype=np.float32) / half)).astype(np.float32)
    invf = np.zeros((128, 1), np.float32)
    for p in range(128):
        if p % 64 < 16:
            invf[p, 0] = inv[(p % 64) % 8]
    c["invf"] = invf
    cidx = np.arange(127)[:, None]
    tt_ = np.arange(T)[None, :]
    c["cmpbias"] = np.where(16 * cidx + 31 <= tt_, 0.0, -BIG).astype(bf)
    ex = np.zeros((32, 16, 128), np.float32)
    for kb in range(16):
        for p in range(128):
            ex[2 * kb + (p >= 64), kb, p] = BIG
    c["expand"] = ex.astype(bf)
    vm = np.zeros((128, 16, 32), np.float32)
    addc = np.zeros((128, 16, 32), np.float32)
    for i in range(16):
        for p in range(128):
            tpos = 128 * i + p
            for n in range(32):
                forced = (n == 0) or (n == tpos // 64)
                valid = 64 * n <= tpos
                if forced:
                    addc[p, i, n] = 1e4
                elif valid:
                    vm[p, i, n] = 1.0
                else:
                    addc[p, i, n] = -1.0
    c["vm"] = vm
    c["addc"] = addc
    cs = np.arange(127)[:, None] * 16
    ss = np.arange(32)[None, :] * 64
    c["ov"] = ((cs < ss + 64) & (cs + 32 > ss)).astype(np.float32).astype(bf)
    return c


CONST_DT = dict(identb=BF16, identf=F32, tri_sb=BF16, tri_c=BF16, tri_band=BF16, negut8=BF16, neg8ones=BF16,
                rotT=BF16, rot_b=BF16, sel_b=BF16, tri_c4=BF16, tri_band4=BF16, exrows=BF16, invf=F32, cmpbias=BF16, expand=BF16, vm=F32, addc=F32, ov=BF16)

W_SHAPES = dict(
    g_pre_mix=[DEPTH, D], g_post_mix=[DEPTH, D], g_pre_mem=[DEPTH, D], g_mem=[DEPTH, D], g_post_mem=[DEPTH, D],
    g_pre_ffn=[DEPTH, D], g_post_ffn=[DEPTH, D], w_in=[DEPTH, D, D_IN], b_fox_f=[DEPTH, 8],
    cmp_pe_k=[DEPTH, 32, 64], cmp_w1_k=[DEPTH, 2048, 256], cmp_b1_k=[DEPTH, 256], cmp_w2_k=[DEPTH, 256, 64],
    cmp_pe_v=[DEPTH, 32, 64], cmp_w1_v=[DEPTH, 2048, 256], cmp_b1_v=[DEPTH, 256], cmp_w2_v=[DEPTH, 256, 64],
    w_up_sb=[DEPTH, 512, D], w_up_nsa=[DEPTH, 512, D], w_up_fox=[DEPTH, 512, D], w_out=[DEPTH, D, D],
    w_mem_q=[DEPTH, D, 256], w_mem_k=[DEPTH, D, 256], w_mem_v=[DEPTH, D, 256], w_mem_o=[DEPTH, 256, D],
    w_ffn_gate=[DEPTH, D, D_FF], w_ffn_up=[DEPTH, D, D_FF], w_ffn_down=[DEPTH, D_FF, D])

C_SBQ, C_SBK, C_SBV = 0, 512, 1024
C_NQ, C_KC, C_VC, C_KS, C_VS, C_KW, C_VW, C_NG = 1536, 2048, 2176, 2304, 2432, 2560, 2688, 2816
C_FQ, C_FK, C_FV, C_FF, C_MG = 2840, 3352, 3864, 4376, 4384


class Prog:
    def __init__(self, depth=DEPTH, dbg=None, stop=None):
        self.depth = depth
        self.dbg = dbg
        self.stop = stop
        nc = self.nc = bass.Bass("TRN2", target_bir_lowering=False)
        self.x_in = nc.dram_tensor("x", [T, D], F32, kind="ExternalInput").ap()
        self.mem_in = nc.dram_tensor("mem", [256, D], F32, kind="ExternalInput").ap()
        self.pos_in = nc.dram_tensor("positions", [1, T], I32, kind="ExternalInput").ap()
        self.w = {k: nc.dram_tensor(k, s, F32, kind="ExternalInput").ap() for k, s in W_SHAPES.items()}
        cs = _consts()
        self.c = {k: nc.dram_tensor("c_" + k, list(v.shape), CONST_DT[k], kind="ExternalInput").ap()
                  for k, v in cs.items()}
        self.out = nc.dram_tensor("out", [T, D], F32, kind="ExternalOutput").ap()
        self.o_scr = nc.dram_tensor("o_scr", [3, T, 512], BF16, kind=("ExternalOutput" if dbg else "Internal")).ap()
        self.row_scr = nc.dram_tensor("row_scr", [8, 4, T], BF16, kind="Internal").ap()
        with contextlib.ExitStack() as st:
            self.S = Sched(nc, st)
            self.ps = [st.enter_context(nc.psum_tensor("ps%d" % i, [128, 512], F32)) for i in range(8)]
            self.pb = [Buf("ps%d" % i) for i in range(8)]
            self.B_x = Buf("x")
            self.B_oscr = Buf("oscr")
            self.st = st
            self.build()

    def mm(self, out, lhsT, rhs, start, stop, R, W):
        self.S.add("pe", lambda e: e.matmul(out, lhsT=lhsT, rhs=rhs, start=start, stop=stop, skip_group_check=True), R, W)

    def tr(self, out, in_, ident, R, W):
        self.S.add("pe", lambda e: e.transpose(out=out, in_=in_, identity=ident), R, W)

    def act(self, out, in_, func, R, W, bias=None, scale=None, accum=None):
        kw = {}
        if bias is not None:
            kw["bias"] = bias
        if scale is not None:
            kw["scale"] = scale
        if accum is not None:
            kw["accum_out"] = accum
        self.S.add("act", lambda e: e.activation(out=out, in_=in_, func=func, **kw), R, W)

    def tt(self, out, in0, in1, op, R, W, eng="dve"):
        self.S.add(eng, lambda e: e.tensor_tensor(out=out, in0=in0, in1=in1, op=op), R, W)

    def ts(self, out, in0, s1, s2, op0, op1, R, W, eng="dve"):
        if op1 is None:
            self.S.add(eng, lambda e: e.tensor_scalar(out=out, in0=in0, scalar1=s1, scalar2=None, op0=op0), R, W)
        else:
            self.S.add(eng, lambda e: e.tensor_scalar(out=out, in0=in0, scalar1=s1, scalar2=s2, op0=op0, op1=op1), R, W)

    def stt(self, out, in0, scalar, in1, op0, op1, R, W):
        self.S.add("dve", lambda e: e.scalar_tensor_tensor(out=out, in0=in0, scalar=scalar, in1=in1, op0=op0, op1=op1), R, W)

    def cp(self, out, in_, R, W, eng="dve"):
        self.S.add(eng, lambda e: e.tensor_copy(out=out, in_=in_), R, W)

    def ms(self, ap, val, W, eng="dve"):
        self.S.add(eng, lambda e: e.memset(ap, val), [], W)

    def rcp(self, out, in_, R, W):
        self.S.add("dve", lambda e: e.reciprocal(out=out, in_=in_), R, W)

    def dma(self, out, in_, R, W, nc_ok=False):
        if nc_ok:
            self.S.add("sp", lambda q: q.dma_start(out=out, in_=in_, allow_slow_non_contiguous=True), R, W, dma=True)
        else:
            self.S.add("sp", lambda q: q.dma_start(out=out, in_=in_), R, W, dma=True)

    def sb(self, st, name, shape, dt):
        self._n = getattr(self, "_n", 0) + 1
        return st.enter_context(self.nc.sbuf_tensor("%s_%d" % (name, self._n), shape, dt))

    def winit(self, st):
        self.wst = [self.sb(st, "wst%d" % i, [128, WMAX], F32) for i in range(2)]
        self.wbf = [self.sb(st, "wbf%d" % i, [128, WMAX], BF16) for i in range(3)]
        self.wst_b = [Buf("wst%d" % i) for i in range(2)]
        self.wbf_b = [Buf("wbf%d" % i) for i in range(3)]
        self.wn = 0

    def wload(self, w2d, r0, kc, c0, n, dst=None, dst_b=None, prows=128):
        assert kc * n <= WMAX
        i = self.wn
        self.wn += 1
        stg, stg_b = self.wst[i % 2], self.wst_b[i % 2]
        src = w2d[r0:r0 + kc * prows, c0:c0 + n].rearrange("(c p) n -> p c n", p=prows)
        sview = stg[0:prows, 0:kc * n].rearrange("p (c n) -> p c n", c=kc)
        self.dma(sview, src, [], [stg_b])
        if dst is None:
            j = i % 3
            dst = self.wbf[j][0:prows, 0:kc * n].rearrange("p (c n) -> p c n", c=kc)
            dst_b = self.wbf_b[j]
        self.cp(dst, sview, [stg_b], [dst_b], eng="pool")
        return dst, dst_b

    def lin_fm(self, w2d, c0, ncols, hT, hT_b, ntok, out_fn, out_b, chunk=128, func=AF.Identity, bias_fn=None,
               banks=(6, 7), bias_b=None):
        tbw = min(512, ntok)
        ntb = ntok // tbw
        per = max(chunk, (WMAX // 8) // chunk * chunk)
        cnt = 0
        for g0 in range(0, ncols, per):
            gn = min(per, ncols - g0)
            wb, wb_b = self.wload(w2d, 0, 8, c0 + g0, gn)
            for cc in range(gn // chunk):
                ci = (g0 // chunk) + cc
                for tb in range(ntb):
                    bk = banks[cnt % len(banks)]
                    cnt += 1
                    for kc in range(8):
                        self.mm(self.ps[bk][0:chunk, 0:tbw], wb[:, kc, cc * chunk:(cc + 1) * chunk],
                                hT[:, kc, tb * tbw:(tb + 1) * tbw], kc == 0, kc == 7, [wb_b, hT_b], [self.pb[bk]])
                    outs = out_fn(ci, tb)
                    if not isinstance(outs, list):
                        outs = [(slice(0, chunk), outs)]
                    for (psl, dst) in outs:
                        self.act(dst, self.ps[bk][psl, 0:tbw], func, [self.pb[bk]] + ([bias_b] if bias_b else []), [out_b],
                                 bias=(bias_fn(ci) if bias_fn else None))

    def lin_tm(self, w2d, c0, ncols, hT, hT_b, ntiles, out_fn, out_b, func=AF.Identity, banks=(6, 7), blk=256):
        cnt = 0
        for cb in range(0, ncols, blk):
            n = min(blk, ncols - cb)
            wb, wb_b = self.wload(w2d, 0, 8, c0 + cb, n)
            for j in range(ntiles):
                bk = banks[cnt % len(banks)]
                cnt += 1
                for kc in range(8):
                    self.mm(self.ps[bk][:, 0:n], hT[:, kc, j * 128:(j + 1) * 128], wb[:, kc, 0:n],
                            kc == 0, kc == 7, [wb_b, hT_b], [self.pb[bk]])
                self.act(out_fn(j, cb, n), self.ps[bk][:, 0:n], func, [self.pb[bk]], [out_b])

    def rstd_from_ss(self, ss, rstd, b_ss, b_rstd, n=D):
        self.ts(rstd, ss, 1.0 / n, 1e-6, ALU.mult, ALU.add, [b_ss], [b_rstd])
        self.act(rstd, rstd, AF.Sqrt, [b_rstd], [b_rstd])
        self.rcp(rstd, rstd, [b_rstd], [b_rstd])

    def norm_to_hT(self, src, ntiles, gname, l, hT, hT_b, st):
        gt = self.sb(st, "n_g", [128, D], F32)
        b_g = Buf("g")
        self.dma(gt[:], self.w[gname][l:l + 1, :].partition_broadcast(128), [], [b_g])
        xs = [self.sb(st, "n_x%d" % i, [128, D], F32) for i in range(2)]
        xb = [Buf() for _ in range(2)]
        sq = self.sb(st, "n_sq", [128, D], F32)
        ssr = [self.sb(st, "n_ss%d" % i, [128, 2], F32) for i in range(2)]
        sb_ = [Buf() for _ in range(2)]
        hb = [self.sb(st, "n_h%d" % i, [128, D], BF16) for i in range(2)]
        hbb = [Buf() for _ in range(2)]
        b_sq = Buf()
        for j in range(ntiles):
            k = j % 2
            self.dma(xs[k][:], src[j * 128:(j + 1) * 128, :], [self.B_x], [xb[k]])
            self.act(sq[:], xs[k][:], AF.Square, [xb[k]], [b_sq, sb_[k]], accum=ssr[k][:, 0:1])
            self.rstd_from_ss(ssr[k][:, 0:1], ssr[k][:, 1:2], sb_[k], sb_[k])
            self.stt(hb[k][:], xs[k][:], ssr[k][:, 1:2], gt[:], ALU.mult, ALU.mult, [xb[k], sb_[k], b_g], [hbb[k]])
            bk = 4 + k
            pT = self.ps[bk][:].bitcast(BF16)
            for c in range(8):
                self.tr(pT[:, c * 128:(c + 1) * 128], hb[k][:, c * 128:(c + 1) * 128], self.identb[:],
                        [hbb[k], self.b_const], [self.pb[bk]])
            self.cp(hT[:, :, j * 128:(j + 1) * 128], pT[:, 0:1024].rearrange("p (c n) -> p c n", c=8),
                    [self.pb[bk]], [hT_b])

    def post_norm_add(self, j, banks, gt, b_g, st_tiles):
        sq, ss, xt, yt, bufs = st_tiles
        k = j % 2
        b_ss, b_x, b_y, b_sq = bufs[k]
        for h in range(2):
            self.act(sq[:, 0:512], self.ps[banks[h]][:, :], AF.Square, [self.pb[banks[h]]], [b_sq, b_ss],
                     accum=ss[k][:, h:h + 1])
        self.tt(ss[k][:, 2:3], ss[k][:, 0:1], ss[k][:, 1:2], ALU.add, [b_ss], [b_ss])
        self.rstd_from_ss(ss[k][:, 2:3], ss[k][:, 3:4], b_ss, b_ss)
        self.dma(xt[k][:], self.out[j * 128:(j + 1) * 128, :], [self.B_x], [b_x])
        for h in range(2):
            self.stt(yt[k][:, h * 512:(h + 1) * 512], self.ps[banks[h]][:, :], ss[k][:, 3:4],
                     gt[:, h * 512:(h + 1) * 512], ALU.mult, ALU.mult, [self.pb[banks[h]], b_ss, b_g], [b_y])
        self.tt(yt[k][:], yt[k][:], xt[k][:], ALU.add, [b_y, b_x], [b_y], eng="pool")
        self.dma(self.out[j * 128:(j + 1) * 128, :], yt[k][:], [b_y], [self.B_x])

    def post_tiles(self, st):
        sq = self.sb(st, "p_sq", [128, 512], F32)
        ss = [self.sb(st, "p_ss%d" % i, [128, 4], F32) for i in range(2)]
        xt = [self.sb(st, "p_x%d" % i, [128, D], F32) for i in range(2)]
        yt = [self.sb(st, "p_y%d" % i, [128, D], F32) for i in range(2)]
        bufs = [(Buf(), Buf(), Buf(), Buf()) for _ in range(2)]
        return (sq, ss, xt, yt, bufs)

    def attn_stages(self, *a, **kw):
        return [lambda: self.attn_step(*a, part="A", **kw), lambda: self.attn_step(*a, part="B", **kw)]

    def attn_step(self, lbank, regions, nk, P, P_b, pvbank, ncol, v_list, acc, acc_b, scale=0.125, part="AB"):
        S = self
        pb = self.pb[lbank]
        for r, mms in enumerate(regions if "A" in part else []):
            cols = slice(r * 128, (r + 1) * 128)
            if isinstance(mms, tuple):
                cols, mms = mms
            for idx, (lhsT, rhs, R) in enumerate(mms):
                o = self.ps[lbank][0:nk, cols]
                if len(rhs.shape) == 3:
                    o = o.rearrange("p (h q) -> p h q", h=rhs.shape[1])
                S.mm(o, lhsT, rhs, idx == 0, idx == len(mms) - 1, R, [pb])
        if "A" in part:
            S.act(P[0:nk, :], self.ps[lbank][0:nk, :], AF.Exp, [pb], [P_b], scale=scale)
        if "B" not in part:
            return
        for r, (rhs, R) in enumerate(v_list):
            S.mm(self.ps[pvbank][:, r * ncol:(r + 1) * ncol], P[0:nk, r * 128:(r + 1) * 128], rhs, True, True,
                 [P_b] + R, [self.pb[pvbank]])
        if acc is not None:
            S.tt(acc[:, 0:4 * ncol], acc[:, 0:4 * ncol], self.ps[pvbank][:, 0:4 * ncol], ALU.add,
                 [acc_b, self.pb[pvbank]], [acc_b])

    def build(self):
        nc = self.nc
        st = self.st
        S = self.S
        self.b_const = Buf("const")
        cst = {}
        for k in ("identb", "tri_sb", "tri_c", "tri_band", "negut8", "neg8ones", "rotT"):
            cst[k] = self.sb(st, "k_" + k, [128, 128], BF16)
            self.dma(cst[k][:], self.c[k], [], [self.b_const])
        self.identb = cst["identb"]
        self.cst = cst
        self.onecol = self.sb(st, "k_one", [128, 1], F32)
        self.ms(self.onecol[:], 1.0, [self.b_const])
        self.negpi = self.sb(st, "k_negpi", [128, 1], F32)
        self.ms(self.negpi[:], -math.pi, [self.b_const])
        with contextlib.ExitStack() as s0:
            xt = [self.sb(s0, "c_x%d" % i, [128, 4, D], F32) for i in range(2)]
            xb = [Buf() for _ in range(2)]
            for j in range(4):
                k = j % 2
                self.dma(xt[k][:], self.x_in[j * 512:(j + 1) * 512, :].rearrange("(c p) n -> p c n", p=128), [], [xb[k]])
                self.dma(self.out[j * 512:(j + 1) * 512, :].rearrange("(c p) n -> p c n", p=128), xt[k][:], [xb[k]], [self.B_x])
            S.emit()
        for l in range(self.depth):
            self.layer(l)

    def layer(self, l):
        S = self.S
        with contextlib.ExitStack() as sl:
            hT = self.sb(sl, "hT", [128, 8, T], BF16)
            hT_b = Buf("hT")
            self.winit(sl)
            with contextlib.ExitStack() as s1:
                self.norm_to_hT(self.out, NT, "g_pre_mix", l, hT, hT_b, s1)
                S.emit()
            for nm, fn in (("sb", self.sb_branch), ("fox", self.fox_branch), ("nsa", self.nsa_branch),
                           ("merge", self.merge), ("mem", self.mem_attn), ("ffn", self.ffn)):
                if self.stop is not None and nm not in self.stop:
                    continue
                fn(l, hT, hT_b)

    def sb_branch(self, l, hT, hT_b):
        S = self.S
        w_in = self.w["w_in"][l]
        with contextlib.ExitStack() as st:
            qT = self.sb(st, "sb_qT", [128, 4, T], BF16)
            kT = self.sb(st, "sb_kT", [128, 8, T], BF16)
            v = self.sb(st, "sb_v", [128, NT, 512], BF16)
            b_q, b_k, b_v = Buf(), Buf(), Buf()
            self.lin_fm(w_in, C_SBQ, 512, hT, hT_b, T, lambda ci, tb: qT[:, ci, tb * 512:(tb + 1) * 512], b_q)
            self.ms(kT[:], 0.0, [b_k])
            self.lin_fm(w_in, C_SBK, 512, hT, hT_b, T,
                        lambda ci, tb: [(slice(0, 64), kT[0:64, 2 * ci, tb * 512:(tb + 1) * 512]),
                                        (slice(64, 128), kT[64:128, 2 * ci + 1, tb * 512:(tb + 1) * 512])], b_k)
            self.lin_tm(w_in, C_SBV, 512, hT, hT_b, NT, lambda j, cb, n: v[:, j, cb:cb + n], b_v)
            e_t = [self.sb(st, "sb_e%d" % i, [128, 512], F32) for i in range(2)]
            L_t = [self.sb(st, "sb_L%d" % i, [128, 512], BF16) for i in range(2)]
            tmp = [self.sb(st, "sb_t%d" % i, [128, 512], F32) for i in range(2)]
            P_t = [self.sb(st, "sb_P%d" % i, [128, 512], BF16) for i in range(2)]
            carry = [self.sb(st, "sb_c%d" % i, [128, 512], F32) for i in range(2)]
            o_t = [self.sb(st, "sb_o%d" % i, [128, 256], BF16) for i in range(2)]
            be, bL, bt, bP, bc, bo = ([Buf() for _ in range(2)] for _ in range(6))
            tri = self.cst["tri_sb"]
            accs = [self.sb(st, "sb_acc%d" % i, [128, 256], F32) for i in range(2)]
            bacc = [Buf() for _ in range(2)]
            pipe = Pipe()
            step = 0
            it = 0
            for hg in range(2):
                for i in range(NT):
                    ci = it % 2
                    it += 1
                    for kb in range(i, -1, -1):
                        s2 = step % 2
                        step += 1

                        def stA(hg=hg, i=i, kb=kb, s2=s2):
                            zb = s2
                            ksl = slice(kb * 128, (kb + 1) * 128)
                            qsl = slice(i * 128, (i + 1) * 128)
                            for hh in range(4):
                                h = 4 * hg + hh
                                cols = slice(hh * 128, (hh + 1) * 128)
                                self.mm(self.ps[zb][:, cols], kT[:, h, ksl], qT[:, h // 2, qsl], True, kb != i,
                                        [b_q, b_k], [self.pb[zb]])
                                if kb == i:
                                    self.mm(self.ps[zb][:, cols], self.identb[:], tri[:], False, True,
                                            [self.b_const], [self.pb[zb]])
                            self.act(e_t[s2][:], self.ps[zb][:, :], AF.Exp, [self.pb[zb]], [be[s2]], scale=0.125)
                            self.act(L_t[s2][:], e_t[s2][:], AF.Ln, [be[s2], self.b_const], [bL[s2]], bias=self.onecol[:, 0:1])

                        def stB(hg=hg, i=i, kb=kb, s2=s2, ci=ci):
                            wbk, cbk = 2 + s2, 6
                            ksl = slice(kb * 128, (kb + 1) * 128)
                            qsl = slice(i * 128, (i + 1) * 128)
                            if kb == i:
                                self.ms(carry[ci][:], 0.0, [bc[ci]])
                            for hh in range(4):
                                h = 4 * hg + hh
                                cols = slice(hh * 128, (hh + 1) * 128)
                                self.mm(self.ps[wbk][:, cols], kT[:, h, ksl], qT[:, h // 2, qsl], True, False,
                                        [b_q, b_k], [self.pb[wbk]])
                                if kb == i:
                                    self.mm(self.ps[wbk][:, cols], self.identb[:], tri[:], False, False,
                                            [self.b_const], [self.pb[wbk]])
                                self.mm(self.ps[wbk][:, cols], self.cst["negut8"][:], L_t[s2][:, cols], False, True,
                                        [bL[s2], self.b_const], [self.pb[wbk]])
                            if kb > 0:
                                self.mm(self.ps[cbk][:, :], self.cst["neg8ones"][:], L_t[s2][:], True, True,
                                        [bL[s2], self.b_const], [self.pb[cbk]])
                            self.tt(tmp[s2][:], self.ps[wbk][:, :], carry[ci][:], ALU.add, [self.pb[wbk], bc[ci]], [bt[s2]])
                            self.act(P_t[s2][:], tmp[s2][:], AF.Exp, [bt[s2]], [bP[s2]], scale=0.125)
                            if kb > 0:
                                self.tt(carry[ci][:], self.ps[cbk][:, :], carry[ci][:], ALU.add, [self.pb[cbk], bc[ci]], [bc[ci]])

                        def stC(hg=hg, i=i, kb=kb, s2=s2, ci=ci):
                            pvb = 4 + s2
                            if kb == i:
                                self.ms(accs[ci][:], 0.0, [bacc[ci]])
                            for hh in range(4):
                                h = 4 * hg + hh
                                self.mm(self.ps[pvb][:, hh * 64:(hh + 1) * 64], P_t[s2][:, hh * 128:(hh + 1) * 128],
                                        v[:, kb, h * 64:(h + 1) * 64], True, True, [bP[s2], b_v], [self.pb[pvb]])
                            self.tt(accs[ci][:], accs[ci][:], self.ps[pvb][:, 0:256], ALU.add, [bacc[ci], self.pb[pvb]], [bacc[ci]])
                            if kb == 0:
                                self.cp(o_t[ci][:], accs[ci][:], [bacc[ci]], [bo[ci]], eng="pool")
                                self.dma(self.o_scr[0, i * 128:(i + 1) * 128, hg * 256:(hg + 1) * 256], o_t[ci][:], [bo[ci]], [self.B_oscr])

                        pipe.push([stA, stB, stC])
            pipe.flush()
            S.emit()

    def fox_branch(self, l, hT, hT_b):
        S = self.S
        w_in = self.w["w_in"][l]
        with contextlib.ExitStack() as st:
            v = self.sb(st, "fx_v", [128, NT, 8, 65], BF16)
            b_v = Buf()
            self.ms(v[:, :, :, 64:65], 1.0, [b_v])
            self.lin_tm(w_in, C_FV, 512, hT, hT_b, NT,
                        lambda j, cb, n: v[:, j, cb // 64:(cb + n) // 64, 0:64], b_v)
            rows = self.sb(st, "fx_rows", [8, 4, T], BF16)
            b_rows = Buf()
            with contextlib.ExitStack() as s2:
                fT = self.sb(s2, "fx_f", [8, T], F32)
                ones = self.sb(s2, "fx_ones", [8, T], F32)
                cT = self.sb(s2, "fx_c", [8, T], F32)
                hif = self.sb(s2, "fx_hif", [8, T], F32)
                bcol = self.sb(s2, "fx_b", [8, 1], F32)
                b_f, b_o, b_c, b_h, b_b = Buf(), Buf(), Buf(), Buf(), Buf()
                self.dma(bcol[:], self.w["b_fox_f"][l:l + 1, :].rearrange("o h -> h o"), [], [b_b], nc_ok=True)
                self.ms(ones[:], 1.0, [b_o])
                self.lin_fm(w_in, C_FF, 8, hT, hT_b, T, lambda ci, tb: fT[:, tb * 512:(tb + 1) * 512], b_f, chunk=8,
                            bias_fn=lambda ci: bcol[:, 0:1], bias_b=b_b)
                self.act(fT[:], fT[:], AF.Exp, [b_f, b_b], [b_f], scale=-1.0)
                self.act(fT[:], fT[:], AF.Ln, [b_f, self.b_const], [b_f], bias=self.onecol[0:8, 0:1])
                self.ts(fT[:], fT[:], -1.0, None, ALU.mult, None, [b_f], [b_f])
                S.add("dve", lambda e: e.tensor_tensor_scan(out=cT[:], data0=fT[:], data1=ones[:], initial=0.0,
                                                            op0=ALU.add, op1=ALU.mult), [b_f, b_o], [b_c])
                self.ts(cT[:], cT[:], -8.0, None, ALU.mult, None, [b_c], [b_c])
                self.cp(rows[:, 0, :], cT[:], [b_c], [b_rows])
                self.cp(hif[:], rows[:, 0, :], [b_rows], [b_h])
                self.tt(rows[:, 1, :], cT[:], hif[:], ALU.subtract, [b_c, b_h], [b_rows])
                self.ts(rows[:, 2, :], hif[:], -1.0, None, ALU.mult, None, [b_h], [b_rows])
                b_rs = Buf()
                self.cp(rows[:, 3, :], ones[:], [b_o], [b_rows])
                self.dma(self.row_scr[:, :, :], rows[:], [b_rows], [b_rs])
                S.emit()
            qa = self.sb(st, "fx_qa", [96, 4, T], BF16)
            ka = self.sb(st, "fx_ka", [96, 4, T], BF16)
            P_t = [self.sb(st, "fx_P%d" % i, [128, 512], BF16) for i in range(2)]
            bP = [Buf() for _ in range(2)]
            rden = [self.sb(st, "fx_rd%d" % i, [128, 4], F32) for i in range(2)]
            o_t = [self.sb(st, "fx_o%d" % i, [128, 4, 64], BF16) for i in range(2)]
            bo = [Buf() for _ in range(2)]
            b_q, b_k = Buf(), Buf()
            tri = self.cst["tri_c"]
            facc = [self.sb(st, "fx_acc%d" % i, [128, 260], F32) for i in range(2)]
            bfacc = [Buf() for _ in range(2)]
            for hg in range(2):
                self.ms(qa[64:96, :, :], 0.0, [b_q])
                self.ms(ka[64:96, :, :], 0.0, [b_k])
                for hh in range(4):
                    h = 4 * hg + hh
                    self.dma(ka[64:66, hh, :], self.row_scr[h, 0:2, :], [], [b_k])
                    self.dma(ka[66:67, hh, :], self.row_scr[h, 3:4, :], [], [b_k])
                    self.dma(qa[64:65, hh, :], self.row_scr[h, 3:4, :], [], [b_q])
                    self.dma(qa[65:66, hh, :], self.row_scr[h, 3:4, :], [], [b_q])
                    self.dma(qa[66:67, hh, :], self.row_scr[h, 2:3, :], [], [b_q])
                self.lin_fm(w_in, C_FQ + hg * 256, 256, hT, hT_b, T, lambda ci, tb: qa[0:64, ci, tb * 512:(tb + 1) * 512],
                            b_q, chunk=64)
                self.lin_fm(w_in, C_FK + hg * 256, 256, hT, hT_b, T, lambda ci, tb: ka[0:64, ci, tb * 512:(tb + 1) * 512],
                            b_k, chunk=64)
                step = 0
                pipe = Pipe()
                for i in range(NT):
                    ci = i % 2
                    qsl = slice(i * 128, (i + 1) * 128)
                    for kb in range(i, -1, -1):
                        s2 = step % 2
                        step += 1
                        ksl = slice(kb * 128, (kb + 1) * 128)
                        regions = []
                        for hh in range(4):
                            mms = [(ka[0:96, hh, ksl], qa[0:96, hh, qsl], [b_q, b_k])]
                            if kb == i:
                                mms.append((self.identb[:], tri[:], [self.b_const]))
                            regions.append(mms)
                        vl = [(v[:, kb, 4 * hg + hh, :], [b_v]) for hh in range(4)]
                        stA, stB0 = self.attn_stages(s2, regions, 128, P_t[s2], bP[s2], 4 + s2, 65, vl, facc[ci], bfacc[ci])

                        def stB(stB0=stB0, i=i, kb=kb, ci=ci, hg=hg):
                            if kb == i:
                                self.ms(facc[ci][:], 0.0, [bfacc[ci]])
                            stB0()
                            if kb == 0:
                                pv3 = facc[ci][:, 0:260].rearrange("p (h c) -> p h c", h=4)
                                self.rcp(rden[ci][:], pv3[:, :, 64], [bfacc[ci]], [bo[ci]])
                                self.tt(o_t[ci][:], pv3[:, :, 0:64], rden[ci][:].unsqueeze(2).to_broadcast([128, 4, 64]), ALU.mult,
                                        [bfacc[ci], bo[ci]], [bo[ci]])
                                self.dma(self.o_scr[2, i * 128:(i + 1) * 128, hg * 256:(hg + 1) * 256],
                                         o_t[ci][:].rearrange("p h c -> p (h c)"), [bo[ci]], [self.B_oscr])

                        pipe.push([stA, stB])
                pipe.flush()
                S.emit()

    def nsa_branch(self, l, hT, hT_b):
        S = self.S
        w_in = self.w["w_in"][l]
        with contextlib.ExitStack() as st:
            qT = self.sb(st, "ns_qT", [128, 4, T], BF16)
            qrT = self.sb(st, "ns_qrT", [128, 8, T], BF16)
            ksr = self.sb(st, "ns_ksr", [128, 2, T], BF16)
            kwr = self.sb(st, "ns_kwr", [128, 2, T], BF16)
            tri4 = self.sb(st, "ns_tri4", [128, 2, 512], BF16)
            rotb = self.sb(st, "ns_rotb", [128, 2, 64], BF16)
            b_tri4 = Buf()
            self.dma(tri4[:, 0, :], self.c["tri_c4"], [], [b_tri4])
            self.dma(tri4[:, 1, :], self.c["tri_band4"], [], [b_tri4])
            self.dma(rotb[:, 0, :], self.c["rot_b"], [], [b_tri4])
            self.dma(rotb[:, 1, :], self.c["sel_b"], [], [b_tri4])
            vs = self.sb(st, "ns_vs", [128, NT, 2, 65], BF16)
            vw = self.sb(st, "ns_vw", [128, NT, 2, 65], BF16)
            gates = self.sb(st, "ns_g", [128, NT, 24], F32)
            kcmpT = self.sb(st, "ns_kcmpT", [128, 2, 127], BF16)
            vcmp = self.sb(st, "ns_vcmp", [127, 2, 97], BF16)
            b_q, b_qr, b_ks, b_kw, b_vs, b_vw, b_g, b_kc, b_vc = (Buf() for _ in range(9))
            with contextlib.ExitStack() as sa:
                kcT = self.sb(sa, "ns_kcT", [128, 2, T], BF16)
                vcT = self.sb(sa, "ns_vcT", [128, 2, T], BF16)
                ksT = self.sb(sa, "ns_ksT", [128, T], BF16)
                kwT = self.sb(sa, "ns_kwT", [128, T], BF16)
                b_kcT, b_vcT, b_ksT, b_kwT = Buf(), Buf(), Buf(), Buf()
                self.lin_fm(w_in, C_NQ, 512, hT, hT_b, T, lambda ci, tb: qT[:, ci, tb * 512:(tb + 1) * 512], b_q)
                for (c0, dst, bb) in ((C_KS, ksT, b_ksT), (C_KW, kwT, b_kwT)):
                    self.lin_fm(w_in, c0, 128, hT, hT_b, T, lambda ci, tb, dst=dst: dst[:, tb * 512:(tb + 1) * 512], bb)
                for (c0, dst, bb) in ((C_KC, kcT, b_kcT), (C_VC, vcT, b_vcT)):
                    self.ms(dst[:], 0.0, [bb])
                    self.lin_fm(w_in, c0, 128, hT, hT_b, T,
                                lambda ci, tb, dst=dst: [(slice(0, 64), dst[0:64, 0, tb * 512:(tb + 1) * 512]),
                                                         (slice(64, 128), dst[64:128, 1, tb * 512:(tb + 1) * 512])], bb)
                self.ms(ksr[:], 0.0, [b_ks])
                self.ms(kwr[:], 0.0, [b_kw])
                self.ms(qrT[64:128, :, :], 0.0, [b_qr])
                for g in range(2):
                    self.dma(ksr[64:96, g, :], self.c["exrows"], [b_ks], [b_ks])
                self.ms(kcmpT[:], 0.0, [b_kc])
                self.ms(vs[:, :, :, 64:65], 1.0, [b_vs])
                self.ms(vw[:, :, :, 64:65], 1.0, [b_vw])
                self.lin_tm(w_in, C_VS, 128, hT, hT_b, NT, lambda j, cb, n: vs[:, j, :, 0:64], b_vs)
                self.lin_tm(w_in, C_VW, 128, hT, hT_b, NT, lambda j, cb, n: vw[:, j, :, 0:64], b_vw)
                self.lin_tm(w_in, C_NG, 24, hT, hT_b, NT, lambda j, cb, n: gates[:, j, :], b_g, func=AF.Sigmoid)
                sinT = self.sb(sa, "ns_sin", [128, T], BF16)
                cosT = self.sb(sa, "ns_cos", [128, T], BF16)
                invf = self.sb(sa, "ns_invf", [128, 1], F32)
                sx1 = contextlib.ExitStack()
                posi = self.sb(sx1, "ns_posi", [128, 512], I32)
                ang = self.sb(sx1, "ns_ang", [128, 512], F32)
                b_pos, b_ang, b_sin, b_cos, b_inv = Buf(), Buf(), Buf(), Buf(), Buf()
                self.dma(invf[:], self.c["invf"], [], [b_inv])
                C1 = 6.28125
                C2 = 2 * math.pi - C1
                tr_r = self.sb(sx1, "ns_trr", [128, 512], F32)
                tr_k = self.sb(sx1, "ns_trk", [128, 512], I32)
                tr_a = self.sb(sx1, "ns_tra", [128, 512], F32)
                tr_u = self.sb(sx1, "ns_tru", [128, 512], F32)
                tr_m = self.sb(sx1, "ns_trm", [128, 512], F32)
                b_tr = Buf()
                for tb in range(4):
                    sl = slice(tb * 512, (tb + 1) * 512)
                    self.dma(posi[:], self.pos_in[:, sl].partition_broadcast(128), [], [b_pos])
                    self.cp(ang[:], posi[:], [b_pos], [b_ang])
                    self.ts(ang[:], ang[:], invf[:, 0:1], None, ALU.mult, None, [b_ang, b_inv], [b_ang])
                    for (dstT, shift, bd) in ((sinT, 0.0, b_sin), (cosT, 0.5 * math.pi, b_cos)):
                        self.ts(tr_a[:], ang[:], shift, None, ALU.add, None, [b_ang], [b_tr])
                        self.ts(tr_r[:], tr_a[:], 1.0 / (2 * math.pi), None, ALU.mult, None, [b_tr], [b_tr])
                        self.cp(tr_k[:], tr_r[:], [b_tr], [b_tr])
                        self.cp(tr_r[:], tr_k[:], [b_tr], [b_tr])
                        self.stt(tr_u[:], tr_r[:], -C1, tr_a[:], ALU.mult, ALU.add, [b_tr], [b_tr])
                        self.stt(tr_u[:], tr_r[:], -C2, tr_u[:], ALU.mult, ALU.add, [b_tr], [b_tr])
                        self.ts(tr_m[:], tr_u[:], math.pi, None, ALU.is_gt, None, [b_tr], [b_tr])
                        self.stt(tr_u[:], tr_m[:], -2 * math.pi, tr_u[:], ALU.mult, ALU.add, [b_tr], [b_tr])
                        self.ts(tr_u[:], tr_u[:], -math.pi, math.pi, ALU.max, ALU.min, [b_tr], [b_tr])
                        self.act(dstT[:, sl], tr_u[:], AF.Sin, [b_tr], [bd])
                S.emit()
                sx1.close()
                sx2 = contextlib.ExitStack()
                t1 = [self.sb(sx2, "ns_t1%d" % i, [64, 512], F32) for i in range(2)]
                t2 = [self.sb(sx2, "ns_t2%d" % i, [64, 512], F32) for i in range(2)]
                bt1 = [Buf() for _ in range(2)]
                bt2 = [Buf() for _ in range(2)]
                t3 = [self.sb(sx2, "ns_t3%d" % i, [64, 512], F32) for i in range(2)]
                t4 = [self.sb(sx2, "ns_t4%d" % i, [64, 512], F32) for i in range(2)]
                bt3 = [Buf() for _ in range(2)]
                bt4 = [Buf() for _ in range(2)]
                rn = 0
                order = [0, 4, 1, 5, 2, 6, 3, 7]
                jobs = [(qT[:, c, :], (lambda sl, c=c: qrT[0:64, order[2 * c], sl]), (lambda sl, c=c: qrT[0:64, order[2 * c + 1], sl]), b_q, b_qr)
                        for c in range(4)]
                jobs.append((ksT[:], (lambda sl: ksr[0:64, 0, sl]), (lambda sl: ksr[0:64, 1, sl]), b_ksT, b_ks))
                jobs.append((kwT[:], (lambda sl: kwr[0:64, 0, sl]), (lambda sl: kwr[0:64, 1, sl]), b_kwT, b_kw))
                for (src, dsta, dstb, bs, bd) in jobs:
                    for tb in range(4):
                        k = rn % 2
                        rn += 1
                        sl = slice(tb * 512, (tb + 1) * 512)
                        self.mm(self.ps[k][0:64, :], self.cst["rotT"][:, 0:64], src[:, sl], True, True, [bs, self.b_const], [self.pb[k]])
                        self.tt(t1[k][0:64, :], self.ps[k][0:64, :], sinT[0:64, sl], ALU.mult, [self.pb[k], b_sin], [bt1[k]])
                        self.tt(t2[k][0:64, :], src[0:64, sl], cosT[0:64, sl], ALU.mult, [bs, b_cos], [bt2[k]], eng="pool")
                        self.tt(dsta(sl), t1[k][0:64, :], t2[k][0:64, :], ALU.add, [bt1[k], bt2[k]], [bd])
                        self.mm(self.ps[2 + k][0:64, :], rotb[:, 0, :], src[:, sl], True, True, [bs, b_tri4], [self.pb[2 + k]])
                        self.mm(self.ps[4 + k][0:64, :], rotb[:, 1, :], src[:, sl], True, True, [bs, b_tri4], [self.pb[4 + k]])
                        self.tt(t3[k][0:64, :], self.ps[2 + k][0:64, :], sinT[0:64, sl], ALU.mult, [self.pb[2 + k], b_sin], [bt3[k]])
                        self.tt(t4[k][0:64, :], self.ps[4 + k][0:64, :], cosT[0:64, sl], ALU.mult, [self.pb[4 + k], b_cos], [bt4[k]])
                        self.tt(dstb(sl), t3[k][0:64, :], t4[k][0:64, :], ALU.add, [bt3[k], bt4[k]], [bd])
                S.emit()
                sx2.close()
                ov_t = self.sb(sa, "ns_ov", [127, 32], BF16)
                b_ov = Buf()
                self.dma(ov_t[:], self.c["ov"], [], [b_ov])
                for g in range(2):
                    self.ms(vcmp[:, g, 64:65], 1.0, [b_vc])
                    self.cp(vcmp[:, g, 65:97], ov_t[:], [b_ov], [b_vc], eng="pool")
                w1 = self.sb(sa, "ns_w1", [128, 32, 256], BF16)
                w2 = self.sb(sa, "ns_w2", [128, 2, 128], BF16)
                pe2 = self.sb(sa, "ns_pe2", [32, 128], F32)
                peT = self.sb(sa, "ns_peT", [128, 32], BF16)
                b1 = self.sb(sa, "ns_b1", [128, 2], F32)
                biasT = self.sb(sa, "ns_biasT", [128, 2], F32)
                hidT = self.sb(sa, "ns_hidT", [128, 2, 127], BF16)
                identf = self.sb(sa, "ns_idf", [128, 128], F32)
                b_w1, b_w2, b_pe, b_peT, b_b1, b_bias, b_hid, b_idf = (Buf() for _ in range(8))
                self.dma(identf[:], self.c["identf"], [], [b_idf])
                for which, srcT, b_src in (("k", kcT, b_kcT), ("v", vcT, b_vcT)):
                    w1d = self.w["cmp_w1_" + which][l]
                    for half in range(2):
                        for l0 in range(0, 32, 8):
                            src = w1d[l0 * 64:(l0 + 8) * 64, :]
                            self.wload(src, 0, 8, 0, 256, dst=w1[half * 64:(half + 1) * 64, l0:l0 + 8, :], dst_b=b_w1, prows=64)
                    w2d = self.w["cmp_w2_" + which][l]
                    for dup in range(2):
                        self.wload(w2d, 0, 2, 0, 64, dst=w2[:, :, dup * 64:(dup + 1) * 64], dst_b=b_w2)
                    for dup in range(2):
                        self.dma(pe2[:, dup * 64:(dup + 1) * 64], self.w["cmp_pe_" + which][l], [], [b_pe])
                    self.tr(self.ps[2][:, 0:32], pe2[:, :], identf[0:32, 0:32], [b_pe, b_idf], [self.pb[2]])
                    self.act(peT[:], self.ps[2][:, 0:32], AF.Identity, [self.pb[2]], [b_peT])
                    self.dma(b1[:], self.w["cmp_b1_" + which][l:l + 1, :].rearrange("o (c p) -> p (o c)", p=128), [], [b_b1], nc_ok=True)
                    for hc in range(2):
                        for ll in range(32):
                            self.mm(self.ps[3][:, hc:hc + 1], w1[0:64, ll, hc * 128:(hc + 1) * 128], peT[0:64, ll:ll + 1],
                                    ll == 0, ll == 31, [b_w1, b_peT], [self.pb[3]])
                    self.tt(biasT[:], self.ps[3][:, 0:2], b1[:], ALU.add, [self.pb[3], b_b1], [b_bias])
                    for g in range(2):
                        base = 64 * g
                        for hc in range(2):
                            bk = hc
                            for ll in range(32):
                                self.mm(self.ps[bk][:, 0:127], w1[:, ll, hc * 128:(hc + 1) * 128],
                                        srcT[:, g, ll:ll + 16 * 126 + 1:16], ll == 0, ll == 31,
                                        [b_w1, b_src], [self.pb[bk]])
                            self.act(hidT[:, hc, :], self.ps[bk][:, 0:127], AF.Silu, [self.pb[bk], b_bias], [b_hid],
                                     bias=biasT[:, hc:hc + 1])
                        if which == "k":
                            for hc in range(2):
                                self.mm(self.ps[2][:, 0:127], w2[:, hc, :], hidT[:, hc, :], hc == 0, hc == 1,
                                        [b_w2, b_hid], [self.pb[2]])
                            self.act(kcmpT[base:base + 64, g, :], self.ps[2][base:base + 64, 0:127], AF.Identity,
                                     [self.pb[2]], [b_kc])
                        else:
                            for hc in range(2):
                                self.mm(self.ps[2][0:127, 0:64], hidT[:, hc, :], w2[:, hc, 0:64], hc == 0, hc == 1,
                                        [b_w2, b_hid], [self.pb[2]])
                            self.act(vcmp[:, g, 0:64], self.ps[2][0:127, 0:64], AF.Identity, [self.pb[2]], [b_vc])
                S.emit()
            cmpb = self.sb(st, "ns_cmpb", [127, T], BF16)
            vm = self.sb(st, "ns_vm", [128, 16, 32], F32)
            addc = self.sb(st, "ns_addc", [128, 16, 32], F32)
            b_k2 = Buf()
            self.dma(cmpb[:], self.c["cmpbias"], [], [b_k2])
            self.dma(vm[:], self.c["vm"], [], [b_k2])
            self.dma(addc[:], self.c["addc"], [], [b_k2])
            P_t = [self.sb(st, "ns_P%d" % i, [128, 512], BF16) for i in range(2)]
            bP = [Buf() for _ in range(2)]
            acc = [self.sb(st, "ns_acc%d" % i, [128, 8, 64], F32) for i in range(2)]
            b_acc = [Buf() for _ in range(2)]
            o_t = [self.sb(st, "ns_o%d" % i, [128, 512], BF16) for i in range(2)]
            b_o = [Buf() for _ in range(2)]
            b_rd_init = Buf()
            rd = self.sb(st, "ns_rd", [128, 4], F32)
            sc = self.sb(st, "ns_sc", [128, 4], F32)
            tmp = self.sb(st, "ns_tmp", [128, 4, 64], F32)
            tslc = self.sb(st, "ns_tslc", [128, 4, 32], F32)
            score = self.sb(st, "ns_score", [128, 32], F32)
            m8 = self.sb(st, "ns_m8", [128, 8], F32)
            selb = self.sb(st, "ns_selb", [128, 96], BF16)
            self.ms(selb[:], 0.0, [b_rd_init])
            b_rd, b_sc, b_tmp, b_tslc, b_score, b_m8, b_selb, b_selbT = (Buf() for _ in range(8))
            b_selrows = {}
            step = 0

            pacc = self.sb(st, "ns_pacc", [128, 260], F32)
            b_pacc = Buf()
            pacc2 = self.sb(st, "ns_pacc2", [128, 260], F32)
            b_pacc2 = Buf()

            def finish(src_ap, src_bufs, i, g, gi, first):
                a = acc[i % 2]
                self.ts(rd[:], src_ap[:, :, 64], 1e-30, None, ALU.max, None, src_bufs, [b_rd])
                self.rcp(rd[:], rd[:], [b_rd], [b_rd])
                gv = gates[:, i, :].rearrange("p (h t) -> p h t", t=3)[:, 4 * g:4 * g + 4, gi]
                self.tt(sc[:], rd[:], gv, ALU.mult, [b_rd, b_g], [b_sc])
                if first:
                    self.tt(a[:, 4 * g:4 * g + 4, :], src_ap[:, :, 0:64], sc[:].unsqueeze(2).to_broadcast([128, 4, 64]), ALU.mult,
                            src_bufs + [b_sc], [b_acc[i % 2]])
                else:
                    self.tt(tmp[:], src_ap[:, :, 0:64], sc[:].unsqueeze(2).to_broadcast([128, 4, 64]), ALU.mult,
                            src_bufs + [b_sc], [b_tmp])
                    self.tt(a[:, 4 * g:4 * g + 4, :], a[:, 4 * g:4 * g + 4, :], tmp[:], ALU.add, [b_tmp, b_acc[i % 2]],
                            [b_acc[i % 2]])

            for i in range(NT):
                qsl = slice(i * 128, (i + 1) * 128)
                for g in range(2):
                    s2 = step % 2
                    step += 1
                    regions = [[(kcmpT[:, g, :], qT[:, r, qsl], [b_kc, b_q]),
                                (self.identb[0:127, 0:127], cmpb[:, qsl], [self.b_const, b_k2])] for r in range(4)]
                    vl = [(vcmp[:, g, :], [b_vc]) for r in range(4)]
                    self.attn_step(s2, regions, 127, P_t[s2], bP[s2], 2, 97, vl, None, None)
                    pv3 = self.ps[2][:, 0:388].rearrange("p (h c) -> p h c", h=4)
                    finish(pv3, [self.pb[2]], i, g, 0, True)
                    self.tt(tslc[:], pv3[:, :, 65:97], rd[:].unsqueeze(2).to_broadcast([128, 4, 32]), ALU.mult,
                            [self.pb[2], b_rd], [b_tslc])
                    S.add("dve", lambda e: e.tensor_reduce(out=score[:], in_=tslc[:].rearrange("p r n -> p n r"),
                                                           axis=AX.X, op=ALU.add), [b_tslc], [b_score])
                    self.tt(score[:], score[:], vm[:, i, :], ALU.mult, [b_score, b_k2], [b_score])
                    self.tt(score[:], score[:], addc[:, i, :], ALU.add, [b_score, b_k2], [b_score])
                    S.add("dve", lambda e: e.max(out=m8[:], in_=score[:]), [b_score], [b_m8])
                    self.ts(score[:], score[:], m8[:, 7:8], None, ALU.is_ge, None, [b_score, b_m8], [b_score])
                    self.ts(selb[:, 64:96], score[:], -1.0, None, ALU.add, None, [b_score, b_rd_init], [b_selb])
                    pT = self.ps[3][:].bitcast(BF16)
                    self.tr(pT[0:96, 0:128], selb[:, :], self.identb[:], [b_selb, self.b_const], [self.pb[3]])
                    bsr = b_selrows.setdefault((i % 2, g), Buf())
                    self.act(qrT[64:96, 4 * g:4 * g + 4, qsl], pT[64:96, 0:128].unsqueeze(1).to_broadcast([32, 4, 128]), AF.Identity,
                             [self.pb[3]], [bsr])
                    pipe = Pipe()
                    kinds = (("sel", list(range(i, -1, -1)), ksr, vs, 1, b_ks, b_vs, pacc, b_pacc),
                             ("win", list(range(i, max(0, i - 4) - 1, -1)), kwr, vw, 2, b_kw, b_vw, pacc2, b_pacc2))
                    for (kind, kbs, kT_, v_, gi, bk_, bv_, pa, b_pa) in kinds:
                        self.ms(pa[:, 0:260], 0.0, [b_pa])
                        for kb in kbs:
                            s2 = step % 2
                            step += 1
                            ksl = slice(kb * 128, (kb + 1) * 128)
                            mms = [(kT_[:, g, ksl], qrT[:, 4 * g:4 * g + 4, qsl], [bk_, b_qr, bsr])]
                            if kb == i:
                                mms.append((self.identb[:], tri4[:, 0, :], [self.b_const, b_tri4]))
                            if kind == "win" and kb == i - 4:
                                mms.append((self.identb[:], tri4[:, 1, :], [self.b_const, b_tri4]))
                            regions = [(slice(0, 512), mms)]
                            vl = [(v_[:, kb, g, :], [bv_]) for r in range(4)]
                            pipe.push(self.attn_stages(s2, regions, 128, P_t[s2], bP[s2], 4 + s2, 65, vl, pa, b_pa))
                    pipe.flush()
                    for (kind, kbs, kT_, v_, gi, bk_, bv_, pa, b_pa) in kinds:
                        finish(pa[:, 0:260].rearrange("p (h c) -> p h c", h=4), [b_pa], i, g, gi, False)
                k = i % 2
                self.cp(o_t[k][:], acc[k][:].rearrange("p h c -> p (h c)"), [b_acc[k]], [b_o[k]], eng="pool")
                self.dma(self.o_scr[1, i * 128:(i + 1) * 128, :], o_t[k][:], [b_o[k]], [self.B_oscr])
            S.emit()

    def merge(self, l, hT, hT_b):
        S = self.S
        with contextlib.ExitStack() as st:
            wm = self.sb(st, "mg_wm", [128, 8, 3072], BF16)
            wup = self.sb(st, "mg_wup", [128, 3, 4, D], BF16)
            wout = self.sb(st, "mg_wout", [128, 8, D], BF16)
            b_wm, b_wup, b_wout = Buf(), Buf(), Buf()
            for cb in range(0, 3072, 256):
                self.wload(self.w["w_in"][l], 0, 8, C_MG + cb, 256, dst=wm[:, :, cb:cb + 256], dst_b=b_wm)
            for b, nm in enumerate(("w_up_sb", "w_up_nsa", "w_up_fox")):
                for cb in range(0, D, 512):
                    self.wload(self.w[nm][l], 0, 4, cb, 512, dst=wup[:, b, :, cb:cb + 512], dst_b=b_wup)
            for cb in range(0, D, 256):
                self.wload(self.w["w_out"][l], 0, 8, cb, 256, dst=wout[:, :, cb:cb + 256], dst_b=b_wout)
            gt = self.sb(st, "mg_g", [128, D], F32)
            b_gt = Buf()
            self.dma(gt[:], self.w["g_post_mix"][l:l + 1, :].partition_broadcast(128), [], [b_gt])
            post = self.post_tiles(st)
            o_in = [self.sb(st, "mg_o%d" % i, [128, 3, 512], BF16) for i in range(2)]
            oT = [self.sb(st, "mg_oT%d" % i, [128, 12, 128], BF16) for i in range(2)]
            sg = self.sb(st, "mg_sg", [128, 512], F32)
            y = self.sb(st, "mg_y", [128, D], F32)
            ytmp = self.sb(st, "mg_yt", [128, 512], F32)
            yb = self.sb(st, "mg_yb", [128, D], BF16)
            yT = self.sb(st, "mg_yT", [128, 8, 128], BF16)
            b_oin = [Buf() for _ in range(2)]
            b_oT = [Buf() for _ in range(2)]
            b_sg, b_y, b_ytmp, b_yb, b_yT = (Buf() for _ in range(5))
            for j in range(NT):
                k = j % 2
                self.dma(o_in[k][:], self.o_scr[:, j * 128:(j + 1) * 128, :].rearrange("b p n -> p b n"), [self.B_oscr], [b_oin[k]])
                for half in range(2):
                    bk = 4 + half
                    pT = self.ps[bk][:].bitcast(BF16)
                    for c in range(6):
                        cc = half * 6 + c
                        self.tr(pT[:, c * 128:(c + 1) * 128], o_in[k][:, cc // 4, (cc % 4) * 128:(cc % 4 + 1) * 128],
                                self.identb[:], [b_oin[k], self.b_const], [self.pb[bk]])
                    self.cp(oT[k][:, half * 6:(half + 1) * 6, :], pT[:, 0:768].rearrange("p (c n) -> p c n", c=6),
                            [self.pb[bk]], [b_oT[k]])
                for cb in range(2):
                    csl = slice(cb * 512, (cb + 1) * 512)
                    for b in range(3):
                        ub, gb = 0 + (b % 2), 2 + (b % 2)
                        for kc in range(4):
                            self.mm(self.ps[ub][:, :], oT[k][:, 4 * b + kc, :], wup[:, b, kc, csl], kc == 0, kc == 3,
                                    [b_oT[k], b_wup], [self.pb[ub]])
                        for kc in range(8):
                            self.mm(self.ps[gb][:, :], hT[:, kc, j * 128:(j + 1) * 128],
                                    wm[:, kc, b * 1024 + cb * 512:b * 1024 + (cb + 1) * 512], kc == 0, kc == 7,
                                    [hT_b, b_wm], [self.pb[gb]])
                        self.act(sg[:], self.ps[gb][:, :], AF.Sigmoid, [self.pb[gb]], [b_sg])
                        if b == 0:
                            self.tt(y[:, csl], self.ps[ub][:, :], sg[:], ALU.mult, [self.pb[ub], b_sg], [b_y])
                        else:
                            self.tt(ytmp[:], self.ps[ub][:, :], sg[:], ALU.mult, [self.pb[ub], b_sg], [b_ytmp])
                            self.tt(y[:, csl], y[:, csl], ytmp[:], ALU.add, [b_y, b_ytmp], [b_y])
                self.cp(yb[:], y[:], [b_y], [b_yb], eng="pool")
                pT = self.ps[6][:].bitcast(BF16)
                for c in range(8):
                    self.tr(pT[:, c * 128:(c + 1) * 128], yb[:, c * 128:(c + 1) * 128], self.identb[:],
                            [b_yb, self.b_const], [self.pb[6]])
                self.cp(yT[:], pT[:, 0:1024].rearrange("p (c n) -> p c n", c=8), [self.pb[6]], [b_yT])
                ob = (7, 4 + (j % 2)) if False else (7, 6)
                for h2 in range(2):
                    bk = ob[h2]
                    for kc in range(8):
                        self.mm(self.ps[bk][:, :], yT[:, kc, :], wout[:, kc, h2 * 512:(h2 + 1) * 512], kc == 0, kc == 7,
                                [b_yT, b_wout], [self.pb[bk]])
                self.post_norm_add(j, ob, gt, b_gt, post)
            S.emit()

    def mem_attn(self, l, hT, hT_b):
        S = self.S
        with contextlib.ExitStack() as st:
            with contextlib.ExitStack() as s1:
                self.norm_to_hT(self.out, NT, "g_pre_mem", l, hT, hT_b, s1)
                S.emit()
            memT = self.sb(st, "mm_memT", [128, 8, 256], BF16)
            b_memT = Buf()
            with contextlib.ExitStack() as s1:
                self.norm_to_hT(self.mem_in, 2, "g_mem", l, memT, b_memT, s1)
                S.emit()
            qT = self.sb(st, "mm_qT", [128, 2, T], BF16)
            kT = self.sb(st, "mm_kT", [128, 4, 256], BF16)
            v = self.sb(st, "mm_v", [128, 2, 4, 65], BF16)
            wo = self.sb(st, "mm_wo", [128, 2, D], BF16)
            b_q, b_k, b_v, b_wo = Buf(), Buf(), Buf(), Buf()
            self.ms(v[:, :, :, 64:65], 1.0, [b_v])
            self.lin_fm(self.w["w_mem_q"][l], 0, 256, hT, hT_b, T, lambda ci, tb: qT[:, ci, tb * 512:(tb + 1) * 512], b_q)
            self.ms(kT[:], 0.0, [b_k])
            self.lin_fm(self.w["w_mem_k"][l], 0, 256, memT, b_memT, 256,
                        lambda ci, tb: [(slice(0, 64), kT[0:64, 2 * ci, :]), (slice(64, 128), kT[64:128, 2 * ci + 1, :])], b_k)
            self.lin_tm(self.w["w_mem_v"][l], 0, 256, memT, b_memT, 2, lambda j, cb, n: v[:, j, :, 0:64], b_v)
            for cb in range(0, D, 512):
                self.wload(self.w["w_mem_o"][l], 0, 2, cb, 512, dst=wo[:, :, cb:cb + 512], dst_b=b_wo)
            gt = self.sb(st, "mm_g", [128, D], F32)
            b_gt = Buf()
            self.dma(gt[:], self.w["g_post_mem"][l:l + 1, :].partition_broadcast(128), [], [b_gt])
            post = self.post_tiles(st)
            P_t = [self.sb(st, "mm_P%d" % i, [128, 512], BF16) for i in range(2)]
            bP = [Buf() for _ in range(2)]
            rden = self.sb(st, "mm_rd", [128, 4], F32)
            o_t = self.sb(st, "mm_o", [128, 4, 64], BF16)
            oT = self.sb(st, "mm_oT", [128, 2, 128], BF16)
            b_o, b_oT = Buf(), Buf()
            macc = self.sb(st, "mm_acc", [128, 260], F32)
            b_macc = Buf()
            step = 0
            for i in range(NT):
                qsl = slice(i * 128, (i + 1) * 128)
                self.ms(macc[:], 0.0, [b_macc])
                pipe = Pipe()
                for kb in range(2):
                    s2 = step % 2
                    step += 1
                    ksl = slice(kb * 128, (kb + 1) * 128)
                    regions = [[(kT[:, hh, ksl], qT[:, hh // 2, qsl], [b_q, b_k])] for hh in range(4)]
                    vl = [(v[:, kb, hh, :], [b_v]) for hh in range(4)]
                    pipe.push(self.attn_stages(s2, regions, 128, P_t[s2], bP[s2], 2 + s2, 65, vl, macc, b_macc))
                pipe.flush()
                pv3 = macc[:, 0:260].rearrange("p (h c) -> p h c", h=4)
                self.rcp(rden[:], pv3[:, :, 64], [b_macc], [b_o])
                self.tt(o_t[:], pv3[:, :, 0:64], rden[:].unsqueeze(2).to_broadcast([128, 4, 64]), ALU.mult,
                        [b_macc, b_o], [b_o])
                pT = self.ps[4][:].bitcast(BF16)
                of = o_t[:].rearrange("p h c -> p (h c)")
                for c in range(2):
                    self.tr(pT[:, c * 128:(c + 1) * 128], of[:, c * 128:(c + 1) * 128], self.identb[:],
                            [b_o, self.b_const], [self.pb[4]])
                self.cp(oT[:], pT[:, 0:256].rearrange("p (c n) -> p c n", c=2), [self.pb[4]], [b_oT])
                ob = (6, 7)
                for h2 in range(2):
                    for kc in range(2):
                        self.mm(self.ps[ob[h2]][:, :], oT[:, kc, :], wo[:, kc, h2 * 512:(h2 + 1) * 512], kc == 0, kc == 1,
                                [b_oT, b_wo], [self.pb[ob[h2]]])
                self.post_norm_add(i, ob, gt, b_gt, post)
            S.emit()

    def ffn(self, l, hT, hT_b):
        S = self.S
        with contextlib.ExitStack() as st:
            with contextlib.ExitStack() as s1:
                self.norm_to_hT(self.out, NT, "g_pre_ffn", l, hT, hT_b, s1)
                S.emit()
            NK = D_FF // 128
            wd = self.sb(st, "ff_wd", [128, NK, D], BF16)
            b_wd = Buf()
            for k0 in range(0, NK, 8):
                kn = min(8, NK - k0)
                for cb in range(0, D, 256):
                    self.wload(self.w["w_ffn_down"][l], k0 * 128, kn, cb, 256, dst=wd[:, k0:k0 + kn, cb:cb + 256], dst_b=b_wd)
            gt = self.sb(st, "ff_g", [128, D], F32)
            b_gt = Buf()
            self.dma(gt[:], self.w["g_post_ffn"][l:l + 1, :].partition_broadcast(128), [], [b_gt])
            post = self.post_tiles(st)
            aT = self.sb(st, "ff_aT", [128, NK, 1024], BF16)
            sg = [self.sb(st, "ff_sg%d" % i, [128, 512], F32) for i in range(2)]
            b_sg = [Buf() for _ in range(2)]
            for half in range(2):
                b_aT = Buf()
                t0 = half * 1024
                cnt = 0
                for c3 in range(0, NK, 2):
                    nch = min(2, NK - c3)
                    wg, wg_b = self.wload(self.w["w_ffn_gate"][l], 0, 8, c3 * 128, nch * 128)
                    wu, wu_b = self.wload(self.w["w_ffn_up"][l], 0, 8, c3 * 128, nch * 128)
                    for cc in range(nch):
                        for tb in range(2):
                            k = cnt % 2
                            cnt += 1
                            gb, ub = k, 2 + k
                            tsl = slice(t0 + tb * 512, t0 + (tb + 1) * 512)
                            for kc in range(8):
                                self.mm(self.ps[gb][:, :], wg[:, kc, cc * 128:(cc + 1) * 128], hT[:, kc, tsl], kc == 0, kc == 7,
                                        [wg_b, hT_b], [self.pb[gb]])
                            for kc in range(8):
                                self.mm(self.ps[ub][:, :], wu[:, kc, cc * 128:(cc + 1) * 128], hT[:, kc, tsl], kc == 0, kc == 7,
                                        [wu_b, hT_b], [self.pb[ub]])
                            self.act(sg[k][:], self.ps[gb][:, :], AF.Silu, [self.pb[gb]], [b_sg[k]])
                            self.tt(aT[:, c3 + cc, tb * 512:(tb + 1) * 512], self.ps[ub][:, :], sg[k][:], ALU.mult,
                                    [self.pb[ub], b_sg[k]], [b_aT])
                for jj in range(8):
                    j = half * 8 + jj
                    ob = (4 + 2 * (jj % 2), 5 + 2 * (jj % 2))
                    for h2 in range(2):
                        for kc in range(NK):
                            self.mm(self.ps[ob[h2]][:, :], aT[:, kc, jj * 128:(jj + 1) * 128], wd[:, kc, h2 * 512:(h2 + 1) * 512],
                                    kc == 0, kc == NK - 1, [b_aT, b_wd], [self.pb[ob[h2]]])
                    self.post_norm_add(j, ob, gt, b_gt, post)
            S.emit()


_CACHE = {}


def _perm_w_in(w_in):
    w = np.array(w_in, copy=True)
    order = [0, 4, 1, 5, 2, 6, 3, 7]
    src = w_in[:, :, C_NQ:C_NQ + 512].reshape(w_in.shape[0], w_in.shape[1], 8, 64)
    w[:, :, C_NQ:C_NQ + 512] = src[:, :, order, :].reshape(w_in.shape[0], w_in.shape[1], 512)
    return w


def kernel(**inputs):
    depth = DEBUG_LAYERS or DEPTH
    if "prog" not in _CACHE:
        _CACHE["prog"] = Prog(depth)
    prog = _CACHE["prog"]
    cs = _consts()
    base = {("c_" + k): v for k, v in cs.items()}
    for k in W_SHAPES:
        a = np.ascontiguousarray(np.asarray(inputs[k], dtype=np.float32))
        if k == "w_in":
            a = _perm_w_in(a)
        base[k] = a
    x = np.asarray(inputs["x"], dtype=np.float32)
    mem = np.asarray(inputs["mem"], dtype=np.float32)
    pos = np.asarray(inputs["positions"], dtype=np.int32)
    in_maps = []
    for b in range(8):
        m = dict(base)
        m["x"] = np.ascontiguousarray(x[b])
        m["mem"] = np.ascontiguousarray(mem[b])
        m["positions"] = np.ascontiguousarray(pos[b:b + 1])
        in_maps.append(m)
    res = run_bass_kernel_spmd(prog.nc, in_maps, core_ids=list(range(8)))
    return np.stack([np.asarray(r["out"], dtype=np.float32) for r in res.results], axis=0)
```

```python
import contextlib
import math
import numpy as np
import ml_dtypes
import concourse.bass as bass
import concourse.mybir as mybir
from concourse.bass_utils import run_bass_kernel_spmd

F32 = mybir.dt.float32
BF16 = mybir.dt.bfloat16
I32 = mybir.dt.int32
AF = mybir.ActivationFunctionType
ALU = mybir.AluOpType
AX = mybir.AxisListType

ENGS = ("pe", "act", "dve", "pool", "sp")
DMA_RING = 6
T = 2048
D = 1024
NT = 16
DEPTH = 2
D_IN = 7456
D_FF = 2816
BIG = 30000.0
WMAX = 2048
DEBUG_LAYERS = None


class Buf:
    __slots__ = ("name", "w", "r")

    def __init__(self, name=""):
        self.name = name
        self.w = None
        self.r = []


class Pipe:
    def __init__(self):
        self.q = []

    def push(self, stages):
        self.q.append(stages)
        n = len(self.q) - 1
        for k in range(3):
            idx = n - k
            if idx >= 0 and k < len(self.q[idx]):
                self.q[idx][k]()

    def flush(self):
        n = len(self.q)
        for extra in range(1, 3):
            for k in range(extra, 3):
                idx = n - 1 - (k - extra)
                if idx >= 0 and k < len(self.q[idx]):
                    self.q[idx][k]()
        self.q = []


class Sched:
    def __init__(self, nc, stack):
        self.nc = nc
        self.sems = {e: stack.enter_context(nc.semaphore("s_" + e)) for e in ENGS}
        self.ring = {q: [stack.enter_context(nc.semaphore("r_%s%d" % (q, i))) for i in range(DMA_RING)]
                     for q in ("sp",)}
        self.cnt = {e: 0 for e in ENGS}
        self.ring_n = {q: 0 for q in self.ring}
        self.ring_val = {q: [0] * DMA_RING for q in self.ring}
        self.reset()

    def reset(self):
        self.ops = []
        self.touched = {}

    def add(self, eng, fn, reads=(), writes=(), dma=False):
        import os
        if len(self.ops) >= int(os.environ.get("NOPS", "100000000")):
            return
        deps = set()
        for b in reads:
            if b.w is not None:
                deps.add(b.w)
        for b in writes:
            if b.w is not None:
                deps.add(b.w)
            deps.update(b.r)
        i = len(self.ops)
        self.ops.append(dict(eng=eng, fn=fn, deps=deps, dma=dma, sig=False))
        for b in reads:
            b.r.append(i)
            self.touched[id(b)] = b
        for b in writes:
            b.w = i
            b.r = []
            self.touched[id(b)] = b
        return i

    def emit(self):
        nc = self.nc
        ops = self.ops
        for op in ops:
            for d in op["deps"]:
                od = ops[d]
                if od["dma"] or od["eng"] != op["eng"] or op["eng"] != "pe":
                    od["sig"] = True
        for op in ops:
            e = op["eng"]
            if op["dma"]:
                n = self.ring_n[e]
                self.ring_n[e] += 1
                slot = n % DMA_RING
                prev = self.ring_val[e][slot]
                self.ring_val[e][slot] = prev + 16
                op["sem"] = self.ring[e][slot]
                op["val"] = prev + 16
                op["prev"] = prev
            elif op["sig"]:
                self.cnt[e] += 1
                op["sem"] = self.sems[e]
                op["val"] = self.cnt[e]
        per = {e: [] for e in ENGS}
        for i, op in enumerate(ops):
            per[op["eng"]].append(i)

        def run(e, engobj):
            seen = {}
            for i in per[e]:
                op = ops[i]
                waits = {}
                for d in sorted(op["deps"]):
                    od = ops[d]
                    if not od["dma"] and od["eng"] == e and e == "pe":
                        continue
                    s = od["sem"]
                    k = id(s)
                    if seen.get(k, 0) >= od["val"]:
                        continue
                    if k not in waits or waits[k][1] < od["val"]:
                        waits[k] = (s, od["val"])
                if op["dma"] and op["prev"] > 0:
                    s = op["sem"]
                    k = id(s)
                    if seen.get(k, 0) < op["prev"]:
                        if k not in waits or waits[k][1] < op["prev"]:
                            waits[k] = (s, op["prev"])
                for k, (s, v) in waits.items():
                    engobj.wait_ge(s, v)
                    seen[k] = v
                ins = op["fn"](engobj)
                if op["dma"]:
                    ins.then_inc(op["sem"], 16)
                elif op["sig"]:
                    ins.then_inc(op["sem"], 1)
            if e in self.ring:
                for slot in range(DMA_RING):
                    v = self.ring_val[e][slot]
                    if v > 0 and seen.get(id(self.ring[e][slot]), 0) < v:
                        engobj.wait_ge(self.ring[e][slot], v)

        with nc.Block() as block:
            @block.tensor
            def _(eng):
                run("pe", eng)

            @block.scalar
            def _(eng):
                run("act", eng)

            @block.vector
            def _(eng):
                run("dve", eng)

            @block.gpsimd
            def _(eng):
                run("pool", eng)

            @block.sync
            def _(eng):
                run("sp", eng)
        for b in self.touched.values():
            b.w = None
            b.r = []
        self.reset()


def _consts():
    bf = ml_dtypes.bfloat16
    j = np.arange(128)[:, None]
    t = np.arange(128)[None, :]
    c = {}
    c["identb"] = np.eye(128, dtype=np.float32).astype(bf)
    c["identf"] = np.eye(128, dtype=np.float32)
    c["tri_sb"] = np.where(j >= t, -BIG, 0.0).astype(bf)
    c["tri_c"] = np.where(j > t, -BIG, 0.0).astype(bf)
    c["tri_band"] = np.where(j <= t, -BIG, 0.0).astype(bf)
    c["negut8"] = np.where(j >= t, -8.0, 0.0).astype(bf)
    c["neg8ones"] = np.full((128, 128), -8.0, np.float32).astype(bf)
    rot = np.zeros((128, 128), np.float32)
    for blk in (0, 64):
        for m in range(8):
            rot[blk + m + 8, blk + m] = -1.0
            rot[blk + m, blk + m + 8] = 1.0
    c["rotT"] = rot.astype(bf)
    rot_b = np.zeros((128, 64), np.float32)
    sel_b = np.zeros((128, 64), np.float32)
    for m in range(64):
        sel_b[64 + m, m] = 1.0
    for m in range(8):
        rot_b[64 + m + 8, m] = -1.0
        rot_b[64 + m, m + 8] = 1.0
    c["rot_b"] = rot_b.astype(bf)
    c["sel_b"] = sel_b.astype(bf)
    c["tri_c4"] = np.tile(np.where(j > t, -BIG, 0.0), (1, 4)).astype(bf)
    c["tri_band4"] = np.tile(np.where(j <= t, -BIG, 0.0), (1, 4)).astype(bf)
    exr = np.zeros((32, T), np.float32)
    for key in range(T):
        exr[key // 64, key] = BIG
    c["exrows"] = exr.astype(bf)
    half = 8
    inv = (500000.0 ** (-np.arange(half, dtype=np.float32) / half)).astype(np.float32)
    invf = np.zeros((128, 1), np.float32)
    for p in range(128):
        if p % 64 < 16:
            invf[p, 0] = inv[(p % 64) % 8]
    c["invf"] = invf
    cidx = np.arange(127)[:, None]
    tt_ = np.arange(T)[None, :]
    c["cmpbias"] = np.where(16 * cidx + 31 <= tt_, 0.0, -BIG).astype(bf)
    ex = np.zeros((32, 16, 128), np.float32)
    for kb in range(16):
        for p in range(128):
            ex[2 * kb + (p >= 64), kb, p] = BIG
    c["expand"] = ex.astype(bf)
    vm = np.zeros((128, 16, 32), np.float32)
    addc = np.zeros((128, 16, 32), np.float32)
    for i in range(16):
        for p in range(128):
            tpos = 128 * i + p
            for n in range(32):
                forced = (n == 0) or (n == tpos // 64)
                valid = 64 * n <= tpos
                if forced:
                    addc[p, i, n] = 1e4
                elif valid:
                    vm[p, i, n] = 1.0
                else:
                    addc[p, i, n] = -1.0
    c["vm"] = vm
    c["addc"] = addc
    cs = np.arange(127)[:, None] * 16
    ss = np.arange(32)[None, :] * 64
    c["ov"] = ((cs < ss + 64) & (cs + 32 > ss)).astype(np.float32).astype(bf)
    return c


CONST_DT = dict(identb=BF16, identf=F32, tri_sb=BF16, tri_c=BF16, tri_band=BF16, negut8=BF16, neg8ones=BF16,
                rotT=BF16, rot_b=BF16, sel_b=BF16, tri_c4=BF16, tri_band4=BF16, exrows=BF16, invf=F32, cmpbias=BF16, expand=BF16, vm=F32, addc=F32, ov=BF16)

W_SHAPES = dict(
    g_pre_mix=[DEPTH, D], g_post_mix=[DEPTH, D], g_pre_mem=[DEPTH, D], g_mem=[DEPTH, D], g_post_mem=[DEPTH, D],
    g_pre_ffn=[DEPTH, D], g_post_ffn=[DEPTH, D], w_in=[DEPTH, D, D_IN], b_fox_f=[DEPTH, 8],
    cmp_pe_k=[DEPTH, 32, 64], cmp_w1_k=[DEPTH, 2048, 256], cmp_b1_k=[DEPTH, 256], cmp_w2_k=[DEPTH, 256, 64],
    cmp_pe_v=[DEPTH, 32, 64], cmp_w1_v=[DEPTH, 2048, 256], cmp_b1_v=[DEPTH, 256], cmp_w2_v=[DEPTH, 256, 64],
    w_up_sb=[DEPTH, 512, D], w_up_nsa=[DEPTH, 512, D], w_up_fox=[DEPTH, 512, D], w_out=[DEPTH, D, D],
    w_mem_q=[DEPTH, D, 256], w_mem_k=[DEPTH, D, 256], w_mem_v=[DEPTH, D, 256], w_mem_o=[DEPTH, 256, D],
    w_ffn_gate=[DEPTH, D, D_FF], w_ffn_up=[DEPTH, D, D_FF], w_ffn_down=[DEPTH, D_FF, D])

C_SBQ, C_SBK, C_SBV = 0, 512, 1024
C_NQ, C_KC, C_VC, C_KS, C_VS, C_KW, C_VW, C_NG = 1536, 2048, 2176, 2304, 2432, 2560, 2688, 2816
C_FQ, C_FK, C_FV, C_FF, C_MG = 2840, 3352, 3864, 4376, 4384


class Prog:
    def __init__(self, depth=DEPTH, dbg=None, stop=None):
        self.depth = depth
        self.dbg = dbg
        self.stop = stop
        nc = self.nc = bass.Bass("TRN2", target_bir_lowering=False)
        self.x_in = nc.dram_tensor("x", [T, D], F32, kind="ExternalInput").ap()
        self.mem_in = nc.dram_tensor("mem", [256, D], F32, kind="ExternalInput").ap()
        self.pos_in = nc.dram_tensor("positions", [1, T], I32, kind="ExternalInput").ap()
        self.w = {k: nc.dram_tensor(k, s, F32, kind="ExternalInput").ap() for k, s in W_SHAPES.items()}
        cs = _consts()
        self.c = {k: nc.dram_tensor("c_" + k, list(v.shape), CONST_DT[k], kind="ExternalInput").ap()
                  for k, v in cs.items()}
        self.out = nc.dram_tensor("out", [T, D], F32, kind="ExternalOutput").ap()
        self.o_scr = nc.dram_tensor("o_scr", [3, T, 512], BF16, kind=("ExternalOutput" if dbg else "Internal")).ap()
        self.row_scr = nc.dram_tensor("row_scr", [8, 4, T], BF16, kind="Internal").ap()
        with contextlib.ExitStack() as st:
            self.S = Sched(nc, st)
            self.ps = [st.enter_context(nc.psum_tensor("ps%d" % i, [128, 512], F32)) for i in range(8)]
            self.pb = [Buf("ps%d" % i) for i in range(8)]
            self.B_x = Buf("x")
            self.B_oscr = Buf("oscr")
            self.st = st
            self.build()

    def mm(self, out, lhsT, rhs, start, stop, R, W):
        self.S.add("pe", lambda e: e.matmul(out, lhsT=lhsT, rhs=rhs, start=start, stop=stop, skip_group_check=True), R, W)

    def tr(self, out, in_, ident, R, W):
        self.S.add("pe", lambda e: e.transpose(out=out, in_=in_, identity=ident), R, W)

    def act(self, out, in_, func, R, W, bias=None, scale=None, accum=None):
        kw = {}
        if bias is not None:
            kw["bias"] = bias
        if scale is not None:
            kw["scale"] = scale
        if accum is not None:
            kw["accum_out"] = accum
        self.S.add("act", lambda e: e.activation(out=out, in_=in_, func=func, **kw), R, W)

    def tt(self, out, in0, in1, op, R, W, eng="dve"):
        self.S.add(eng, lambda e: e.tensor_tensor(out=out, in0=in0, in1=in1, op=op), R, W)

    def ts(self, out, in0, s1, s2, op0, op1, R, W, eng="dve"):
        if op1 is None:
            self.S.add(eng, lambda e: e.tensor_scalar(out=out, in0=in0, scalar1=s1, scalar2=None, op0=op0), R, W)
        else:
            self.S.add(eng, lambda e: e.tensor_scalar(out=out, in0=in0, scalar1=s1, scalar2=s2, op0=op0, op1=op1), R, W)

    def stt(self, out, in0, scalar, in1, op0, op1, R, W):
        self.S.add("dve", lambda e: e.scalar_tensor_tensor(out=out, in0=in0, scalar=scalar, in1=in1, op0=op0, op1=op1), R, W)

    def cp(self, out, in_, R, W, eng="dve"):
        self.S.add(eng, lambda e: e.tensor_copy(out=out, in_=in_), R, W)

    def ms(self, ap, val, W, eng="dve"):
        self.S.add(eng, lambda e: e.memset(ap, val), [], W)

    def rcp(self, out, in_, R, W):
        self.S.add("dve", lambda e: e.reciprocal(out=out, in_=in_), R, W)

    def dma(self, out, in_, R, W, nc_ok=False):
        if nc_ok:
            self.S.add("sp", lambda q: q.dma_start(out=out, in_=in_, allow_slow_non_contiguous=True), R, W, dma=True)
        else:
            self.S.add("sp", lambda q: q.dma_start(out=out, in_=in_), R, W, dma=True)

    def sb(self, st, name, shape, dt):
        self._n = getattr(self, "_n", 0) + 1
        return st.enter_context(self.nc.sbuf_tensor("%s_%d" % (name, self._n), shape, dt))

    def winit(self, st):
        self.wst = [self.sb(st, "wst%d" % i, [128, WMAX], F32) for i in range(2)]
        self.wbf = [self.sb(st, "wbf%d" % i, [128, WMAX], BF16) for i in range(3)]
        self.wst_b = [Buf("wst%d" % i) for i in range(2)]
        self.wbf_b = [Buf("wbf%d" % i) for i in range(3)]
        self.wn = 0

    def wload(self, w2d, r0, kc, c0, n, dst=None, dst_b=None, prows=128):
        assert kc * n <= WMAX
        i = self.wn
        self.wn += 1
        stg, stg_b = self.wst[i % 2], self.wst_b[i % 2]
        src = w2d[r0:r0 + kc * prows, c0:c0 + n].rearrange("(c p) n -> p c n", p=prows)
        sview = stg[0:prows, 0:kc * n].rearrange("p (c n) -> p c n", c=kc)
        self.dma(sview, src, [], [stg_b])
        if dst is None:
            j = i % 3
            dst = self.wbf[j][0:prows, 0:kc * n].rearrange("p (c n) -> p c n", c=kc)
            dst_b = self.wbf_b[j]
        self.cp(dst, sview, [stg_b], [dst_b], eng="pool")
        return dst, dst_b

    def lin_fm(self, w2d, c0, ncols, hT, hT_b, ntok, out_fn, out_b, chunk=128, func=AF.Identity, bias_fn=None,
               banks=(6, 7), bias_b=None):
        tbw = min(512, ntok)
        ntb = ntok // tbw
        per = max(chunk, (WMAX // 8) // chunk * chunk)
        cnt = 0
        for g0 in range(0, ncols, per):
            gn = min(per, ncols - g0)
            wb, wb_b = self.wload(w2d, 0, 8, c0 + g0, gn)
            for cc in range(gn // chunk):
                ci = (g0 // chunk) + cc
                for tb in range(ntb):
                    bk = banks[cnt % len(banks)]
                    cnt += 1
                    for kc in range(8):
                        self.mm(self.ps[bk][0:chunk, 0:tbw], wb[:, kc, cc * chunk:(cc + 1) * chunk],
                                hT[:, kc, tb * tbw:(tb + 1) * tbw], kc == 0, kc == 7, [wb_b, hT_b], [self.pb[bk]])
                    outs = out_fn(ci, tb)
                    if not isinstance(outs, list):
                        outs = [(slice(0, chunk), outs)]
                    for (psl, dst) in outs:
                        self.act(dst, self.ps[bk][psl, 0:tbw], func, [self.pb[bk]] + ([bias_b] if bias_b else []), [out_b],
                                 bias=(bias_fn(ci) if bias_fn else None))

    def lin_tm(self, w2d, c0, ncols, hT, hT_b, ntiles, out_fn, out_b, func=AF.Identity, banks=(6, 7), blk=256):
        cnt = 0
        for cb in range(0, ncols, blk):
            n = min(blk, ncols - cb)
            wb, wb_b = self.wload(w2d, 0, 8, c0 + cb, n)
            for j in range(ntiles):
                bk = banks[cnt % len(banks)]
                cnt += 1
                for kc in range(8):
                    self.mm(self.ps[bk][:, 0:n], hT[:, kc, j * 128:(j + 1) * 128], wb[:, kc, 0:n],
                            kc == 0, kc == 7, [wb_b, hT_b], [self.pb[bk]])
                self.act(out_fn(j, cb, n), self.ps[bk][:, 0:n], func, [self.pb[bk]], [out_b])

    def rstd_from_ss(self, ss, rstd, b_ss, b_rstd, n=D):
        self.ts(rstd, ss, 1.0 / n, 1e-6, ALU.mult, ALU.add, [b_ss], [b_rstd])
        self.act(rstd, rstd, AF.Sqrt, [b_rstd], [b_rstd])
        self.rcp(rstd, rstd, [b_rstd], [b_rstd])

    def norm_to_hT(self, src, ntiles, gname, l, hT, hT_b, st):
        gt = self.sb(st, "n_g", [128, D], F32)
        b_g = Buf("g")
        self.dma(gt[:], self.w[gname][l:l + 1, :].partition_broadcast(128), [], [b_g])
        xs = [self.sb(st, "n_x%d" % i, [128, D], F32) for i in range(2)]
        xb = [Buf() for _ in range(2)]
        sq = self.sb(st, "n_sq", [128, D], F32)
        ssr = [self.sb(st, "n_ss%d" % i, [128, 2], F32) for i in range(2)]
        sb_ = [Buf() for _ in range(2)]
        hb = [self.sb(st, "n_h%d" % i, [128, D], BF16) for i in range(2)]
        hbb = [Buf() for _ in range(2)]
        b_sq = Buf()
        for j in range(ntiles):
            k = j % 2
            self.dma(xs[k][:], src[j * 128:(j + 1) * 128, :], [self.B_x], [xb[k]])
            self.act(sq[:], xs[k][:], AF.Square, [xb[k]], [b_sq, sb_[k]], accum=ssr[k][:, 0:1])
            self.rstd_from_ss(ssr[k][:, 0:1], ssr[k][:, 1:2], sb_[k], sb_[k])
            self.stt(hb[k][:], xs[k][:], ssr[k][:, 1:2], gt[:], ALU.mult, ALU.mult, [xb[k], sb_[k], b_g], [hbb[k]])
            bk = 4 + k
            pT = self.ps[bk][:].bitcast(BF16)
            for c in range(8):
                self.tr(pT[:, c * 128:(c + 1) * 128], hb[k][:, c * 128:(c + 1) * 128], self.identb[:],
                        [hbb[k], self.b_const], [self.pb[bk]])
            self.cp(hT[:, :, j * 128:(j + 1) * 128], pT[:, 0:1024].rearrange("p (c n) -> p c n", c=8),
                    [self.pb[bk]], [hT_b])

    def post_norm_add(self, j, banks, gt, b_g, st_tiles):
        sq, ss, xt, yt, bufs = st_tiles
        k = j % 2
        b_ss, b_x, b_y, b_sq = bufs[k]
        for h in range(2):
            self.act(sq[:, 0:512], self.ps[banks[h]][:, :], AF.Square, [self.pb[banks[h]]], [b_sq, b_ss],
                     accum=ss[k][:, h:h + 1])
        self.tt(ss[k][:, 2:3], ss[k][:, 0:1], ss[k][:, 1:2], ALU.add, [b_ss], [b_ss])
        self.rstd_from_ss(ss[k][:, 2:3], ss[k][:, 3:4], b_ss, b_ss)
        self.dma(xt[k][:], self.out[j * 128:(j + 1) * 128, :], [self.B_x], [b_x])
        for h in range(2):
            self.stt(yt[k][:, h * 512:(h + 1) * 512], self.ps[banks[h]][:, :], ss[k][:, 3:4],
                     gt[:, h * 512:(h + 1) * 512], ALU.mult, ALU.mult, [self.pb[banks[h]], b_ss, b_g], [b_y])
        self.tt(yt[k][:], yt[k][:], xt[k][:], ALU.add, [b_y, b_x], [b_y], eng="pool")
        self.dma(self.out[j * 128:(j + 1) * 128, :], yt[k][:], [b_y], [self.B_x])

    def post_tiles(self, st):
        sq = self.sb(st, "p_sq", [128, 512], F32)
        ss = [self.sb(st, "p_ss%d" % i, [128, 4], F32) for i in range(2)]
        xt = [self.sb(st, "p_x%d" % i, [128, D], F32) for i in range(2)]
        yt = [self.sb(st, "p_y%d" % i, [128, D], F32) for i in range(2)]
        bufs = [(Buf(), Buf(), Buf(), Buf()) for _ in range(2)]
        return (sq, ss, xt, yt, bufs)

    def attn_stages(self, *a, **kw):
        return [lambda: self.attn_step(*a, part="A", **kw), lambda: self.attn_step(*a, part="B", **kw)]

    def attn_step(self, lbank, regions, nk, P, P_b, pvbank, ncol, v_list, acc, acc_b, scale=0.125, part="AB"):
        S = self
        pb = self.pb[lbank]
        for r, mms in enumerate(regions if "A" in part else []):
            cols = slice(r * 128, (r + 1) * 128)
            if isinstance(mms, tuple):
                cols, mms = mms
            for idx, (lhsT, rhs, R) in enumerate(mms):
                o = self.ps[lbank][0:nk, cols]
                if len(rhs.shape) == 3:
                    o = o.rearrange("p (h q) -> p h q", h=rhs.shape[1])
                S.mm(o, lhsT, rhs, idx == 0, idx == len(mms) - 1, R, [pb])
        if "A" in part:
            S.act(P[0:nk, :], self.ps[lbank][0:nk, :], AF.Exp, [pb], [P_b], scale=scale)
        if "B" not in part:
            return
        for r, (rhs, R) in enumerate(v_list):
            S.mm(self.ps[pvbank][:, r * ncol:(r + 1) * ncol], P[0:nk, r * 128:(r + 1) * 128], rhs, True, True,
                 [P_b] + R, [self.pb[pvbank]])
        if acc is not None:
            S.tt(acc[:, 0:4 * ncol], acc[:, 0:4 * ncol], self.ps[pvbank][:, 0:4 * ncol], ALU.add,
                 [acc_b, self.pb[pvbank]], [acc_b])

    def build(self):
        nc = self.nc
        st = self.st
        S = self.S
        self.b_const = Buf("const")
        cst = {}
        for k in ("identb", "tri_sb", "tri_c", "tri_band", "negut8", "neg8ones", "rotT"):
            cst[k] = self.sb(st, "k_" + k, [128, 128], BF16)
            self.dma(cst[k][:], self.c[k], [], [self.b_const])
        self.identb = cst["identb"]
        self.cst = cst
        self.onecol = self.sb(st, "k_one", [128, 1], F32)
        self.ms(self.onecol[:], 1.0, [self.b_const])
        self.negpi = self.sb(st, "k_negpi", [128, 1], F32)
        self.ms(self.negpi[:], -math.pi, [self.b_const])
        with contextlib.ExitStack() as s0:
            xt = [self.sb(s0, "c_x%d" % i, [128, 4, D], F32) for i in range(2)]
            xb = [Buf() for _ in range(2)]
            for j in range(4):
                k = j % 2
                self.dma(xt[k][:], self.x_in[j * 512:(j + 1) * 512, :].rearrange("(c p) n -> p c n", p=128), [], [xb[k]])
                self.dma(self.out[j * 512:(j + 1) * 512, :].rearrange("(c p) n -> p c n", p=128), xt[k][:], [xb[k]], [self.B_x])
            S.emit()
        for l in range(self.depth):
            self.layer(l)

    def layer(self, l):
        S = self.S
        with contextlib.ExitStack() as sl:
            hT = self.sb(sl, "hT", [128, 8, T], BF16)
            hT_b = Buf("hT")
            self.winit(sl)
            with contextlib.ExitStack() as s1:
                self.norm_to_hT(self.out, NT, "g_pre_mix", l, hT, hT_b, s1)
                S.emit()
            for nm, fn in (("sb", self.sb_branch), ("fox", self.fox_branch), ("nsa", self.nsa_branch),
                           ("merge", self.merge), ("mem", self.mem_attn), ("ffn", self.ffn)):
                if self.stop is not None and nm not in self.stop:
                    continue
                fn(l, hT, hT_b)

    def sb_branch(self, l, hT, hT_b):
        S = self.S
        w_in = self.w["w_in"][l]
        with contextlib.ExitStack() as st:
            qT = self.sb(st, "sb_qT", [128, 4, T], BF16)
            kT = self.sb(st, "sb_kT", [128, 8, T], BF16)
            v = self.sb(st, "sb_v", [128, NT, 512], BF16)
            b_q, b_k, b_v = Buf(), Buf(), Buf()
            self.lin_fm(w_in, C_SBQ, 512, hT, hT_b, T, lambda ci, tb: qT[:, ci, tb * 512:(tb + 1) * 512], b_q)
            self.ms(kT[:], 0.0, [b_k])
            self.lin_fm(w_in, C_SBK, 512, hT, hT_b, T,
                        lambda ci, tb: [(slice(0, 64), kT[0:64, 2 * ci, tb * 512:(tb + 1) * 512]),
                                        (slice(64, 128), kT[64:128, 2 * ci + 1, tb * 512:(tb + 1) * 512])], b_k)
            self.lin_tm(w_in, C_SBV, 512, hT, hT_b, NT, lambda j, cb, n: v[:, j, cb:cb + n], b_v)
            e_t = [self.sb(st, "sb_e%d" % i, [128, 512], F32) for i in range(2)]
            L_t = [self.sb(st, "sb_L%d" % i, [128, 512], BF16) for i in range(2)]
            tmp = [self.sb(st, "sb_t%d" % i, [128, 512], F32) for i in range(2)]
            P_t = [self.sb(st, "sb_P%d" % i, [128, 512], BF16) for i in range(2)]
            carry = [self.sb(st, "sb_c%d" % i, [128, 512], F32) for i in range(2)]
            o_t = [self.sb(st, "sb_o%d" % i, [128, 256], BF16) for i in range(2)]
            be, bL, bt, bP, bc, bo = ([Buf() for _ in range(2)] for _ in range(6))
            tri = self.cst["tri_sb"]
            accs = [self.sb(st, "sb_acc%d" % i, [128, 256], F32) for i in range(2)]
            bacc = [Buf() for _ in range(2)]
            pipe = Pipe()
            step = 0
            it = 0
            for hg in range(2):
                for i in range(NT):
                    ci = it % 2
                    it += 1
                    for kb in range(i, -1, -1):
                        s2 = step % 2
                        step += 1

                        def stA(hg=hg, i=i, kb=kb, s2=s2):
                            zb = s2
                            ksl = slice(kb * 128, (kb + 1) * 128)
                            qsl = slice(i * 128, (i + 1) * 128)
                            for hh in range(4):
                                h = 4 * hg + hh
                                cols = slice(hh * 128, (hh + 1) * 128)
                                self.mm(self.ps[zb][:, cols], kT[:, h, ksl], qT[:, h // 2, qsl], True, kb != i,
                                        [b_q, b_k], [self.pb[zb]])
                                if kb == i:
                                    self.mm(self.ps[zb][:, cols], self.identb[:], tri[:], False, True,
                                            [self.b_const], [self.pb[zb]])
                            self.act(e_t[s2][:], self.ps[zb][:, :], AF.Exp, [self.pb[zb]], [be[s2]], scale=0.125)
                            self.act(L_t[s2][:], e_t[s2][:], AF.Ln, [be[s2], self.b_const], [bL[s2]], bias=self.onecol[:, 0:1])

                        def stB(hg=hg, i=i, kb=kb, s2=s2, ci=ci):
                            wbk, cbk = 2 + s2, 6
                            ksl = slice(kb * 128, (kb + 1) * 128)
                            qsl = slice(i * 128, (i + 1) * 128)
                            if kb == i:
                                self.ms(carry[ci][:], 0.0, [bc[ci]])
                            for hh in range(4):
                                h = 4 * hg + hh
                                cols = slice(hh * 128, (hh + 1) * 128)
                                self.mm(self.ps[wbk][:, cols], kT[:, h, ksl], qT[:, h // 2, qsl], True, False,
                                        [b_q, b_k], [self.pb[wbk]])
                                if kb == i:
                                    self.mm(self.ps[wbk][:, cols], self.identb[:], tri[:], False, False,
                                            [self.b_const], [self.pb[wbk]])
                                self.mm(self.ps[wbk][:, cols], self.cst["negut8"][:], L_t[s2][:, cols], False, True,
                                        [bL[s2], self.b_const], [self.pb[wbk]])
                            if kb > 0:
                                self.mm(self.ps[cbk][:, :], self.cst["neg8ones"][:], L_t[s2][:], True, True,
                                        [bL[s2], self.b_const], [self.pb[cbk]])
                            self.tt(tmp[s2][:], self.ps[wbk][:, :], carry[ci][:], ALU.add, [self.pb[wbk], bc[ci]], [bt[s2]])
                            self.act(P_t[s2][:], tmp[s2][:], AF.Exp, [bt[s2]], [bP[s2]], scale=0.125)
                            if kb > 0:
                                self.tt(carry[ci][:], self.ps[cbk][:, :], carry[ci][:], ALU.add, [self.pb[cbk], bc[ci]], [bc[ci]])

                        def stC(hg=hg, i=i, kb=kb, s2=s2, ci=ci):
                            pvb = 4 + s2
                            if kb == i:
                                self.ms(accs[ci][:], 0.0, [bacc[ci]])
                            for hh in range(4):
                                h = 4 * hg + hh
                                self.mm(self.ps[pvb][:, hh * 64:(hh + 1) * 64], P_t[s2][:, hh * 128:(hh + 1) * 128],
                                        v[:, kb, h * 64:(h + 1) * 64], True, True, [bP[s2], b_v], [self.pb[pvb]])
                            self.tt(accs[ci][:], accs[ci][:], self.ps[pvb][:, 0:256], ALU.add, [bacc[ci], self.pb[pvb]], [bacc[ci]])
                            if kb == 0:
                                self.cp(o_t[ci][:], accs[ci][:], [bacc[ci]], [bo[ci]], eng="pool")
                                self.dma(self.o_scr[0, i * 128:(i + 1) * 128, hg * 256:(hg + 1) * 256], o_t[ci][:], [bo[ci]], [self.B_oscr])

                        pipe.push([stA, stB, stC])
            pipe.flush()
            S.emit()

    def fox_branch(self, l, hT, hT_b):
        S = self.S
        w_in = self.w["w_in"][l]
        with contextlib.ExitStack() as st:
            v = self.sb(st, "fx_v", [128, NT, 8, 65], BF16)
            b_v = Buf()
            self.ms(v[:, :, :, 64:65], 1.0, [b_v])
            self.lin_tm(w_in, C_FV, 512, hT, hT_b, NT,
                        lambda j, cb, n: v[:, j, cb // 64:(cb + n) // 64, 0:64], b_v)
            rows = self.sb(st, "fx_rows", [8, 4, T], BF16)
            b_rows = Buf()
            with contextlib.ExitStack() as s2:
                fT = self.sb(s2, "fx_f", [8, T], F32)
                ones = self.sb(s2, "fx_ones", [8, T], F32)
                cT = self.sb(s2, "fx_c", [8, T], F32)
                hif = self.sb(s2, "fx_hif", [8, T], F32)
                bcol = self.sb(s2, "fx_b", [8, 1], F32)
                b_f, b_o, b_c, b_h, b_b = Buf(), Buf(), Buf(), Buf(), Buf()
                self.dma(bcol[:], self.w["b_fox_f"][l:l + 1, :].rearrange("o h -> h o"), [], [b_b], nc_ok=True)
                self.ms(ones[:], 1.0, [b_o])
                self.lin_fm(w_in, C_FF, 8, hT, hT_b, T, lambda ci, tb: fT[:, tb * 512:(tb + 1) * 512], b_f, chunk=8,
                            bias_fn=lambda ci: bcol[:, 0:1], bias_b=b_b)
                self.act(fT[:], fT[:], AF.Exp, [b_f, b_b], [b_f], scale=-1.0)
                self.act(fT[:], fT[:], AF.Ln, [b_f, self.b_const], [b_f], bias=self.onecol[0:8, 0:1])
                self.ts(fT[:], fT[:], -1.0, None, ALU.mult, None, [b_f], [b_f])
                S.add("dve", lambda e: e.tensor_tensor_scan(out=cT[:], data0=fT[:], data1=ones[:], initial=0.0,
                                                            op0=ALU.add, op1=ALU.mult), [b_f, b_o], [b_c])
                self.ts(cT[:], cT[:], -8.0, None, ALU.mult, None, [b_c], [b_c])
                self.cp(rows[:, 0, :], cT[:], [b_c], [b_rows])
                self.cp(hif[:], rows[:, 0, :], [b_rows], [b_h])
                self.tt(rows[:, 1, :], cT[:], hif[:], ALU.subtract, [b_c, b_h], [b_rows])
                self.ts(rows[:, 2, :], hif[:], -1.0, None, ALU.mult, None, [b_h], [b_rows])
                b_rs = Buf()
                self.cp(rows[:, 3, :], ones[:], [b_o], [b_rows])
                self.dma(self.row_scr[:, :, :], rows[:], [b_rows], [b_rs])
                S.emit()
            qa = self.sb(st, "fx_qa", [96, 4, T], BF16)
            ka = self.sb(st, "fx_ka", [96, 4, T], BF16)
            P_t = [self.sb(st, "fx_P%d" % i, [128, 512], BF16) for i in range(2)]
            bP = [Buf() for _ in range(2)]
            rden = [self.sb(st, "fx_rd%d" % i, [128, 4], F32) for i in range(2)]
            o_t = [self.sb(st, "fx_o%d" % i, [128, 4, 64], BF16) for i in range(2)]
            bo = [Buf() for _ in range(2)]
            b_q, b_k = Buf(), Buf()
            tri = self.cst["tri_c"]
            facc = [self.sb(st, "fx_acc%d" % i, [128, 260], F32) for i in range(2)]
            bfacc = [Buf() for _ in range(2)]
            for hg in range(2):
                self.ms(qa[64:96, :, :], 0.0, [b_q])
                self.ms(ka[64:96, :, :], 0.0, [b_k])
                for hh in range(4):
                    h = 4 * hg + hh
                    self.dma(ka[64:66, hh, :], self.row_scr[h, 0:2, :], [], [b_k])
                    self.dma(ka[66:67, hh, :], self.row_scr[h, 3:4, :], [], [b_k])
                    self.dma(qa[64:65, hh, :], self.row_scr[h, 3:4, :], [], [b_q])
                    self.dma(qa[65:66, hh, :], self.row_scr[h, 3:4, :], [], [b_q])
                    self.dma(qa[66:67, hh, :], self.row_scr[h, 2:3, :], [], [b_q])
                self.lin_fm(w_in, C_FQ + hg * 256, 256, hT, hT_b, T, lambda ci, tb: qa[0:64, ci, tb * 512:(tb + 1) * 512],
                            b_q, chunk=64)
                self.lin_fm(w_in, C_FK + hg * 256, 256, hT, hT_b, T, lambda ci, tb: ka[0:64, ci, tb * 512:(tb + 1) * 512],
                            b_k, chunk=64)
                step = 0
                pipe = Pipe()
                for i in range(NT):
                    ci = i % 2
                    qsl = slice(i * 128, (i + 1) * 128)
                    for kb in range(i, -1, -1):
                        s2 = step % 2
                        step += 1
                        ksl = slice(kb * 128, (kb + 1) * 128)
                        regions = []
                        for hh in range(4):
                            mms = [(ka[0:96, hh, ksl], qa[0:96, hh, qsl], [b_q, b_k])]
                            if kb == i:
                                mms.append((self.identb[:], tri[:], [self.b_const]))
                            regions.append(mms)
                        vl = [(v[:, kb, 4 * hg + hh, :], [b_v]) for hh in range(4)]
                        stA, stB0 = self.attn_stages(s2, regions, 128, P_t[s2], bP[s2], 4 + s2, 65, vl, facc[ci], bfacc[ci])

                        def stB(stB0=stB0, i=i, kb=kb, ci=ci, hg=hg):
                            if kb == i:
                                self.ms(facc[ci][:], 0.0, [bfacc[ci]])
                            stB0()
                            if kb == 0:
                                pv3 = facc[ci][:, 0:260].rearrange("p (h c) -> p h c", h=4)
                                self.rcp(rden[ci][:], pv3[:, :, 64], [bfacc[ci]], [bo[ci]])
                                self.tt(o_t[ci][:], pv3[:, :, 0:64], rden[ci][:].unsqueeze(2).to_broadcast([128, 4, 64]), ALU.mult,
                                        [bfacc[ci], bo[ci]], [bo[ci]])
                                self.dma(self.o_scr[2, i * 128:(i + 1) * 128, hg * 256:(hg + 1) * 256],
                                         o_t[ci][:].rearrange("p h c -> p (h c)"), [bo[ci]], [self.B_oscr])

                        pipe.push([stA, stB])
                pipe.flush()
                S.emit()

    def nsa_branch(self, l, hT, hT_b):
        S = self.S
        w_in = self.w["w_in"][l]
        with contextlib.ExitStack() as st:
            qT = self.sb(st, "ns_qT", [128, 4, T], BF16)
            qrT = self.sb(st, "ns_qrT", [128, 8, T], BF16)
            ksr = self.sb(st, "ns_ksr", [128, 2, T], BF16)
            kwr = self.sb(st, "ns_kwr", [128, 2, T], BF16)
            tri4 = self.sb(st, "ns_tri4", [128, 2, 512], BF16)
            rotb = self.sb(st, "ns_rotb", [128, 2, 64], BF16)
            b_tri4 = Buf()
            self.dma(tri4[:, 0, :], self.c["tri_c4"], [], [b_tri4])
            self.dma(tri4[:, 1, :], self.c["tri_band4"], [], [b_tri4])
            self.dma(rotb[:, 0, :], self.c["rot_b"], [], [b_tri4])
            self.dma(rotb[:, 1, :], self.c["sel_b"], [], [b_tri4])
            vs = self.sb(st, "ns_vs", [128, NT, 2, 65], BF16)
            vw = self.sb(st, "ns_vw", [128, NT, 2, 65], BF16)
            gates = self.sb(st, "ns_g", [128, NT, 24], F32)
            kcmpT = self.sb(st, "ns_kcmpT", [128, 2, 127], BF16)
            vcmp = self.sb(st, "ns_vcmp", [127, 2, 97], BF16)
            b_q, b_qr, b_ks, b_kw, b_vs, b_vw, b_g, b_kc, b_vc = (Buf() for _ in range(9))
            with contextlib.ExitStack() as sa:
                kcT = self.sb(sa, "ns_kcT", [128, 2, T], BF16)
                vcT = self.sb(sa, "ns_vcT", [128, 2, T], BF16)
                ksT = self.sb(sa, "ns_ksT", [128, T], BF16)
                kwT = self.sb(sa, "ns_kwT", [128, T], BF16)
                b_kcT, b_vcT, b_ksT, b_kwT = Buf(), Buf(), Buf(), Buf()
                self.lin_fm(w_in, C_NQ, 512, hT, hT_b, T, lambda ci, tb: qT[:, ci, tb * 512:(tb + 1) * 512], b_q)
                for (c0, dst, bb) in ((C_KS, ksT, b_ksT), (C_KW, kwT, b_kwT)):
                    self.lin_fm(w_in, c0, 128, hT, hT_b, T, lambda ci, tb, dst=dst: dst[:, tb * 512:(tb + 1) * 512], bb)
                for (c0, dst, bb) in ((C_KC, kcT, b_kcT), (C_VC, vcT, b_vcT)):
                    self.ms(dst[:], 0.0, [bb])
                    self.lin_fm(w_in, c0, 128, hT, hT_b, T,
                                lambda ci, tb, dst=dst: [(slice(0, 64), dst[0:64, 0, tb * 512:(tb + 1) * 512]),
                                                         (slice(64, 128), dst[64:128, 1, tb * 512:(tb + 1) * 512])], bb)
                self.ms(ksr[:], 0.0, [b_ks])
                self.ms(kwr[:], 0.0, [b_kw])
                self.ms(qrT[64:128, :, :], 0.0, [b_qr])
                for g in range(2):
                    self.dma(ksr[64:96, g, :], self.c["exrows"], [b_ks], [b_ks])
                self.ms(kcmpT[:], 0.0, [b_kc])
                self.ms(vs[:, :, :, 64:65], 1.0, [b_vs])
                self.ms(vw[:, :, :, 64:65], 1.0, [b_vw])
                self.lin_tm(w_in, C_VS, 128, hT, hT_b, NT, lambda j, cb, n: vs[:, j, :, 0:64], b_vs)
                self.lin_tm(w_in, C_VW, 128, hT, hT_b, NT, lambda j, cb, n: vw[:, j, :, 0:64], b_vw)
                self.lin_tm(w_in, C_NG, 24, hT, hT_b, NT, lambda j, cb, n: gates[:, j, :], b_g, func=AF.Sigmoid)
                sinT = self.sb(sa, "ns_sin", [128, T], BF16)
                cosT = self.sb(sa, "ns_cos", [128, T], BF16)
                invf = self.sb(sa, "ns_invf", [128, 1], F32)
                sx1 = contextlib.ExitStack()
                posi = self.sb(sx1, "ns_posi", [128, 512], I32)
                ang = self.sb(sx1, "ns_ang", [128, 512], F32)
                b_pos, b_ang, b_sin, b_cos, b_inv = Buf(), Buf(), Buf(), Buf(), Buf()
                self.dma(invf[:], self.c["invf"], [], [b_inv])
                C1 = 6.28125
                C2 = 2 * math.pi - C1
                tr_r = self.sb(sx1, "ns_trr", [128, 512], F32)
                tr_k = self.sb(sx1, "ns_trk", [128, 512], I32)
                tr_a = self.sb(sx1, "ns_tra", [128, 512], F32)
                tr_u = self.sb(sx1, "ns_tru", [128, 512], F32)
                tr_m = self.sb(sx1, "ns_trm", [128, 512], F32)
                b_tr = Buf()
                for tb in range(4):
                    sl = slice(tb * 512, (tb + 1) * 512)
                    self.dma(posi[:], self.pos_in[:, sl].partition_broadcast(128), [], [b_pos])
                    self.cp(ang[:], posi[:], [b_pos], [b_ang])
                    self.ts(ang[:], ang[:], invf[:, 0:1], None, ALU.mult, None, [b_ang, b_inv], [b_ang])
                    for (dstT, shift, bd) in ((sinT, 0.0, b_sin), (cosT, 0.5 * math.pi, b_cos)):
                        self.ts(tr_a[:], ang[:], shift, None, ALU.add, None, [b_ang], [b_tr])
                        self.ts(tr_r[:], tr_a[:], 1.0 / (2 * math.pi), None, ALU.mult, None, [b_tr], [b_tr])
                        self.cp(tr_k[:], tr_r[:], [b_tr], [b_tr])
                        self.cp(tr_r[:], tr_k[:], [b_tr], [b_tr])
                        self.stt(tr_u[:], tr_r[:], -C1, tr_a[:], ALU.mult, ALU.add, [b_tr], [b_tr])
                        self.stt(tr_u[:], tr_r[:], -C2, tr_u[:], ALU.mult, ALU.add, [b_tr], [b_tr])
                        self.ts(tr_m[:], tr_u[:], math.pi, None, ALU.is_gt, None, [b_tr], [b_tr])
                        self.stt(tr_u[:], tr_m[:], -2 * math.pi, tr_u[:], ALU.mult, ALU.add, [b_tr], [b_tr])
                        self.ts(tr_u[:], tr_u[:], -math.pi, math.pi, ALU.max, ALU.min, [b_tr], [b_tr])
                        self.act(dstT[:, sl], tr_u[:], AF.Sin, [b_tr], [bd])
                S.emit()
                sx1.close()
                sx2 = contextlib.ExitStack()
                t1 = [self.sb(sx2, "ns_t1%d" % i, [64, 512], F32) for i in range(2)]
                t2 = [self.sb(sx2, "ns_t2%d" % i, [64, 512], F32) for i in range(2)]
                bt1 = [Buf() for _ in range(2)]
                bt2 = [Buf() for _ in range(2)]
                t3 = [self.sb(sx2, "ns_t3%d" % i, [64, 512], F32) for i in range(2)]
                t4 = [self.sb(sx2, "ns_t4%d" % i, [64, 512], F32) for i in range(2)]
                bt3 = [Buf() for _ in range(2)]
                bt4 = [Buf() for _ in range(2)]
                rn = 0
                order = [0, 4, 1, 5, 2, 6, 3, 7]
                jobs = [(qT[:, c, :], (lambda sl, c=c: qrT[0:64, order[2 * c], sl]), (lambda sl, c=c: qrT[0:64, order[2 * c + 1], sl]), b_q, b_qr)
                        for c in range(4)]
                jobs.append((ksT[:], (lambda sl: ksr[0:64, 0, sl]), (lambda sl: ksr[0:64, 1, sl]), b_ksT, b_ks))
                jobs.append((kwT[:], (lambda sl: kwr[0:64, 0, sl]), (lambda sl: kwr[0:64, 1, sl]), b_kwT, b_kw))
                for (src, dsta, dstb, bs, bd) in jobs:
                    for tb in range(4):
                        k = rn % 2
                        rn += 1
                        sl = slice(tb * 512, (tb + 1) * 512)
                        self.mm(self.ps[k][0:64, :], self.cst["rotT"][:, 0:64], src[:, sl], True, True, [bs, self.b_const], [self.pb[k]])
                        self.tt(t1[k][0:64, :], self.ps[k][0:64, :], sinT[0:64, sl], ALU.mult, [self.pb[k], b_sin], [bt1[k]])
                        self.tt(t2[k][0:64, :], src[0:64, sl], cosT[0:64, sl], ALU.mult, [bs, b_cos], [bt2[k]], eng="pool")
                        self.tt(dsta(sl), t1[k][0:64, :], t2[k][0:64, :], ALU.add, [bt1[k], bt2[k]], [bd])
                        self.mm(self.ps[2 + k][0:64, :], rotb[:, 0, :], src[:, sl], True, True, [bs, b_tri4], [self.pb[2 + k]])
                        self.mm(self.ps[4 + k][0:64, :], rotb[:, 1, :], src[:, sl], True, True, [bs, b_tri4], [self.pb[4 + k]])
                        self.tt(t3[k][0:64, :], self.ps[2 + k][0:64, :], sinT[0:64, sl], ALU.mult, [self.pb[2 + k], b_sin], [bt3[k]])
                        self.tt(t4[k][0:64, :], self.ps[4 + k][0:64, :], cosT[0:64, sl], ALU.mult, [self.pb[4 + k], b_cos], [bt4[k]])
                        self.tt(dstb(sl), t3[k][0:64, :], t4[k][0:64, :], ALU.add, [bt3[k], bt4[k]], [bd])
                S.emit()
                sx2.close()
                ov_t = self.sb(sa, "ns_ov", [127, 32], BF16)
                b_ov = Buf()
                self.dma(ov_t[:], self.c["ov"], [], [b_ov])
                for g in range(2):
                    self.ms(vcmp[:, g, 64:65], 1.0, [b_vc])
                    self.cp(vcmp[:, g, 65:97], ov_t[:], [b_ov], [b_vc], eng="pool")
                w1 = self.sb(sa, "ns_w1", [128, 32, 256], BF16)
                w2 = self.sb(sa, "ns_w2", [128, 2, 128], BF16)
                pe2 = self.sb(sa, "ns_pe2", [32, 128], F32)
                peT = self.sb(sa, "ns_peT", [128, 32], BF16)
                b1 = self.sb(sa, "ns_b1", [128, 2], F32)
                biasT = self.sb(sa, "ns_biasT", [128, 2], F32)
                hidT = self.sb(sa, "ns_hidT", [128, 2, 127], BF16)
                identf = self.sb(sa, "ns_idf", [128, 128], F32)
                b_w1, b_w2, b_pe, b_peT, b_b1, b_bias, b_hid, b_idf = (Buf() for _ in range(8))
                self.dma(identf[:], self.c["identf"], [], [b_idf])
                for which, srcT, b_src in (("k", kcT, b_kcT), ("v", vcT, b_vcT)):
                    w1d = self.w["cmp_w1_" + which][l]
                    for half in range(2):
                        for l0 in range(0, 32, 8):
                            src = w1d[l0 * 64:(l0 + 8) * 64, :]
                            self.wload(src, 0, 8, 0, 256, dst=w1[half * 64:(half + 1) * 64, l0:l0 + 8, :], dst_b=b_w1, prows=64)
                    w2d = self.w["cmp_w2_" + which][l]
                    for dup in range(2):
                        self.wload(w2d, 0, 2, 0, 64, dst=w2[:, :, dup * 64:(dup + 1) * 64], dst_b=b_w2)
                    for dup in range(2):
                        self.dma(pe2[:, dup * 64:(dup + 1) * 64], self.w["cmp_pe_" + which][l], [], [b_pe])
                    self.tr(self.ps[2][:, 0:32], pe2[:, :], identf[0:32, 0:32], [b_pe, b_idf], [self.pb[2]])
                    self.act(peT[:], self.ps[2][:, 0:32], AF.Identity, [self.pb[2]], [b_peT])
                    self.dma(b1[:], self.w["cmp_b1_" + which][l:l + 1, :].rearrange("o (c p) -> p (o c)", p=128), [], [b_b1], nc_ok=True)
                    for hc in range(2):
                        for ll in range(32):
                            self.mm(self.ps[3][:, hc:hc + 1], w1[0:64, ll, hc * 128:(hc + 1) * 128], peT[0:64, ll:ll + 1],
                                    ll == 0, ll == 31, [b_w1, b_peT], [self.pb[3]])
                    self.tt(biasT[:], self.ps[3][:, 0:2], b1[:], ALU.add, [self.pb[3], b_b1], [b_bias])
                    for g in range(2):
                        base = 64 * g
                        for hc in range(2):
                            bk = hc
                            for ll in range(32):
                                self.mm(self.ps[bk][:, 0:127], w1[:, ll, hc * 128:(hc + 1) * 128],
                                        srcT[:, g, ll:ll + 16 * 126 + 1:16], ll == 0, ll == 31,
                                        [b_w1, b_src], [self.pb[bk]])
                            self.act(hidT[:, hc, :], self.ps[bk][:, 0:127], AF.Silu, [self.pb[bk], b_bias], [b_hid],
                                     bias=biasT[:, hc:hc + 1])
                        if which == "k":
                            for hc in range(2):
                                self.mm(self.ps[2][:, 0:127], w2[:, hc, :], hidT[:, hc, :], hc == 0, hc == 1,
                                        [b_w2, b_hid], [self.pb[2]])
                            self.act(kcmpT[base:base + 64, g, :], self.ps[2][base:base + 64, 0:127], AF.Identity,
                                     [self.pb[2]], [b_kc])
                        else:
                            for hc in range(2):
                                self.mm(self.ps[2][0:127, 0:64], hidT[:, hc, :], w2[:, hc, 0:64], hc == 0, hc == 1,
                                        [b_w2, b_hid], [self.pb[2]])
                            self.act(vcmp[:, g, 0:64], self.ps[2][0:127, 0:64], AF.Identity, [self.pb[2]], [b_vc])
                S.emit()
            cmpb = self.sb(st, "ns_cmpb", [127, T], BF16)
            vm = self.sb(st, "ns_vm", [128, 16, 32], F32)
            addc = self.sb(st, "ns_addc", [128, 16, 32], F32)
            b_k2 = Buf()
            self.dma(cmpb[:], self.c["cmpbias"], [], [b_k2])
            self.dma(vm[:], self.c["vm"], [], [b_k2])
            self.dma(addc[:], self.c["addc"], [], [b_k2])
            P_t = [self.sb(st, "ns_P%d" % i, [128, 512], BF16) for i in range(2)]
            bP = [Buf() for _ in range(2)]
            acc = [self.sb(st, "ns_acc%d" % i, [128, 8, 64], F32) for i in range(2)]
            b_acc = [Buf() for _ in range(2)]
            o_t = [self.sb(st, "ns_o%d" % i, [128, 512], BF16) for i in range(2)]
            b_o = [Buf() for _ in range(2)]
            b_rd_init = Buf()
            rd = self.sb(st, "ns_rd", [128, 4], F32)
            sc = self.sb(st, "ns_sc", [128, 4], F32)
            tmp = self.sb(st, "ns_tmp", [128, 4, 64], F32)
            tslc = self.sb(st, "ns_tslc", [128, 4, 32], F32)
            score = self.sb(st, "ns_score", [128, 32], F32)
            m8 = self.sb(st, "ns_m8", [128, 8], F32)
            selb = self.sb(st, "ns_selb", [128, 96], BF16)
            self.ms(selb[:], 0.0, [b_rd_init])
            b_rd, b_sc, b_tmp, b_tslc, b_score, b_m8, b_selb, b_selbT = (Buf() for _ in range(8))
            b_selrows = {}
            step = 0

            pacc = self.sb(st, "ns_pacc", [128, 260], F32)
            b_pacc = Buf()
            pacc2 = self.sb(st, "ns_pacc2", [128, 260], F32)
            b_pacc2 = Buf()

            def finish(src_ap, src_bufs, i, g, gi, first):
                a = acc[i % 2]
                self.ts(rd[:], src_ap[:, :, 64], 1e-30, None, ALU.max, None, src_bufs, [b_rd])
                self.rcp(rd[:], rd[:], [b_rd], [b_rd])
                gv = gates[:, i, :].rearrange("p (h t) -> p h t", t=3)[:, 4 * g:4 * g + 4, gi]
                self.tt(sc[:], rd[:], gv, ALU.mult, [b_rd, b_g], [b_sc])
                if first:
                    self.tt(a[:, 4 * g:4 * g + 4, :], src_ap[:, :, 0:64], sc[:].unsqueeze(2).to_broadcast([128, 4, 64]), ALU.mult,
                            src_bufs + [b_sc], [b_acc[i % 2]])
                else:
                    self.tt(tmp[:], src_ap[:, :, 0:64], sc[:].unsqueeze(2).to_broadcast([128, 4, 64]), ALU.mult,
                            src_bufs + [b_sc], [b_tmp])
                    self.tt(a[:, 4 * g:4 * g + 4, :], a[:, 4 * g:4 * g + 4, :], tmp[:], ALU.add, [b_tmp, b_acc[i % 2]],
                            [b_acc[i % 2]])

            for i in range(NT):
                qsl = slice(i * 128, (i + 1) * 128)
                for g in range(2):
                    s2 = step % 2
                    step += 1
                    regions = [[(kcmpT[:, g, :], qT[:, r, qsl], [b_kc, b_q]),
                                (self.identb[0:127, 0:127], cmpb[:, qsl], [self.b_const, b_k2])] for r in range(4)]
                    vl = [(vcmp[:, g, :], [b_vc]) for r in range(4)]
                    self.attn_step(s2, regions, 127, P_t[s2], bP[s2], 2, 97, vl, None, None)
                    pv3 = self.ps[2][:, 0:388].rearrange("p (h c) -> p h c", h=4)
                    finish(pv3, [self.pb[2]], i, g, 0, True)
                    self.tt(tslc[:], pv3[:, :, 65:97], rd[:].unsqueeze(2).to_broadcast([128, 4, 32]), ALU.mult,
                            [self.pb[2], b_rd], [b_tslc])
                    S.add("dve", lambda e: e.tensor_reduce(out=score[:], in_=tslc[:].rearrange("p r n -> p n r"),
                                                           axis=AX.X, op=ALU.add), [b_tslc], [b_score])
                    self.tt(score[:], score[:], vm[:, i, :], ALU.mult, [b_score, b_k2], [b_score])
                    self.tt(score[:], score[:], addc[:, i, :], ALU.add, [b_score, b_k2], [b_score])
                    S.add("dve", lambda e: e.max(out=m8[:], in_=score[:]), [b_score], [b_m8])
                    self.ts(score[:], score[:], m8[:, 7:8], None, ALU.is_ge, None, [b_score, b_m8], [b_score])
                    self.ts(selb[:, 64:96], score[:], -1.0, None, ALU.add, None, [b_score, b_rd_init], [b_selb])
                    pT = self.ps[3][:].bitcast(BF16)
                    self.tr(pT[0:96, 0:128], selb[:, :], self.identb[:], [b_selb, self.b_const], [self.pb[3]])
                    bsr = b_selrows.setdefault((i % 2, g), Buf())
                    self.act(qrT[64:96, 4 * g:4 * g + 4, qsl], pT[64:96, 0:128].unsqueeze(1).to_broadcast([32, 4, 128]), AF.Identity,
                             [self.pb[3]], [bsr])
                    pipe = Pipe()
                    kinds = (("win", list(range(i, max(0, i - 4) - 1, -1)), kwr, vw, 2, b_kw, b_vw, pacc2, b_pacc2),
                             ("sel", list(range(i, -1, -1)), ksr, vs, 1, b_ks, b_vs, pacc, b_pacc))
                    for (kind, kbs, kT_, v_, gi, bk_, bv_, pa, b_pa) in kinds:
                        self.ms(pa[:, 0:260], 0.0, [b_pa])
                        for kb in kbs:
                            s2 = step % 2
                            step += 1
                            ksl = slice(kb * 128, (kb + 1) * 128)
                            mms = [(kT_[:, g, ksl], qrT[:, 4 * g:4 * g + 4, qsl], [bk_, b_qr, bsr])]
                            if kb == i:
                                mms.append((self.identb[:], tri4[:, 0, :], [self.b_const, b_tri4]))
                            if kind == "win" and kb == i - 4:
                                mms.append((self.identb[:], tri4[:, 1, :], [self.b_const, b_tri4]))
                            regions = [(slice(0, 512), mms)]
                            vl = [(v_[:, kb, g, :], [bv_]) for r in range(4)]
                            pipe.push(self.attn_stages(s2, regions, 128, P_t[s2], bP[s2], 4 + s2, 65, vl, pa, b_pa))
                    pipe.flush()
                    for (kind, kbs, kT_, v_, gi, bk_, bv_, pa, b_pa) in kinds:
                        finish(pa[:, 0:260].rearrange("p (h c) -> p h c", h=4), [b_pa], i, g, gi, False)
                k = i % 2
                self.cp(o_t[k][:], acc[k][:].rearrange("p h c -> p (h c)"), [b_acc[k]], [b_o[k]], eng="pool")
                self.dma(self.o_scr[1, i * 128:(i + 1) * 128, :], o_t[k][:], [b_o[k]], [self.B_oscr])
            S.emit()

    def merge(self, l, hT, hT_b):
        S = self.S
        with contextlib.ExitStack() as st:
            wm = self.sb(st, "mg_wm", [128, 8, 3072], BF16)
            wup = self.sb(st, "mg_wup", [128, 3, 4, D], BF16)
            wout = self.sb(st, "mg_wout", [128, 8, D], BF16)
            b_wm, b_wup, b_wout = Buf(), Buf(), Buf()
            for cb in range(0, 3072, 256):
                self.wload(self.w["w_in"][l], 0, 8, C_MG + cb, 256, dst=wm[:, :, cb:cb + 256], dst_b=b_wm)
            for b, nm in enumerate(("w_up_sb", "w_up_nsa", "w_up_fox")):
                for cb in range(0, D, 512):
                    self.wload(self.w[nm][l], 0, 4, cb, 512, dst=wup[:, b, :, cb:cb + 512], dst_b=b_wup)
            for cb in range(0, D, 256):
                self.wload(self.w["w_out"][l], 0, 8, cb, 256, dst=wout[:, :, cb:cb + 256], dst_b=b_wout)
            gt = self.sb(st, "mg_g", [128, D], F32)
            b_gt = Buf()
            self.dma(gt[:], self.w["g_post_mix"][l:l + 1, :].partition_broadcast(128), [], [b_gt])
            post = self.post_tiles(st)
            o_in = [self.sb(st, "mg_o%d" % i, [128, 3, 512], BF16) for i in range(2)]
            oT = [self.sb(st, "mg_oT%d" % i, [128, 12, 128], BF16) for i in range(2)]
            sg = [self.sb(st, "mg_sg%d" % i, [128, 512], F32) for i in range(2)]
            y = [self.sb(st, "mg_y%d" % i, [128, D], F32) for i in range(2)]
            ytmp = self.sb(st, "mg_yt", [128, 512], F32)
            yb = [self.sb(st, "mg_yb%d" % i, [128, D], BF16) for i in range(2)]
            yT = [self.sb(st, "mg_yT%d" % i, [128, 8, 128], BF16) for i in range(2)]
            b_oin = [Buf() for _ in range(2)]
            b_oT = [Buf() for _ in range(2)]
            b_sg = [Buf() for _ in range(2)]
            b_y = [Buf() for _ in range(2)]
            b_yb = [Buf() for _ in range(2)]
            b_yT = [Buf() for _ in range(2)]
            b_ytmp = Buf()
            pipe = Pipe()
            for j in range(NT):
                k = j % 2

                def stA(j=j, k=k):
                    self.dma(o_in[k][:], self.o_scr[:, j * 128:(j + 1) * 128, :].rearrange("b p n -> p b n"), [self.B_oscr], [b_oin[k]])
                    for half in range(2):
                        bk = 4 + half
                        pT = self.ps[bk][:].bitcast(BF16)
                        for c in range(6):
                            cc = half * 6 + c
                            self.tr(pT[:, c * 128:(c + 1) * 128], o_in[k][:, cc // 4, (cc % 4) * 128:(cc % 4 + 1) * 128],
                                    self.identb[:], [b_oin[k], self.b_const], [self.pb[bk]])
                        self.cp(oT[k][:, half * 6:(half + 1) * 6, :], pT[:, 0:768].rearrange("p (c n) -> p c n", c=6),
                                [self.pb[bk]], [b_oT[k]])

                def stB(j=j, k=k):
                    n = 0
                    for cb in range(2):
                        csl = slice(cb * 512, (cb + 1) * 512)
                        for b in range(3):
                            ub, gb = 0 + (n % 2), 2 + (n % 2)
                            s2 = n % 2
                            n += 1
                            for kc in range(4):
                                self.mm(self.ps[ub][:, :], oT[k][:, 4 * b + kc, :], wup[:, b, kc, csl], kc == 0, kc == 3,
                                        [b_oT[k], b_wup], [self.pb[ub]])
                            for kc in range(8):
                                self.mm(self.ps[gb][:, :], hT[:, kc, j * 128:(j + 1) * 128],
                                        wm[:, kc, b * 1024 + cb * 512:b * 1024 + (cb + 1) * 512], kc == 0, kc == 7,
                                        [hT_b, b_wm], [self.pb[gb]])
                            self.act(sg[s2][:], self.ps[gb][:, :], AF.Sigmoid, [self.pb[gb]], [b_sg[s2]])
                            if b == 0:
                                self.tt(y[k][:, csl], self.ps[ub][:, :], sg[s2][:], ALU.mult, [self.pb[ub], b_sg[s2]], [b_y[k]])
                            else:
                                self.tt(ytmp[:], self.ps[ub][:, :], sg[s2][:], ALU.mult, [self.pb[ub], b_sg[s2]], [b_ytmp])
                                self.tt(y[k][:, csl], y[k][:, csl], ytmp[:], ALU.add, [b_y[k], b_ytmp], [b_y[k]], eng="pool")
                    self.cp(yb[k][:], y[k][:], [b_y[k]], [b_yb[k]], eng="pool")

                def stC(j=j, k=k):
                    pT = self.ps[6][:].bitcast(BF16)
                    for c in range(8):
                        self.tr(pT[:, c * 128:(c + 1) * 128], yb[k][:, c * 128:(c + 1) * 128], self.identb[:],
                                [b_yb[k], self.b_const], [self.pb[6]])
                    self.cp(yT[k][:], pT[:, 0:1024].rearrange("p (c n) -> p c n", c=8), [self.pb[6]], [b_yT[k]])
                    ob = (7, 6)
                    for h2 in range(2):
                        bk = ob[h2]
                        for kc in range(8):
                            self.mm(self.ps[bk][:, :], yT[k][:, kc, :], wout[:, kc, h2 * 512:(h2 + 1) * 512], kc == 0, kc == 7,
                                    [b_yT[k], b_wout], [self.pb[bk]])
                    self.post_norm_add(j, ob, gt, b_gt, post)

                pipe.push([stA, stB, stC])
            pipe.flush()
            S.emit()

    def mem_attn(self, l, hT, hT_b):
        S = self.S
        with contextlib.ExitStack() as st:
            with contextlib.ExitStack() as s1:
                self.norm_to_hT(self.out, NT, "g_pre_mem", l, hT, hT_b, s1)
                S.emit()
            memT = self.sb(st, "mm_memT", [128, 8, 256], BF16)
            b_memT = Buf()
            with contextlib.ExitStack() as s1:
                self.norm_to_hT(self.mem_in, 2, "g_mem", l, memT, b_memT, s1)
                S.emit()
            qT = self.sb(st, "mm_qT", [128, 2, T], BF16)
            kT = self.sb(st, "mm_kT", [128, 4, 256], BF16)
            v = self.sb(st, "mm_v", [128, 2, 4, 65], BF16)
            wo = self.sb(st, "mm_wo", [128, 2, D], BF16)
            b_q, b_k, b_v, b_wo = Buf(), Buf(), Buf(), Buf()
            self.ms(v[:, :, :, 64:65], 1.0, [b_v])
            self.lin_fm(self.w["w_mem_q"][l], 0, 256, hT, hT_b, T, lambda ci, tb: qT[:, ci, tb * 512:(tb + 1) * 512], b_q)
            self.ms(kT[:], 0.0, [b_k])
            self.lin_fm(self.w["w_mem_k"][l], 0, 256, memT, b_memT, 256,
                        lambda ci, tb: [(slice(0, 64), kT[0:64, 2 * ci, :]), (slice(64, 128), kT[64:128, 2 * ci + 1, :])], b_k)
            self.lin_tm(self.w["w_mem_v"][l], 0, 256, memT, b_memT, 2, lambda j, cb, n: v[:, j, :, 0:64], b_v)
            for cb in range(0, D, 512):
                self.wload(self.w["w_mem_o"][l], 0, 2, cb, 512, dst=wo[:, :, cb:cb + 512], dst_b=b_wo)
            gt = self.sb(st, "mm_g", [128, D], F32)
            b_gt = Buf()
            self.dma(gt[:], self.w["g_post_mem"][l:l + 1, :].partition_broadcast(128), [], [b_gt])
            post = self.post_tiles(st)
            P_t = [self.sb(st, "mm_P%d" % i, [128, 512], BF16) for i in range(2)]
            bP = [Buf() for _ in range(2)]
            rden = [self.sb(st, "mm_rd%d" % i, [128, 4], F32) for i in range(2)]
            o_t = [self.sb(st, "mm_o%d" % i, [128, 4, 64], BF16) for i in range(2)]
            oT = [self.sb(st, "mm_oT%d" % i, [128, 2, 128], BF16) for i in range(2)]
            b_o = [Buf() for _ in range(2)]
            b_oT = [Buf() for _ in range(2)]
            macc = [self.sb(st, "mm_acc%d" % i, [128, 260], F32) for i in range(2)]
            b_macc = [Buf() for _ in range(2)]
            step = 0
            pipe = Pipe()
            for i in range(NT):
                qsl = slice(i * 128, (i + 1) * 128)
                k = i % 2
                for kb in range(2):
                    s2 = step % 2
                    step += 1
                    ksl = slice(kb * 128, (kb + 1) * 128)
                    regions = [[(kT[:, hh, ksl], qT[:, hh // 2, qsl], [b_q, b_k])] for hh in range(4)]
                    vl = [(v[:, kb, hh, :], [b_v]) for hh in range(4)]
                    stA, stB0 = self.attn_stages(s2, regions, 128, P_t[s2], bP[s2], 2 + s2, 65, vl, macc[k], b_macc[k])

                    def stB(stB0=stB0, kb=kb, k=k):
                        if kb == 0:
                            self.ms(macc[k][:], 0.0, [b_macc[k]])
                        stB0()

                    def stC(kb=kb, k=k, i=i):
                        if kb != 1:
                            return
                        pv3 = macc[k][:, 0:260].rearrange("p (h c) -> p h c", h=4)
                        self.rcp(rden[k][:], pv3[:, :, 64], [b_macc[k]], [b_o[k]])
                        self.tt(o_t[k][:], pv3[:, :, 0:64], rden[k][:].unsqueeze(2).to_broadcast([128, 4, 64]), ALU.mult,
                                [b_macc[k], b_o[k]], [b_o[k]])
                        pT = self.ps[4][:].bitcast(BF16)
                        of = o_t[k][:].rearrange("p h c -> p (h c)")
                        for c in range(2):
                            self.tr(pT[:, c * 128:(c + 1) * 128], of[:, c * 128:(c + 1) * 128], self.identb[:],
                                    [b_o[k], self.b_const], [self.pb[4]])
                        self.cp(oT[k][:], pT[:, 0:256].rearrange("p (c n) -> p c n", c=2), [self.pb[4]], [b_oT[k]])
                        ob = (6, 7)
                        for h2 in range(2):
                            for kc in range(2):
                                self.mm(self.ps[ob[h2]][:, :], oT[k][:, kc, :], wo[:, kc, h2 * 512:(h2 + 1) * 512], kc == 0, kc == 1,
                                        [b_oT[k], b_wo], [self.pb[ob[h2]]])
                        self.post_norm_add(i, ob, gt, b_gt, post)

                    pipe.push([stA, stB, stC])
            pipe.flush()
            S.emit()

    def ffn(self, l, hT, hT_b):
        S = self.S
        with contextlib.ExitStack() as st:
            with contextlib.ExitStack() as s1:
                self.norm_to_hT(self.out, NT, "g_pre_ffn", l, hT, hT_b, s1)
                S.emit()
            NK = D_FF // 128
            wd = self.sb(st, "ff_wd", [128, NK, D], BF16)
            b_wd = Buf()
            for k0 in range(0, NK, 8):
                kn = min(8, NK - k0)
                for cb in range(0, D, 256):
                    self.wload(self.w["w_ffn_down"][l], k0 * 128, kn, cb, 256, dst=wd[:, k0:k0 + kn, cb:cb + 256], dst_b=b_wd)
            gt = self.sb(st, "ff_g", [128, D], F32)
            b_gt = Buf()
            self.dma(gt[:], self.w["g_post_ffn"][l:l + 1, :].partition_broadcast(128), [], [b_gt])
            post = self.post_tiles(st)
            aT = self.sb(st, "ff_aT", [128, NK, 1024], BF16)
            sg = [self.sb(st, "ff_sg%d" % i, [128, 512], F32) for i in range(2)]
            b_sg = [Buf() for _ in range(2)]
            for half in range(2):
                b_aT = Buf()
                t0 = half * 1024
                cnt = 0
                for c3 in range(0, NK, 2):
                    nch = min(2, NK - c3)
                    wg, wg_b = self.wload(self.w["w_ffn_gate"][l], 0, 8, c3 * 128, nch * 128)
                    wu, wu_b = self.wload(self.w["w_ffn_up"][l], 0, 8, c3 * 128, nch * 128)
                    for cc in range(nch):
                        for tb in range(2):
                            k = cnt % 2
                            cnt += 1
                            gb, ub = k, 2 + k
                            tsl = slice(t0 + tb * 512, t0 + (tb + 1) * 512)
                            for kc in range(8):
                                self.mm(self.ps[gb][:, :], wg[:, kc, cc * 128:(cc + 1) * 128], hT[:, kc, tsl], kc == 0, kc == 7,
                                        [wg_b, hT_b], [self.pb[gb]])
                            for kc in range(8):
                                self.mm(self.ps[ub][:, :], wu[:, kc, cc * 128:(cc + 1) * 128], hT[:, kc, tsl], kc == 0, kc == 7,
                                        [wu_b, hT_b], [self.pb[ub]])
                            self.act(sg[k][:], self.ps[gb][:, :], AF.Silu, [self.pb[gb]], [b_sg[k]])
                            self.tt(aT[:, c3 + cc, tb * 512:(tb + 1) * 512], self.ps[ub][:, :], sg[k][:], ALU.mult,
                                    [self.pb[ub], b_sg[k]], [b_aT])
                for jj in range(8):
                    j = half * 8 + jj
                    ob = (4 + 2 * (jj % 2), 5 + 2 * (jj % 2))
                    for h2 in range(2):
                        for kc in range(NK):
                            self.mm(self.ps[ob[h2]][:, :], aT[:, kc, jj * 128:(jj + 1) * 128], wd[:, kc, h2 * 512:(h2 + 1) * 512],
                                    kc == 0, kc == NK - 1, [b_aT, b_wd], [self.pb[ob[h2]]])
                    self.post_norm_add(j, ob, gt, b_gt, post)
            S.emit()


_CACHE = {}


def _perm_w_in(w_in):
    w = np.array(w_in, copy=True)
    order = [0, 4, 1, 5, 2, 6, 3, 7]
    src = w_in[:, :, C_NQ:C_NQ + 512].reshape(w_in.shape[0], w_in.shape[1], 8, 64)
    w[:, :, C_NQ:C_NQ + 512] = src[:, :, order, :].reshape(w_in.shape[0], w_in.shape[1], 512)
    return w


def kernel(**inputs):
    depth = DEBUG_LAYERS or DEPTH
    if "prog" not in _CACHE:
        _CACHE["prog"] = Prog(depth)
    prog = _CACHE["prog"]
    cs = _consts()
    base = {("c_" + k): v for k, v in cs.items()}
    for k in W_SHAPES:
        a = np.ascontiguousarray(np.asarray(inputs[k], dtype=np.float32))
        if k == "w_in":
            a = _perm_w_in(a)
        base[k] = a
    x = np.asarray(inputs["x"], dtype=np.float32)
    mem = np.asarray(inputs["mem"], dtype=np.float32)
    pos = np.asarray(inputs["positions"], dtype=np.int32)
    in_maps = []
    for b in range(8):
        m = dict(base)
        m["x"] = np.ascontiguousarray(x[b])
        m["mem"] = np.ascontiguousarray(mem[b])
        m["positions"] = np.ascontiguousarray(pos[b:b + 1])
        in_maps.append(m)
    res = run_bass_kernel_spmd(prog.nc, in_maps, core_ids=list(range(8)))
    return np.stack([np.asarray(r["out"], dtype=np.float32) for r in res.results], axis=0)
```
